# Optimizing a Trainium2 kernel written in Bass

```python
import jax, jax.numpy as jnp
from jax import lax
import numpy as np

D_MODEL = 1024
BATCH = 1
SEQ = 16384
DEPTH = 4
DEC_BATCH = 16
DEC_SEQ = 4096
PAST_LEN = 128

D_RNN = D_MODEL
N_LRU_BLOCKS = 4
LRU_BLOCK = D_RNN // N_LRU_BLOCKS
LRU_C = 8.0
CONV_WIDTH = 4
CONV_PAD = (2, 1)
DILATION_GROUPS = ((128, 1), (512, 4), (2048, 16))
N_GROUPS = len(DILATION_GROUPS)
HEADS_PER_GROUP = 8
HEAD_DIM = 64
ROT_DIM = HEAD_DIM // 4
ROPE_THETA = 500000.0
BLOCK_POS = 128
D_FF = 2816
N_EXPERTS = 8
TOP_K = 2
D_EXPERT = 3584
RMS_EPS = 1e-6
NEG_INF = -1e30
N_RG_LAYERS = (DEPTH + 1) // 2
N_ATTN_LAYERS = DEPTH // 2

kernel_name = "hybrid_rglru_dilated_attn_adaln_encoder"


def _rmsnorm(x, g):
    xf = x.astype(jnp.float32)
    y = xf * lax.rsqrt(jnp.mean(xf * xf, axis=-1, keepdims=True) + RMS_EPS)
    return (y * g.astype(jnp.float32)).astype(x.dtype)


def _rope_tables(s):
    inv = ROPE_THETA ** (-jnp.arange(0, ROT_DIM, 2, dtype=jnp.float32) / ROT_DIM)
    ang = jnp.arange(s, dtype=jnp.float32)[:, None] * inv[None, :]
    return jnp.cos(ang)[:, None, :], jnp.sin(ang)[:, None, :]


def _partial_rope(x, cos, sin):
    xr = x[..., :ROT_DIM].astype(jnp.float32)
    x1, x2 = jnp.split(xr, 2, axis=-1)
    rot = jnp.concatenate([x1 * cos - x2 * sin, x2 * cos + x1 * sin], axis=-1)
    return jnp.concatenate([rot.astype(x.dtype), x[..., ROT_DIM:]], axis=-1)


def _lin_comb(left, right):
    a_l, b_l = left
    a_r, b_r = right
    return a_l * a_r, a_r * b_l + b_r


def _rglru_block(h, w_in, conv_w, conv_b, gate_w, gate_b, lam, w_out):
    b, s, _ = h.shape
    xb, yb = jnp.split(h @ w_in, 2, axis=-1)
    yb = jax.nn.gelu(yb)
    xb = lax.conv_general_dilated(xb, conv_w[:, None, :], window_strides=(1,), padding=[CONV_PAD],
                                  dimension_numbers=('NWC', 'WIO', 'NWC'),
                                  feature_group_count=D_RNN) + conv_b
    xblk = xb.reshape(b, s, N_LRU_BLOCKS, LRU_BLOCK)
    pre = jnp.einsum('bsnc,zgncd->zgbsnd', xblk, gate_w).reshape(2, 2, b, s, D_RNN)
    gates = jax.nn.sigmoid((pre + gate_b[:, :, None, None, :]).astype(jnp.float32))
    r, ig = gates[:, 0], gates[:, 1]
    log_a = LRU_C * r * jax.nn.log_sigmoid(lam.astype(jnp.float32))[:, None, None, :]
    a = jnp.exp(log_a)
    u = jnp.sqrt(-jnp.expm1(2.0 * log_a)) * ig * xb.astype(jnp.float32)[None]
    _, h_fwd = lax.associative_scan(_lin_comb, (a[0], u[0]), axis=1)
    _, h_bwd = lax.associative_scan(_lin_comb, (a[1], u[1]), axis=1, reverse=True)
    y = (h_fwd + h_bwd).astype(h.dtype) * yb
    return y @ w_out


def _band_attention(q, k, v, dil, half):
    b, s, h, hd = q.shape
    L = s // dil
    n = b * dil
    qb = BLOCK_POS // dil
    nb = L // qb
    span = qb + 2 * half

    def to_sub(t):
        return t.reshape(b, L, dil, h, hd).transpose(0, 2, 1, 3, 4).reshape(n, L, h, hd)

    qs = to_sub(q).reshape(n, nb, qb, h, hd)
    pad = ((0, 0), (half, half), (0, 0), (0, 0))
    ks = jnp.pad(to_sub(k), pad)
    vs = jnp.pad(to_sub(v), pad)
    idx = jnp.arange(nb)[:, None] * qb + jnp.arange(span)[None, :]
    kb = ks[:, idx]
    vb = vs[:, idx]
    scores = jnp.einsum('nbqhd,nbkhd->nbhqk', qs, kb).astype(jnp.float32) * (HEAD_DIM ** -0.5)
    qpos = jnp.arange(nb)[:, None] * qb + jnp.arange(qb)[None, :]
    kpos = (idx - half)[:, None, :]
    valid = (jnp.abs(kpos - qpos[:, :, None]) <= half) & (kpos >= 0) & (kpos < L)
    scores = jnp.where(valid[None, :, None], scores, NEG_INF)
    m = jnp.max(scores, axis=-1, keepdims=True)
    p = jnp.exp(scores - m)
    den = jnp.sum(p, axis=-1, keepdims=True)
    out = jnp.einsum('nbhqk,nbkhd->nbqhd', (p / den).astype(v.dtype), vb).reshape(n, L, h, hd)
    lse = (m + jnp.log(den))[..., 0].transpose(0, 1, 3, 2).reshape(n, L, h)
    out = out.reshape(b, dil, L, h, hd).swapaxes(1, 2).reshape(b, s, h, hd)
    lse = lse.reshape(b, dil, L, h).swapaxes(1, 2).reshape(b, s, h)
    return out, lse


def _dilated_attention(h, w_qkv, q_norm, k_norm, w_o):
    b, s, _ = h.shape
    qkv = (h @ w_qkv).reshape(b, s, N_GROUPS, 3, HEADS_PER_GROUP, HEAD_DIM)
    cos, sin = _rope_tables(s)
    outs, lses = [], []
    for g, (window, dil) in enumerate(DILATION_GROUPS):
        q = _partial_rope(_rmsnorm(qkv[:, :, g, 0], q_norm), cos, sin)
        k = _partial_rope(_rmsnorm(qkv[:, :, g, 1], k_norm), cos, sin)
        o, lse = _band_attention(q, k, qkv[:, :, g, 2], dil, window // (2 * dil))
        outs.append(o)
        lses.append(lse)
    wts = jax.nn.softmax(jnp.stack(lses), axis=0)
    o = jnp.einsum('gbsh,gbshd->bshd', wts.astype(h.dtype), jnp.stack(outs))
    return o.reshape(b, s, HEADS_PER_GROUP * HEAD_DIM) @ w_o


def _swiglu(h, w_gu, w_down):
    g, u = jnp.split(h @ w_gu, 2, axis=-1)
    return (jax.nn.silu(g) * u) @ w_down


def _moe(h, router, w_gu, w_down):
    b, s, d = h.shape
    t = h.reshape(b * s, d)
    logits = (t @ router).astype(jnp.float32)
    top_v, top_i = lax.top_k(logits, TOP_K)
    top_w = jax.nn.softmax(top_v, axis=-1)
    combine = jnp.sum(jax.nn.one_hot(top_i, N_EXPERTS, dtype=jnp.float32) * top_w[..., None], axis=1)
    combine = combine.astype(t.dtype)
    y = jnp.zeros_like(t)
    for e in range(N_EXPERTS):
        y = y + combine[:, e:e + 1] * _swiglu(t, w_gu[e], w_down[e])
    return y.reshape(b, s, d)


def _trunk(x, c, ada_w, ada_b, norm_mix, norm_ffn, rg_w_in, rg_conv_w, rg_conv_b, rg_gate_w,
           rg_gate_b, rg_lambda, rg_w_out, at_w_qkv, at_q_norm, at_k_norm, at_w_o, ff_w_gu,
           ff_w_down, moe_router, moe_w_gu, moe_w_down):
    for i in range(DEPTH):
        j = i // 2
        mod = jax.nn.silu(c) @ ada_w[i] + ada_b[i]
        sh1, sc1, g1, sh2, sc2, g2 = jnp.split(mod[:, None, :], 6, axis=-1)
        hm = _rmsnorm(x, norm_mix[i]) * (1.0 + sc1) + sh1
        if i % 2 == 0:
            mix = _rglru_block(hm, rg_w_in[j], rg_conv_w[j], rg_conv_b[j], rg_gate_w[j],
                               rg_gate_b[j], rg_lambda[j], rg_w_out[j])
        else:
            mix = _dilated_attention(hm, at_w_qkv[j], at_q_norm[j], at_k_norm[j], at_w_o[j])
        x = x + g1 * mix
        hf = _rmsnorm(x, norm_ffn[i]) * (1.0 + sc2) + sh2
        if i % 2 == 0:
            ff = _swiglu(hf, ff_w_gu[j], ff_w_down[j])
        else:
            ff = _moe(hf, moe_router[j], moe_w_gu[j], moe_w_down[j])
        x = x + g2 * ff
    return x


def setup_inputs(seed: int = 0) -> dict:
    key = jax.random.key(seed)
    ks = jax.random.split(key, 24)
    f32 = jnp.float32
    d = D_MODEL
    nr, na = N_RG_LAYERS, N_ATTN_LAYERS
    hw = HEADS_PER_GROUP * HEAD_DIM

    def nrm(k, shape, scale):
        return jax.random.normal(k, shape, f32) * scale

    u = jax.random.uniform(ks[12], (nr, 2, D_RNN), f32, minval=0.9, maxval=0.999)
    sa = u ** (1.0 / LRU_C)
    return {
        "x_prompt": nrm(ks[0], (BATCH, SEQ, d), 1.0),
        "x_sample": nrm(ks[1], (DEC_BATCH, DEC_SEQ, d), 1.0),
        "c_prompt": nrm(ks[2], (BATCH, d), 1.0),
        "c_sample": nrm(ks[3], (DEC_BATCH, d), 1.0),
        "ada_w": nrm(ks[4], (DEPTH, d, 6 * d), 0.02),
        "ada_b": nrm(ks[5], (DEPTH, 6 * d), 0.02),
        "norm_mix": 1.0 + nrm(ks[6], (DEPTH, d), 0.05),
        "norm_ffn": 1.0 + nrm(ks[7], (DEPTH, d), 0.05),
        "rg_w_in": nrm(ks[8], (nr, d, 2 * D_RNN), d ** -0.5),
        "rg_conv_w": nrm(ks[9], (nr, CONV_WIDTH, D_RNN), CONV_WIDTH ** -0.5),
        "rg_conv_b": nrm(ks[10], (nr, D_RNN), 0.02),
        "rg_gate_w": nrm(ks[11], (nr, 2, 2, N_LRU_BLOCKS, LRU_BLOCK, LRU_BLOCK), LRU_BLOCK ** -0.5),
        "rg_gate_b": nrm(ks[13], (nr, 2, 2, D_RNN), 0.02),
        "rg_lambda": jnp.log(sa) - jnp.log1p(-sa),
        "rg_w_out": nrm(ks[14], (nr, D_RNN, d), D_RNN ** -0.5),
        "at_w_qkv": nrm(ks[15], (na, d, N_GROUPS * 3 * hw), d ** -0.5),
        "at_q_norm": 1.0 + nrm(ks[16], (na, HEAD_DIM), 0.05),
        "at_k_norm": 1.0 + nrm(ks[17], (na, HEAD_DIM), 0.05),
        "at_w_o": nrm(ks[18], (na, hw, d), hw ** -0.5),
        "ff_w_gu": nrm(ks[19], (nr, d, 2 * D_FF), d ** -0.5),
        "ff_w_down": nrm(ks[20], (nr, D_FF, d), D_FF ** -0.5),
        "moe_router": nrm(ks[21], (na, d, N_EXPERTS), d ** -0.5),
        "moe_w_gu": nrm(ks[22], (na, N_EXPERTS, d, 2 * D_EXPERT), d ** -0.5),
        "moe_w_down": nrm(ks[23], (na, N_EXPERTS, D_EXPERT, d), D_EXPERT ** -0.5),
    }


def reference(x_prompt, x_sample, c_prompt, c_sample, ada_w, ada_b, norm_mix, norm_ffn, rg_w_in,
              rg_conv_w, rg_conv_b, rg_gate_w, rg_gate_b, rg_lambda, rg_w_out, at_w_qkv, at_q_norm,
              at_k_norm, at_w_o, ff_w_gu, ff_w_down, moe_router, moe_w_gu, moe_w_down):
    y_prompt = _trunk(x_prompt, c_prompt, ada_w, ada_b, norm_mix, norm_ffn, rg_w_in, rg_conv_w,
                      rg_conv_b, rg_gate_w, rg_gate_b, rg_lambda, rg_w_out, at_w_qkv, at_q_norm,
                      at_k_norm, at_w_o, ff_w_gu, ff_w_down, moe_router, moe_w_gu, moe_w_down)
    y_sample = _trunk(x_sample, c_sample, ada_w, ada_b, norm_mix, norm_ffn, rg_w_in, rg_conv_w,
                      rg_conv_b, rg_gate_w, rg_gate_b, rg_lambda, rg_w_out, at_w_qkv, at_q_norm,
                      at_k_norm, at_w_o, ff_w_gu, ff_w_down, moe_router, moe_w_gu, moe_w_down)
    return (y_prompt, y_sample)
```

```python
import numpy as np
from contextlib import ExitStack
import concourse.bass as bass
import concourse.mybir as mybir
from concourse.bass_utils import run_bass_kernel_spmd

F32 = mybir.dt.float32
BF16 = mybir.dt.bfloat16
AF = mybir.ActivationFunctionType
ALU = mybir.AluOpType

D = 1024
KC = 8
SLOT = 4096
PAD = 1024
DFF = 2816
DEX = 3584
NEXP = 8
GROUPS = ((128, 1), (512, 4), (2048, 16))
EPS = 1e-6
GEL_C = 1.5957691216057308
SKIP_ATTN = False
SKIP_FFN = False
ATTN_MAXTILES = None
ATTN_STAGE = 9
SKIP_CAST = False
FFN_MAXST = None
MOE_STAGE = 9
SAME_SYNC = True
ATTN_GROUPS = (0, 1, 2)


class Buf:
    __slots__ = ("name", "t", "w", "r", "ds")

    def __init__(self, name, t):
        self.name = name
        self.t = t
        self.w = None
        self.r = {}
        self.ds = None


class Eng:
    def __init__(self, key, h, sem):
        self.key = key
        self.h = h
        self.sem = sem
        self.n = 0
        self.known = {}


class K:
    def __init__(self, nc, es, ndsem=70):
        self.nc = nc
        self.es = es
        self.eng = {}
        self.sems = {}
        self.total = {}
        for key, h in (("pe", nc.tensor), ("act", nc.scalar), ("dve", nc.vector),
                       ("pool", nc.gpsimd), ("sp", nc.sync)):
            sem = es.enter_context(nc.semaphore("s_" + key))
            self.eng[key] = Eng(key, h, sem)
            self.sems[key] = sem
            self.total[key] = 0
        self.free_ds = []
        for i in range(ndsem):
            k = "d%d" % i
            self.sems[k] = es.enter_context(nc.semaphore("sd%d" % i))
            self.total[k] = 0
            self.free_ds.append(k)
        self.pstack = None
        self.pbufs = []
        self.ps = []
        for i in range(8):
            t = es.enter_context(nc.psum_tensor("psb%d" % i, [128, 512], F32))
            self.ps.append(Buf("psb%d" % i, t))
        self.psi = 0
        self.uid = 0

    def gsb(self, name, shape, dt):
        t = self.es.enter_context(self.nc.sbuf_tensor("g_" + name, list(shape), dt))
        return Buf(name, t)

    def sb(self, name, shape, dt):
        self.uid += 1
        t = self.pstack.enter_context(self.nc.sbuf_tensor("p_%s_%d" % (name, self.uid), list(shape), dt))
        b = Buf(name, t)
        self.pbufs.append(b)
        return b

    def bank(self):
        b = self.ps[self.psi]
        self.psi = (self.psi + 1) % 8
        return b

    def begin(self):
        self.pstack = ExitStack()
        self.pbufs = []

    def end(self):
        self.barrier()
        for b in self.pbufs:
            if b.ds is not None:
                self.free_ds.append(b.ds)
                b.ds = None
        self.pstack.close()
        self.pstack = None
        self.pbufs = []

    def _waits(self, E, rd, wr):
        need = {}
        for b in rd:
            if b.w is not None:
                k, v = b.w
                if need.get(k, 0) < v:
                    need[k] = v
        for b in wr:
            if b.w is not None:
                k, v = b.w
                if need.get(k, 0) < v:
                    need[k] = v
            for k, v in b.r.items():
                if need.get(k, 0) < v:
                    need[k] = v
        for k, v in need.items():
            if k == E.key and (k == "pe" or not SAME_SYNC):
                continue
            if E.known.get(k, 0) >= v:
                continue
            E.h.wait_ge(self.sems[k], v)
            E.known[k] = v

    def op(self, e, fn, rd=(), wr=(), inc=True):
        E = self.eng[e]
        self._waits(E, rd, wr)
        ins = fn(E.h)
        if inc:
            E.n += 1
            self.total[E.key] = E.n
            ins.then_inc(E.sem, 1)
            tok = (E.key, E.n)
        else:
            tok = (E.key, E.n + 1)
        for b in rd:
            if b.r.get(tok[0], 0) < tok[1]:
                b.r[tok[0]] = tok[1]
        for b in wr:
            b.w = tok
            b.r = {}
        return ins

    def dma(self, out, in_, rd, wr, owner, q="sp"):
        E = self.eng[q]
        self._waits(E, rd, wr)
        if owner.ds is None:
            owner.ds = self.free_ds.pop()
        k = owner.ds
        ins = E.h.dma_start(out=out, in_=in_)
        self.total[k] += 16
        ins.then_inc(self.sems[k], 16)
        tok = (k, self.total[k])
        for b in rd:
            if b.r.get(k, 0) < tok[1]:
                b.r[k] = tok[1]
        for b in wr:
            b.w = tok
            b.r = {}

    def barrier(self):
        for E in self.eng.values():
            for k, v in self.total.items():
                if v > E.known.get(k, 0):
                    if k == E.key and k == "pe":
                        continue
                    E.h.wait_ge(self.sems[k], v)
                    E.known[k] = v


def act(k, out, in_, func, rd, wr, **kw):
    return k.op("act", lambda h: h.activation(out=out, in_=in_, func=func, **kw), rd, wr)


def ts(k, e, out, in0, s1, s2, op0, op1, rd, wr):
    if s2 is None:
        return k.op(e, lambda h: h.tensor_scalar(out=out, in0=in0, scalar1=s1, scalar2=None, op0=op0), rd, wr)
    return k.op(e, lambda h: h.tensor_scalar(out=out, in0=in0, scalar1=s1, scalar2=s2, op0=op0, op1=op1), rd, wr)


def tt(k, e, out, in0, in1, op, rd, wr):
    return k.op(e, lambda h: h.tensor_tensor(out=out, in0=in0, in1=in1, op=op), rd, wr)


def stt(k, out, in0, scalar, in1, op0, op1, rd, wr):
    return k.op("dve", lambda h: h.scalar_tensor_tensor(out=out, in0=in0, scalar=scalar, in1=in1,
                                                        op0=op0, op1=op1), rd, wr)


def cp(k, e, out, in_, rd, wr):
    if e == "act":
        return act(k, out, in_, AF.Copy, rd, wr)
    return k.op(e, lambda h: h.tensor_copy(out=out, in_=in_), rd, wr)


def mm(k, out, lhsT, rhs, start, stop, rd, wr, inc=None):
    if inc is None:
        inc = stop
    return k.op("pe", lambda h: h.matmul(out, lhsT=lhsT, rhs=rhs, start=start, stop=stop), rd, wr, inc=inc)


class Ctx:
    pass


def build(NSLOT, layers=(0, 1, 2, 3), dbg=None):
    T = NSLOT * SLOT
    TP = T + 2 * PAD
    nc = bass.Bass("TRN2", target_bir_lowering=False)
    c = Ctx()
    c.nc = nc
    c.NSLOT = NSLOT
    c.T = T

    def din(name, shape, dt=F32):
        return nc.dram_tensor(name, list(shape), dt, kind="ExternalInput").ap()

    def dint(name, shape, dt):
        return nc.dram_tensor(name, list(shape), dt, kind="Internal").ap()

    c.x_in = din("x_in", [T, D])
    c.cT = din("cT", [128, KC, NSLOT])
    c.flags = din("flags", [128, 2])
    c.ident = din("ident", [128, 128])
    c.bones = din("bones", [128, 128])
    c.pswap = din("pswap", [128, 128])
    c.bandb = din("bandb", [128, 256])
    c.rope = din("rope", [3, 2, 128, T + 128])
    c.ada_w = din("ada_w", [4, 128, KC, 6 * D])
    c.ada_bT = din("ada_bT", [128, 4, 48])
    c.ada_brep = din("ada_brep", [4, 2, 128, D])
    c.nrm = din("nrm", [128, 4, 2, KC])
    c.rg_conv = din("rg_conv", [128, 2, KC, 5])
    c.rg_gb = din("rg_gb", [128, 2, 2, 2, KC])
    c.rg_lam = din("rg_lam", [128, 2, 2, KC])
    c.at_qkn = din("at_qkn", [128, 2, 2])
    c.moe_rt = din("moe_rt", [128, 2, KC, NEXP])
    wspec = {
        "rg_w_in": [2, 128, KC * 2048],
        "rg_gate_w": [2, 128, 8192],
        "rg_w_out": [2, 128, KC * D],
        "at_qkv": [6, 128, KC * 1536],
        "at_wo": [2, 128, 4 * D],
        "ff_gu": [2 * 22, 128, KC * 256],
        "ff_dn": [2 * 22, 128, D],
    }
    for i in range(2 * NEXP):
        wspec["moe_gu%d" % i] = [28, 128, KC * 256]
        wspec["moe_dn%d" % i] = [28, 128, D]
    c.wf = {}
    c.wb = {}
    for n, shp in wspec.items():
        c.wf[n] = din(n, shp)
        c.wb[n] = dint(n + "_b", shp, BF16)
    c.y = nc.dram_tensor("y", [T, D], F32, kind="ExternalOutput").ap()
    c.X = [dint("XA", [TP, D], F32), dint("XB", [TP, D], F32)]
    c.XBs = dint("XBs", [KC, 128, T + 8], F32)
    c.GYs = dint("GYs", [KC, 128, T], BF16)
    c.XCs = dint("XCs", [KC, 128, T], F32)
    c.HFs = dint("HFs", [KC, 128, T], F32)
    c.OACC = dint("OACC", [TP, 528], F32)
    c.GREP = dint("GREP", [4, 2, NSLOT, 128, D], F32)
    if dbg is not None:
        c.dbg = nc.dram_tensor("dbg", list(dbg), F32, kind="ExternalOutput").ap()

    es = ExitStack()
    with es:
        k = K(nc, es)
        c.k = k
        c.s_ident = k.gsb("ident", [128, 128], F32)
        c.s_flags = k.gsb("flags", [128, 2], F32)
        c.s_mods = k.gsb("mods", [128, 4, 4, KC, NSLOT], F32)
        k.dma(c.s_ident.t[:], c.ident[:, :], [], [c.s_ident], c.s_ident)
        k.dma(c.s_flags.t[:], c.flags[:, :], [], [c.s_flags], c.s_flags)
        phase_init(c)
        if not SKIP_CAST:
            phase_cast(c, layers)
        phase_mods(c, layers)
        cur = 0
        for li in layers:
            j = li // 2
            if li % 2 == 0:
                phase_r1(c, li, j, cur)
                phase_r2(c, li, j)
                phase_r3(c, li, j, cur)
                phase_ffn(c, li, j, cur, moe=False)
            else:
                if not SKIP_ATTN:
                    for gi in ATTN_GROUPS:
                        phase_attn(c, li, j, gi, cur)
                    cur = 1 - cur
                if not SKIP_FFN:
                    phase_ffn(c, li, j, cur, moe=True)
        phase_out(c, cur)
    return nc


def phase_init(c):
    k = c.k
    k.begin()
    z = k.sb("z", [128, D], F32)
    k.op("pool", lambda h: h.memset(z.t[:], 0.0), [], [z])
    dX = [Buf("XA", None), Buf("XB", None)]
    T = c.T
    for xi in range(2):
        X = c.X[xi]
        for r0 in list(range(0, PAD, 128)) + list(range(PAD + T, PAD + T + PAD, 128)):
            k.dma(X[r0:r0 + 128, :], z.t[:], [z], [dX[xi]], z)
    for r0 in range(0, T, 512):
        k.dma(c.X[0][PAD + r0:PAD + r0 + 512, :], c.x_in[r0:r0 + 512, :], [], [dX[0]], z)
    k.end()


def phase_cast(c, layers):
    k = c.k
    k.begin()
    need = set()
    for li in layers:
        if li % 2 == 0:
            need |= {("rg_w_in", li // 2), ("rg_gate_w", li // 2), ("rg_w_out", li // 2),
                     ("ff_gu", li // 2), ("ff_dn", li // 2)}
        else:
            need |= {("at_qkv", li // 2), ("at_wo", li // 2)}
            if not SKIP_FFN:
                need |= {("moe_gu", li // 2), ("moe_dn", li // 2)}
    NB = 3
    st = [k.sb("cst%d" % i, [128, 4096], F32) for i in range(NB)]
    bf = [k.sb("cbf%d" % i, [128, 4096], BF16) for i in range(NB)]
    engs = ["pool", "dve", "act"]
    it = 0
    for n in c.wf:
        src = c.wf[n]
        dst = c.wb[n]
        nb, _, F = src.shape
        per = nb // 2
        ismoe = n.startswith("moe_")
        if ismoe:
            per = nb
        for jj in range(1 if ismoe else 2):
            if ismoe:
                if (n[:6], int(n[6:]) // NEXP) not in need:
                    continue
            elif (n, jj) not in need:
                continue
            if F >= 4096:
                units = [(b, f0, 1) for b in range(jj * per, (jj + 1) * per) for f0 in range(0, F, 4096)]
            else:
                g = 4096 // F
                units = []
                b = jj * per
                while b < (jj + 1) * per:
                    gg = min(g, (jj + 1) * per - b)
                    units.append((b, 0, gg))
                    b += gg
            for (b, f0, gg) in units:
                s = st[it % NB]
                d = bf[it % NB]
                e = engs[it % 3]
                it += 1
                if F >= 4096:
                    k.dma(s.t[:, :], src[b, :, f0:f0 + 4096], [], [s], s)
                    cp(k, e, d.t[:, :], s.t[:, :], [s], [d])
                    k.dma(dst[b, :, f0:f0 + 4096], d.t[:, :], [d], [], d)
                else:
                    sv = s.t[:, 0:gg * F].rearrange("p (n f) -> p n f", f=F)
                    dv = d.t[:, 0:gg * F].rearrange("p (n f) -> p n f", f=F)
                    k.dma(sv, src[b:b + gg, :, :].rearrange("n p f -> p n f"), [], [s], s)
                    cp(k, e, d.t[:, 0:gg * F], s.t[:, 0:gg * F], [s], [d])
                    k.dma(dst[b:b + gg, :, :].rearrange("n p f -> p n f"), dv, [d], [], d)
    k.end()


def phase_mods(c, layers):
    k = c.k
    NS = c.NSLOT
    k.begin()
    cT = k.sb("cT", [128, KC, NS], F32)
    sc = k.sb("sc", [128, KC, NS], F32)
    e1 = k.sb("e1", [128, KC, NS], F32)
    ones = k.sb("ones", [128, 128], F32)
    screp = k.sb("screp", [128, KC, NS, 128], F32)
    abT = k.sb("abT", [128, 4, 48], F32)
    nrm = k.sb("nrm", [128, 4, 2, KC], F32)
    tmp = k.sb("tmp", [128, KC, NS], F32)
    w = [k.sb("w%d" % i, [128, KC, D], F32) for i in range(2)]
    brep = [k.sb("brep%d" % i, [128, D], F32) for i in range(2)]
    go = [k.sb("go%d" % i, [128, D], F32) for i in range(2)]
    k.dma(cT.t[:], c.cT[:, :, :], [], [cT], cT)
    k.dma(abT.t[:], c.ada_bT[:, :, :], [], [abT], abT)
    k.dma(nrm.t[:], c.nrm[:, :, :, :], [], [nrm], nrm)
    k.op("pool", lambda h: h.memset(ones.t[:], 1.0), [], [ones])
    act(k, e1.t[:], cT.t[:], AF.Exp, [cT], [e1], scale=-1.0)
    ts(k, "dve", e1.t[:], e1.t[:], 1.0, None, ALU.add, None, [e1], [e1])
    k.op("dve", lambda h: h.reciprocal(out=e1.t[:], in_=e1.t[:]), [e1], [e1])
    tt(k, "dve", sc.t[:], cT.t[:], e1.t[:], ALU.mult, [cT, e1], [sc])
    for kc in range(KC):
        for s in range(NS):
            ts(k, "pool", screp.t[:, kc, s, :], ones.t[:], sc.t[:, kc, s:s + 1], None, ALU.mult, None,
               [ones, sc], [screp])
    wi = 0
    gi_ = 0
    for li in layers:
        for part in range(6):
            wt = w[wi % 2]
            wi += 1
            k.dma(wt.t[:], c.ada_w[li, :, :, part * D:(part + 1) * D], [], [wt], wt)
            if part in (2, 5):
                g = 0 if part == 2 else 1
                br = brep[gi_ % 2]
                k.dma(br.t[:], c.ada_brep[li, g, :, :], [], [br], br)
                for s in range(NS):
                    gt = go[gi_ % 2]
                    gi_ += 1
                    for half in range(2):
                        bk = k.bank()
                        for kc in range(KC):
                            mm(k, bk.t[:, :], screp.t[:, kc, s, :], wt.t[:, kc, half * 512:(half + 1) * 512],
                               kc == 0, kc == KC - 1, [screp, wt], [bk])
                        tt(k, "dve", gt.t[:, half * 512:(half + 1) * 512], bk.t[:, :],
                           br.t[:, half * 512:(half + 1) * 512], ALU.add, [bk, br], [gt])
                    k.dma(c.GREP[li, g, s, :, :], gt.t[:], [gt], [], gt)
            else:
                bk = k.bank()
                for oc in range(KC):
                    for kc in range(KC):
                        mm(k, bk.t[:, oc * NS:(oc + 1) * NS], wt.t[:, kc, oc * 128:(oc + 1) * 128],
                           sc.t[:, kc, :], kc == 0, kc == KC - 1, [wt, sc], [bk])
                bv = bk.t[:, 0:KC * NS].rearrange("p (o s) -> p o s", s=NS)
                for s in range(NS):
                    tt(k, "dve", tmp.t[:, :, s], bv[:, :, s], abT.t[:, li, part * 8:(part + 1) * 8], ALU.add,
                       [bk, abT], [tmp])
                which = 0 if part < 3 else 1
                if part in (1, 4):
                    for s in range(NS):
                        stt(k, c.s_mods.t[:, li, 2 * which, :, s], tmp.t[:, :, s], 1.0, nrm.t[:, li, which, :],
                            ALU.add, ALU.mult, [tmp, nrm], [c.s_mods])
                else:
                    cp(k, "dve", c.s_mods.t[:, li, 2 * which + 1, :, :], tmp.t[:, :, :], [tmp], [c.s_mods])
    k.end()


def norm_block(c, xin, nsub, ss, lnv, rstd, junk, engs=("dve", "act")):
    k = c.k
    for u in range(nsub):
        act(k, junk.t[:], xin.t[:, u, :], AF.Square, [xin], [junk, ss], accum_out=ss.t[:, u:u + 1])
    act(k, lnv.t[:, 0:nsub], ss.t[:, 0:nsub], AF.Ln, [ss], [lnv], scale=1.0 / D, bias=EPS)
    act(k, rstd.t[:, 0:nsub], lnv.t[:, 0:nsub], AF.Exp, [lnv], [rstd], scale=-0.5)
    for u in range(nsub):
        e = engs[u % len(engs)]
        if e == "act":
            act(k, xin.t[:, u, :], xin.t[:, u, :], AF.Identity, [xin, rstd], [xin], scale=rstd.t[:, u:u + 1])
        else:
            ts(k, e, xin.t[:, u, :], xin.t[:, u, :], rstd.t[:, u:u + 1], None, ALU.mult, None,
               [xin, rstd], [xin])


def transpose_block(c, xin, nsub, hT, col0, li, which, slot, extra=None):
    k = c.k
    A = c.s_mods.t[:, li, 2 * which, :, :]
    B = c.s_mods.t[:, li, 2 * which + 1, :, :]
    n = 0
    for kc in range(KC):
        for u0 in range(0, nsub, 4):
            nu = min(4, nsub - u0)
            bk = k.bank()
            for u in range(nu):
                k.op("pe", lambda h, u=u: h.transpose(out=bk.t[:, u * 128:(u + 1) * 128],
                                                       in_=xin.t[:, u0 + u, kc * 128:(kc + 1) * 128],
                                                       identity=c.s_ident.t[:]),
                     [xin, c.s_ident], [bk], inc=(u == nu - 1))
            o = hT.t[:, kc, col0 + u0 * 128: col0 + (u0 + nu) * 128]
            if n % 2 == 0:
                act(k, o, bk.t[:, 0:nu * 128], AF.Identity, [bk, c.s_mods], [hT],
                    scale=A[:, kc, slot:slot + 1], bias=B[:, kc, slot:slot + 1])
            else:
                ts(k, "dve", o, bk.t[:, 0:nu * 128], A[:, kc, slot:slot + 1], B[:, kc, slot:slot + 1],
                   ALU.mult, ALU.add, [bk, c.s_mods], [hT])
            if extra is not None:
                extra(kc, u0, nu, bk)
            n += 1


def phase_r1(c, li, j, cur):
    k = c.k
    T = c.T
    X = c.X[cur]
    k.begin()
    w_in = k.sb("w_in", [128, KC, 2048], BF16)
    k.dma(w_in.t[:].rearrange("p k n -> p (k n)"), c.wb["rg_w_in"][j, :, :], [], [w_in], w_in)
    xin = [k.sb("xin%d" % i, [128, 4, D], F32) for i in range(2)]
    junk = k.sb("junk", [128, D], F32)
    sm = [[k.sb("sm%d_%d" % (i, q), [128, 4], F32) for q in range(3)] for i in range(2)]
    hT = [k.sb("hT%d" % i, [128, KC, 512], BF16) for i in range(2)]
    xbT = [k.sb("xbT%d" % i, [128, KC, 512], F32) for i in range(2)]
    gyT = [k.sb("gyT%d" % i, [128, KC, 512], BF16) for i in range(2)]
    tm = [[k.sb("tm%d_%d" % (i, q), [128, 512], F32) for q in range(4)] for i in range(2)]
    ntile = T // 512

    def load(i):
        k.dma(xin[i % 2].t[:], X[PAD + i * 512:PAD + i * 512 + 512, :].rearrange("(s p) f -> p s f", p=128),
              [], [xin[i % 2]], xin[i % 2])

    load(0)
    for i in range(ntile):
        t0 = i * 512
        slot = i // 8
        xi = xin[i % 2]
        if i + 1 < ntile:
            load(i + 1)
        ss, lnv, rstd = sm[i % 2]
        norm_block(c, xi, 4, ss, lnv, rstd, junk)
        h = hT[i % 2]
        transpose_block(c, xi, 4, h, 0, li, 0, slot)
        xo = xbT[i % 2]
        go = gyT[i % 2]
        for oc in range(16):
            bk = k.bank()
            for kc in range(KC):
                mm(k, bk.t[:, :], w_in.t[:, kc, oc * 128:(oc + 1) * 128], h.t[:, kc, :], kc == 0, kc == KC - 1,
                   [w_in, h], [bk])
            if oc < 8:
                cp(k, "act" if oc % 2 == 0 else "dve", xo.t[:, oc, :], bk.t[:, :], [bk], [xo])
            else:
                xs, sq, inn, sg = tm[oc % 2]
                cp(k, "act", xs.t[:], bk.t[:, :], [bk], [xs])
                act(k, sq.t[:], bk.t[:, :], AF.Square, [bk], [sq])
                ts(k, "dve", inn.t[:], sq.t[:], 0.044715, 1.0, ALU.mult, ALU.add, [sq], [inn])
                tt(k, "dve", inn.t[:], inn.t[:], xs.t[:], ALU.mult, [inn, xs], [inn])
                act(k, sg.t[:], inn.t[:], AF.Exp, [inn], [sg], scale=-GEL_C)
                act(k, sg.t[:], sg.t[:], AF.Ln, [sg], [sg], bias=1.0)
                act(k, sg.t[:], sg.t[:], AF.Exp, [sg], [sg], scale=-1.0)
                tt(k, "dve", go.t[:, oc - 8, :], xs.t[:], sg.t[:], ALU.mult, [xs, sg], [go])
        k.dma(c.XBs[:, :, 4 + t0:4 + t0 + 512].rearrange("c p t -> p c t"), xo.t[:], [xo], [], xo)
        k.dma(c.GYs[:, :, t0:t0 + 512].rearrange("c p t -> p c t"), go.t[:], [go], [], go)
    k.end()


def rg_consts(c, j, z):
    k = c.k
    gw = k.sb("gw", [128, 8192], BF16)
    k.dma(gw.t[:], c.wb["rg_gate_w"][j, :, :], [], [gw], gw)
    gb = k.sb("gb", [128, 2, 2, 2, KC], F32)
    k.dma(gb.t[:], c.rg_gb[:, :, :, :, :], [], [gb], gb)
    negb = k.sb("negb", [128, 2, KC], F32)
    ts(k, "dve", negb.t[:], gb.t[:, j, z, :, :], -1.0, None, ALU.mult, None, [gb], [negb])
    lam = k.sb("lam", [128, 2, 2, KC], F32)
    k.dma(lam.t[:], c.rg_lam[:, :, :, :], [], [lam], lam)
    c8 = k.sb("c8", [128, KC], F32)
    c16 = k.sb("c16", [128, KC], F32)
    act(k, c8.t[:], lam.t[:, j, z, :], AF.Exp, [lam], [c8], scale=-1.0)
    act(k, c8.t[:], c8.t[:], AF.Ln, [c8], [c8], bias=1.0)
    ts(k, "dve", c16.t[:], c8.t[:], -16.0, None, ALU.mult, None, [c8], [c16])
    ts(k, "dve", c8.t[:], c8.t[:], -8.0, None, ALU.mult, None, [c8], [c8])
    return gw, negb, c8, c16


def rg_gates(c, z, gw, negb, c8, c16, xc, xcb, tmps, hout, carry, reverse):
    k = c.k
    gv = gw.t[:].rearrange("p (z g n k d) -> p z g n k d", z=2, g=2, n=4, k=2)
    for g0 in (0, 4):
        ocs = list(range(g0, g0 + 4))
        bk_r = {}
        bk_i = {}
        for oc in ocs:
            n_, half = oc // 2, oc % 2
            bk_r[oc] = k.bank()
            for kk in range(2):
                mm(k, bk_r[oc].t[:, :], gv[:, z, 0, n_, kk, half * 128:(half + 1) * 128], xcb.t[:, 2 * n_ + kk, :],
                   kk == 0, kk == 1, [gw, xcb], [bk_r[oc]])
            bk_i[oc] = k.bank()
            for kk in range(2):
                mm(k, bk_i[oc].t[:, :], gv[:, z, 1, n_, kk, half * 128:(half + 1) * 128], xcb.t[:, 2 * n_ + kk, :],
                   kk == 0, kk == 1, [gw, xcb], [bk_i[oc]])
        T_ = {oc: tmps[oc % 4] for oc in ocs}
        for oc in ocs:
            er, ei, a, a2, u = T_[oc]
            act(k, er.t[:], bk_r[oc].t[:, :], AF.Exp, [bk_r[oc], negb], [er], scale=-1.0, bias=negb.t[:, 0, oc:oc + 1])
            act(k, ei.t[:], bk_i[oc].t[:, :], AF.Exp, [bk_i[oc], negb], [ei], scale=-1.0, bias=negb.t[:, 1, oc:oc + 1])
        for oc in ocs:
            er = T_[oc][0]
            ts(k, "dve", er.t[:], er.t[:], 1.0, None, ALU.add, None, [er], [er])
        for oc in ocs:
            ei = T_[oc][1]
            act(k, ei.t[:], ei.t[:], AF.Ln, [ei], [ei], bias=1.0)
        for oc in ocs:
            er = T_[oc][0]
            k.op("dve", lambda h, er=er: h.reciprocal(out=er.t[:], in_=er.t[:]), [er], [er])
        for oc in ocs:
            ei = T_[oc][1]
            act(k, ei.t[:], ei.t[:], AF.Exp, [ei], [ei], scale=-1.0)
        for oc in ocs:
            er, ei, a, a2, u = T_[oc]
            tt(k, "pool", u.t[:], ei.t[:], xc.t[:, oc, :], ALU.mult, [ei, xc], [u])
        for oc in ocs:
            er, ei, a, a2, u = T_[oc]
            act(k, a.t[:], er.t[:], AF.Exp, [er, c8], [a], scale=c8.t[:, oc:oc + 1])
        for oc in ocs:
            er, ei, a, a2, u = T_[oc]
            act(k, a2.t[:], er.t[:], AF.Exp, [er, c16], [a2], scale=c16.t[:, oc:oc + 1])
        for oc in ocs:
            a2 = T_[oc][3]
            act(k, a2.t[:], a2.t[:], AF.Ln, [a2], [a2], scale=-1.0, bias=1.0)
        for oc in ocs:
            a2 = T_[oc][3]
            act(k, a2.t[:], a2.t[:], AF.Exp, [a2], [a2], scale=0.5)
        for oc in ocs:
            er, ei, a, a2, u = T_[oc]
            tt(k, "dve", u.t[:], u.t[:], a2.t[:], ALU.mult, [u, a2], [u])
        for oc in ocs:
            er, ei, a, a2, u = T_[oc]
            cr = carry[oc]
            if reverse:
                k.op("dve", lambda h, a=a, u=u, cr=cr, oc=oc: h.tensor_tensor_scan(
                    out=hout[oc].t[:, oc, ::-1], data0=a.t[:, ::-1], data1=u.t[:, ::-1], initial=cr.t[:, 0:1],
                    op0=ALU.mult, op1=ALU.add), [a, u, cr], [hout[oc]])
                cp(k, "pool", cr.t[:, 0:1], hout[oc].t[:, oc, 0:1], [hout[oc]], [cr])
            else:
                k.op("dve", lambda h, a=a, u=u, cr=cr, oc=oc: h.tensor_tensor_scan(
                    out=hout[oc].t[:, oc, :], data0=a.t[:, :], data1=u.t[:, :], initial=cr.t[:, 0:1],
                    op0=ALU.mult, op1=ALU.add), [a, u, cr], [hout[oc]])
                cp(k, "pool", cr.t[:, 0:1], hout[oc].t[:, oc, 511:512], [hout[oc]], [cr])


def phase_r2(c, li, j):
    k = c.k
    T = c.T
    k.begin()
    gw, negb, c8, c16 = rg_consts(c, j, 0)
    cw = k.sb("cw", [128, 2, KC, 5], F32)
    k.dma(cw.t[:], c.rg_conv[:, :, :, :], [], [cw], cw)
    dg = k.sb("dg", [128, KC, 4, 128], F32)
    for ch in range(KC):
        for tp in range(4):
            ts(k, "pool", dg.t[:, ch, tp, :], c.s_ident.t[:], cw.t[:, j, ch, tp:tp + 1], None, ALU.mult, None,
               [c.s_ident, cw], [dg])
    xbw = [k.sb("xbw%d" % i, [128, KC, 515], F32) for i in range(2)]
    xc = [k.sb("xc%d" % i, [128, KC, 512], F32) for i in range(2)]
    xcb = [k.sb("xcb%d" % i, [128, KC, 512], BF16) for i in range(2)]
    hf = [k.sb("hf%d" % i, [128, KC, 512], F32) for i in range(2)]
    hfv = [[hf[i]] + [Buf("hfv", hf[i].t) for _ in range(KC - 1)] for i in range(2)]
    tmps = [[k.sb("gt%d_%d" % (i, q), [128, 512], F32) for q in range(5)] for i in range(4)]
    carry = [k.sb("carry%d" % i, [128, 1], F32) for i in range(KC)]
    for cr in carry:
        k.op("pool", lambda h, cr=cr: h.memset(cr.t[:], 0.0), [], [cr])
    ntile = T // 512
    def load(i):
        k.dma(xbw[i % 2].t[:], c.XBs[:, :, 4 + i * 512 - 2:4 + i * 512 + 513].rearrange("c p t -> p c t"),
              [], [xbw[i % 2]], xbw[i % 2])

    load(0)
    for i in range(ntile):
        t0 = i * 512
        xw = xbw[i % 2]
        if i + 1 < ntile:
            load(i + 1)
        if i == 0:
            k.op("pool", lambda h: h.memset(xw.t[:, :, 0:2], 0.0), [], [xw])
        elif i % 8 == 0:
            ts(k, "pool", xw.t[:, :, 0:2], xw.t[:, :, 0:2], c.s_flags.t[:, 0:1], None, ALU.mult, None,
               [xw, c.s_flags], [xw])
            for cr in carry:
                ts(k, "pool", cr.t[:], cr.t[:], c.s_flags.t[:, 0:1], None, ALU.mult, None,
                   [cr, c.s_flags], [cr])
        if i == ntile - 1:
            k.op("pool", lambda h: h.memset(xw.t[:, :, 514:515], 0.0), [], [xw])
        elif i % 8 == 7:
            ts(k, "pool", xw.t[:, :, 514:515], xw.t[:, :, 514:515], c.s_flags.t[:, 0:1], None, ALU.mult, None,
               [xw, c.s_flags], [xw])
        xcc = xc[i % 2]
        xcbb = xcb[i % 2]
        for ch in range(KC):
            bk = k.bank()
            for tp in range(4):
                mm(k, bk.t[:, :], dg.t[:, ch, tp, :], xw.t[:, ch, tp:tp + 512], tp == 0, tp == 3, [dg, xw], [bk])
            act(k, xcc.t[:, ch, :], bk.t[:, :], AF.Identity, [bk, cw], [xcc], bias=cw.t[:, j, ch, 4:5])
            cp(k, "pool", xcbb.t[:, ch, :], xcc.t[:, ch, :], [xcc], [xcbb])
        hh = hfv[i % 2]
        rg_gates(c, 0, gw, negb, c8, c16, xcc, xcbb, tmps, hh, carry, False)
        k.dma(c.XCs[:, :, t0:t0 + 512].rearrange("c p t -> p c t"), xcc.t[:], [xcc], [], xcc)
        k.dma(c.HFs[:, :, t0:t0 + 512].rearrange("c p t -> p c t"), hh[0].t[:], hh, [], hh[0])
    k.end()


def phase_r3(c, li, j, cur):
    k = c.k
    T = c.T
    X = c.X[cur]
    k.begin()
    gw, negb, c8, c16 = rg_consts(c, j, 1)
    w_out = k.sb("w_out", [128, KC, D], BF16)
    k.dma(w_out.t[:].rearrange("p k n -> p (k n)"), c.wb["rg_w_out"][j, :, :], [], [w_out], w_out)
    xc = k.sb("xc", [128, KC, 512], F32)
    xcb = k.sb("xcb", [128, KC, 512], BF16)
    hfl = k.sb("hfl", [128, KC, 512], F32)
    gyl = k.sb("gyl", [128, KC, 512], BF16)
    hb = k.sb("hb", [128, KC, 512], F32)
    hbv = [hb] + [Buf("hbv", hb.t) for _ in range(KC - 1)]
    yT = k.sb("yT", [128, KC, 512], BF16)
    xin = k.sb("xin", [128, 4, D], F32)
    g1 = [k.sb("g1_%d" % i, [128, D], F32) for i in range(2)]
    tmps = [[k.sb("gt%d_%d" % (i, q), [128, 512], F32) for q in range(5)] for i in range(4)]
    tmo = [k.sb("tmo%d" % i, [128, 512], F32) for i in range(2)]
    carry = [k.sb("carry%d" % i, [128, 1], F32) for i in range(KC)]
    for cr in carry:
        k.op("pool", lambda h, cr=cr: h.memset(cr.t[:], 0.0), [], [cr])
    ntile = T // 512
    gcur = None
    for i in range(ntile - 1, -1, -1):
        t0 = i * 512
        slot = i // 8
        if gcur is None or gcur[0] != slot:
            gb_ = g1[slot % 2]
            k.dma(gb_.t[:], c.GREP[li, 0, slot, :, :], [], [gb_], gb_)
            gcur = (slot, gb_)
        gb_ = gcur[1]
        k.dma(xc.t[:], c.XCs[:, :, t0:t0 + 512].rearrange("c p t -> p c t"), [], [xc], xc)
        k.dma(hfl.t[:], c.HFs[:, :, t0:t0 + 512].rearrange("c p t -> p c t"), [], [hfl], hfl)
        k.dma(gyl.t[:], c.GYs[:, :, t0:t0 + 512].rearrange("c p t -> p c t"), [], [gyl], gyl)
        k.dma(xin.t[:], X[PAD + t0:PAD + t0 + 512, :].rearrange("(s p) f -> p s f", p=128), [], [xin], xin)
        if i % 8 == 7 and i != ntile - 1:
            for cr in carry:
                ts(k, "pool", cr.t[:], cr.t[:], c.s_flags.t[:, 0:1], None, ALU.mult, None,
                   [cr, c.s_flags], [cr])
        for ch in range(KC):
            cp(k, "pool", xcb.t[:, ch, :], xc.t[:, ch, :], [xc], [xcb])
        rg_gates(c, 1, gw, negb, c8, c16, xc, xcb, tmps, hbv, carry, True)
        for ch in range(KC):
            tt(k, "pool", hfl.t[:, ch, :], hfl.t[:, ch, :], hb.t[:, ch, :], ALU.add, [hfl, hbv[ch]], [hfl])
            tt(k, "dve", yT.t[:, ch, :], hfl.t[:, ch, :], gyl.t[:, ch, :], ALU.mult, [hfl, gyl], [yT])
        n = 0
        for u in range(4):
            for half in range(2):
                bk = k.bank()
                for kc in range(KC):
                    mm(k, bk.t[:, :], yT.t[:, kc, u * 128:(u + 1) * 128], w_out.t[:, kc, half * 512:(half + 1) * 512],
                       kc == 0, kc == KC - 1, [yT, w_out], [bk])
                tm_ = tmo[n % 2]
                n += 1
                tt(k, "dve", tm_.t[:], bk.t[:, :], gb_.t[:, half * 512:(half + 1) * 512], ALU.mult, [bk, gb_], [tm_])
                tt(k, "pool", xin.t[:, u, half * 512:(half + 1) * 512], xin.t[:, u, half * 512:(half + 1) * 512],
                   tm_.t[:], ALU.add, [xin, tm_], [xin])
        k.dma(X[PAD + t0:PAD + t0 + 512, :].rearrange("(s p) f -> p s f", p=128), xin.t[:], [xin], [], xin)
    k.end()


def phase_ffn(c, li, j, cur, moe):
    k = c.k
    T = c.T
    X = c.X[cur]
    ST = 1024
    k.begin()
    xin = [k.sb("xin%d" % i, [128, 2, D], F32) for i in range(2)]
    junk = k.sb("junk", [128, D], F32)
    sm = [[k.sb("sm%d_%d" % (i, q), [128, 4], F32) for q in range(3)] for i in range(2)]
    hTs = [k.sb("hT%d" % i, [128, KC, ST], BF16) for i in range(2)]
    acc = k.sb("acc", [128, 8, D], F32)
    SL = 4
    hs = [k.sb("hs%d" % i, [128, SL, ST], BF16) for i in range(2)]
    wgu = [k.sb("wgu%d" % i, [128, KC, 256], BF16) for i in range(4)]
    wdn = [k.sb("wdn%d" % i, [128, SL, D], BF16) for i in range(2)]
    sg = [k.sb("sg%d" % i, [128, 512], F32) for i in range(4)]
    tmo = [k.sb("tmo%d" % i, [128, 512], F32) for i in range(2)]
    g2 = [k.sb("g2_%d" % i, [128, D], F32) for i in range(2)]
    if moe:
        rt = k.sb("rt", [128, 2, KC, NEXP], F32)
        k.dma(rt.t[:], c.moe_rt[:, :, :, :], [], [rt], rt)
        rtb = k.sb("rtb", [128, KC, 128], BF16)
        k.op("pool", lambda h: h.memset(rtb.t[:], 0.0), [], [rtb])
        cp(k, "dve", rtb.t[:, :, 0:NEXP], rt.t[:, j, :, :], [rt], [rtb])
        lgs = [k.sb("lg%d" % i, [128, 8, NEXP], F32) for i in range(2)]
        comb = k.sb("comb", [128, 8, NEXP], F32)
        m1 = k.sb("m1", [128, 8], F32)
        m2 = k.sb("m2", [128, 8], F32)
        k1 = k.sb("k1", [128, 8, NEXP], F32)
        k2 = k.sb("k2", [128, 8, NEXP], F32)
        l2 = k.sb("l2", [128, 8, NEXP], F32)
        w1 = k.sb("w1", [128, 8], F32)
        w2 = k.sb("w2", [128, 8], F32)
    if moe:
        nch = 28
        experts = range(NEXP)
        gu_src = None
        dn_src = None
    else:
        nch = 22
        experts = [0]
        gu_src = c.wb["ff_gu"]
        dn_src = c.wb["ff_dn"]
    slabs = []
    for e in experts:
        f = 0
        while f < nch:
            n = min(SL, nch - f)
            base = 0 if moe else (j * 22)
            slabs.append((e, base + f, n))
            f += n
    nst = T // ST
    wg_i = 0
    evn = 0
    gcur = None
    def get_g2(slot):
        nonlocal gcur
        if gcur is None or gcur[0] != slot:
            gb__ = g2[slot % 2]
            k.dma(gb__.t[:], c.GREP[li, 1, slot, :, :], [], [gb__], gb__)
            gcur = (slot, gb__)
        return gcur[1]

    def normT(q):
        t0 = q * ST
        slot = t0 // SLOT
        hTq = hTs[q % 2]
        lgq = lgs[q % 2] if moe else None
        for pr in range(4):
            xi = xin[pr % 2]
            r0 = PAD + t0 + pr * 256
            k.dma(xi.t[:], X[r0:r0 + 256, :].rearrange("(s p) f -> p s f", p=128), [], [xi], xi)
            ss, lnv, rstd = sm[pr % 2]
            norm_block(c, xi, 2, ss, lnv, rstd, junk, engs=("act", "dve"))
            transpose_block(c, xi, 2, hTq, pr * 256, li, 1, slot)
            if moe:
                for u in range(2):
                    sub = pr * 2 + u
                    bk = k.bank()
                    for kc in range(KC):
                        mm(k, bk.t[:, 0:128], hTq.t[:, kc, sub * 128:(sub + 1) * 128], rtb.t[:, kc, :], kc == 0,
                           kc == KC - 1, [hTq, rtb], [bk])
                    cp(k, "dve", lgq.t[:, sub, :], bk.t[:, 0:NEXP], [bk], [lgq])

    if FFN_MAXST is not None:
        nst = min(nst, FFN_MAXST)
    normT(0)
    for q in range(nst):
        t0 = q * ST
        slot = t0 // SLOT
        gb_ = get_g2(slot)
        hT = hTs[q % 2]
        if moe:
            lg = lgs[q % 2]
        if moe and MOE_STAGE < 1:
            k.op("pool", lambda h: h.memset(comb.t[:], 0.125), [], [comb])
        if moe and MOE_STAGE >= 1:
            k.op("dve", lambda h: h.tensor_reduce(out=m1.t[:], in_=lg.t[:], axis=mybir.AxisListType.X, op=ALU.max),
                 [lg], [m1])
            for sub in range(8):
                ts(k, "dve", k1.t[:, sub, :], lg.t[:, sub, :], m1.t[:, sub:sub + 1], None, ALU.is_equal, None,
                   [lg, m1], [k1])
            stt(k, l2.t[:], k1.t[:], -1e30, lg.t[:], ALU.mult, ALU.add, [k1, lg], [l2])
            k.op("dve", lambda h: h.tensor_reduce(out=m2.t[:], in_=l2.t[:], axis=mybir.AxisListType.X, op=ALU.max),
                 [l2], [m2])
            for sub in range(8):
                ts(k, "dve", k2.t[:, sub, :], l2.t[:, sub, :], m2.t[:, sub:sub + 1], None, ALU.is_equal, None,
                   [l2, m2], [k2])
            tt(k, "dve", w2.t[:], m2.t[:], m1.t[:], ALU.subtract, [m1, m2], [w2])
            act(k, w2.t[:], w2.t[:], AF.Exp, [w2], [w2])
            ts(k, "dve", w1.t[:], w2.t[:], 1.0, None, ALU.add, None, [w2], [w1])
            k.op("dve", lambda h: h.reciprocal(out=w1.t[:], in_=w1.t[:]), [w1], [w1])
            tt(k, "dve", w2.t[:], w2.t[:], w1.t[:], ALU.mult, [w2, w1], [w2])
            for sub in range(8):
                ts(k, "dve", k1.t[:, sub, :], k1.t[:, sub, :], w1.t[:, sub:sub + 1], None, ALU.mult, None,
                   [k1, w1], [k1])
                stt(k, comb.t[:, sub, :], k2.t[:, sub, :], w2.t[:, sub:sub + 1], k1.t[:, sub, :], ALU.mult, ALU.add,
                    [k2, w2, k1], [comb])
        first_acc = [True] * 16

        def do_gu(si):
            nonlocal wg_i
            e, cb, n = slabs[si]
            hsl = hs[si % 2]
            gsrc = c.wb["moe_gu%d" % (j * NEXP + e)] if moe else gu_src
            for f in range(n):
                wg = wgu[wg_i % 4]
                wg_i += 1
                k.dma(wg.t[:].rearrange("p k n -> p (k n)"), gsrc[cb + f, :, :], [], [wg], wg)
                for tk in range(2):
                    bg = k.bank()
                    for kc in range(KC):
                        mm(k, bg.t[:, :], wg.t[:, kc, 0:128], hT.t[:, kc, tk * 512:(tk + 1) * 512], kc == 0,
                           kc == KC - 1, [wg, hT], [bg])
                    bu = k.bank()
                    for kc in range(KC):
                        mm(k, bu.t[:, :], wg.t[:, kc, 128:256], hT.t[:, kc, tk * 512:(tk + 1) * 512], kc == 0,
                           kc == KC - 1, [wg, hT], [bu])
                    s_ = sg[(f * 2 + tk) % 4]
                    act(k, s_.t[:], bg.t[:, :], AF.Silu, [bg], [s_])
                    tt(k, "dve", hsl.t[:, f, tk * 512:(tk + 1) * 512], s_.t[:], bu.t[:, :], ALU.mult, [s_, bu], [hsl])

        def pre_dn(si):
            e, cb, n = slabs[si]
            wd = wdn[si % 2]
            dsrc = c.wb["moe_dn%d" % (j * NEXP + e)] if moe else dn_src
            k.dma(wd.t[:, 0:n, :], dsrc[cb:cb + n, :, :].rearrange("n p f -> p n f"), [], [wd], wd)

        def do_dn(si):
            nonlocal evn
            e, cb, n = slabs[si]
            hsl = hs[si % 2]
            wd = wdn[si % 2]
            for sub in range(8):
                for half in range(2):
                    bk = k.bank()
                    for f in range(n):
                        mm(k, bk.t[:, :], hsl.t[:, f, sub * 128:(sub + 1) * 128], wd.t[:, f, half * 512:(half + 1) * 512],
                           f == 0, f == n - 1, [hsl, wd], [bk])
                    av = acc.t[:, sub, half * 512:(half + 1) * 512]
                    bi = sub * 2 + half
                    if moe:
                        cs = comb.t[:, sub, e:e + 1]
                        if first_acc[bi]:
                            ts(k, "dve", av, bk.t[:, :], cs, None, ALU.mult, None, [bk, comb], [acc])
                        elif evn % 2 == 0:
                            stt(k, av, bk.t[:, :], cs, av, ALU.mult, ALU.add, [bk, comb, acc], [acc])
                        else:
                            tm_ = tmo[(evn // 2) % 2]
                            act(k, tm_.t[:], bk.t[:, :], AF.Identity, [bk, comb], [tm_], scale=cs)
                            tt(k, "pool", av, av, tm_.t[:], ALU.add, [acc, tm_], [acc])
                    else:
                        if first_acc[bi]:
                            cp(k, "act", av, bk.t[:, :], [bk], [acc])
                        elif evn % 2 == 0:
                            tt(k, "dve", av, bk.t[:, :], av, ALU.add, [bk, acc], [acc])
                        else:
                            tm_ = tmo[(evn // 2) % 2]
                            cp(k, "act", tm_.t[:], bk.t[:, :], [bk], [tm_])
                            tt(k, "pool", av, av, tm_.t[:], ALU.add, [acc, tm_], [acc])
                    first_acc[bi] = False
                    evn += 1

        do_gu(0)
        nmid = max(0, len(slabs) - 3)
        for si in range(len(slabs)):
            pre_dn(si)
            if si + 1 < len(slabs):
                do_gu(si + 1)
            if si == nmid and q + 1 < nst:
                normT(q + 1)
            do_dn(si)
        for pr in range(4):
            xi = xin[pr % 2]
            r0 = PAD + t0 + pr * 256
            k.dma(xi.t[:], X[r0:r0 + 256, :].rearrange("(s p) f -> p s f", p=128), [], [xi], xi)
            for u in range(2):
                sub = pr * 2 + u
                tt(k, "dve", acc.t[:, sub, :], acc.t[:, sub, :], gb_.t[:], ALU.mult, [acc, gb_], [acc])
                tt(k, "pool", xi.t[:, u, :], xi.t[:, u, :], acc.t[:, sub, :], ALU.add, [xi, acc], [xi])
            k.dma(X[r0:r0 + 256, :].rearrange("(s p) f -> p s f", p=128), xi.t[:], [xi], [], xi)
    k.end()


def phase_attn(c, li, j, gi, cur):
    k = c.k
    T = c.T
    NS = c.NSLOT
    Xr = c.X[cur]
    Xw = c.X[1 - cur]
    win, d = GROUPS[gi]
    L = SLOT // d
    NT = 512 if L >= 512 else L
    nQB = NT // 128
    nKB = nQB + 1
    W = NT + 128
    ntr = L // NT
    last = (gi == 2)
    k.begin()
    wq = k.sb("wq", [128, KC, 1536], BF16)
    k.dma(wq.t[:].rearrange("p k n -> p (k n)"), c.wb["at_qkv"][j * 3 + gi, :, :], [], [wq], wq)
    qkn = k.sb("qkn", [128, 2, 2], F32)
    k.dma(qkn.t[:], c.at_qkn[:, :, :], [], [qkn], qkn)
    gq = k.sb("gq", [128, 2], F32)
    cp(k, "dve", gq.t[:], qkn.t[:, j, :], [qkn], [gq])
    ts(k, "dve", gq.t[:, 0:1], gq.t[:, 0:1], 0.125, None, ALU.mult, None, [gq], [gq])
    bones = k.sb("bones", [128, 128], F32)
    k.dma(bones.t[:], c.bones[:, :], [], [bones], bones)
    pswap = k.sb("pswap", [128, 128], F32)
    k.dma(pswap.t[:], c.pswap[:, :], [], [pswap], pswap)
    bb32 = k.sb("bb32", [128, 256], F32)
    k.dma(bb32.t[:], c.bandb[:, :], [], [bb32], bb32)
    bb = k.sb("bb", [128, 256], BF16)
    cp(k, "dve", bb.t[:], bb32.t[:], [bb32], [bb])
    idb = k.sb("idb", [128, 128], BF16)
    cp(k, "dve", idb.t[:], c.s_ident.t[:], [c.s_ident], [idb])
    xin = k.sb("xin", [128, nKB, D], F32)
    junk = k.sb("junk", [128, D], F32)
    sm = [k.sb("sm%d" % q, [128, 8], F32) for q in range(3)]
    hT = k.sb("hT", [128, KC, W], BF16)
    ctab = k.sb("ctab", [128, W], F32)
    stab = k.sb("stab", [128, W], F32)
    QT = k.sb("QT", [128, 4, NT], BF16)
    KT = k.sb("KT", [128, 4, W], BF16)
    qt_ = [[k.sb("qt%d_%d" % (i, q), [128, 512], F32) for q in range(4)] for i in range(2)]
    VE = k.sb("VE", [128, nKB, 8, 66], BF16)
    PT = k.sb("PT", [128, nKB, 8, 256], BF16)
    osb = [k.sb("osb%d" % i, [128, 528], F32) for i in range(2)]
    oac = [k.sb("oac%d" % i, [128, 528], F32) for i in range(2)]
    if last:
        wo = k.sb("wo", [128, 4, D], BF16)
        k.dma(wo.t[:].rearrange("p k n -> p (k n)"), c.wb["at_wo"][j, :, :], [], [wo], wo)
        rden = k.sb("rden", [128, 8], F32)
        on = k.sb("on", [128, 512], F32)
        oT = k.sb("oT", [128, 4, 128], BF16)
        xres = [k.sb("xres%d" % i, [128, D], F32) for i in range(2)]
        g1 = [k.sb("g1_%d" % i, [128, D], F32) for i in range(2)]
        tmo = [k.sb("tmo%d" % i, [128, 512], F32) for i in range(2)]
    gcur = None
    un = 0
    on_ = 0
    for slot in range(NS):
        if last:
            gb_ = g1[slot % 2]
            k.dma(gb_.t[:], c.GREP[li, 0, slot, :, :], [], [gb_], gb_)
        for r in range(d):
            for w_ in range(ntr):
                if ATTN_MAXTILES is not None and un >= ATTN_MAXTILES * 12:
                    continue
                j0 = w_ * NT
                row0 = PAD + slot * SLOT + r + d * (j0 - 64)
                k.dma(xin.t[:], Xr[row0:row0 + d * (W - 1) + 1:d, :].rearrange("(b p) f -> p b f", p=128), [], [xin], xin)
                tcol = 64 + r * (NS * L) + slot * L + j0 - 64
                k.dma(ctab.t[:], c.rope[gi, 0, :, tcol:tcol + W], [], [ctab], ctab)
                k.dma(stab.t[:], c.rope[gi, 1, :, tcol:tcol + W], [], [stab], stab)
                norm_block(c, xin, nKB, sm[0], sm[1], sm[2], junk)
                transpose_block(c, xin, nKB, hT, 0, li, 0, slot)
                units = []
                for ch in range(4):
                    units.append((0, ch, 64, NT, QT, 0))
                for ch in range(4):
                    c0 = 0
                    while c0 < W:
                        n = min(512, W - c0)
                        units.append((1, ch, c0, n, KT, c0))
                        c0 += n
                for (isk, ch, h0, n, dst, d0) in units:
                    qg, sq, rs, t1 = qt_[un % 2]
                    un += 1
                    bk = k.bank()
                    wc = isk * 512 + ch * 128
                    for kc in range(KC):
                        mm(k, bk.t[:, 0:n], wq.t[:, kc, wc:wc + 128], hT.t[:, kc, h0:h0 + n], kc == 0, kc == KC - 1,
                           [wq, hT], [bk])
                    act(k, qg.t[:, 0:n], bk.t[:, 0:n], AF.Identity, [bk, gq], [qg], scale=gq.t[:, isk:isk + 1])
                    act(k, sq.t[:, 0:n], bk.t[:, 0:n], AF.Square, [bk], [sq])
                    b2 = k.bank()
                    mm(k, b2.t[:, 0:n], bones.t[:, :], sq.t[:, 0:n], True, True, [bones, sq], [b2])
                    b3 = k.bank()
                    mm(k, b3.t[:, 0:n], pswap.t[:, :], qg.t[:, 0:n], True, True, [pswap, qg], [b3])
                    act(k, rs.t[:, 0:n], b2.t[:, 0:n], AF.Ln, [b2], [rs], scale=1.0 / 64, bias=EPS)
                    act(k, rs.t[:, 0:n], rs.t[:, 0:n], AF.Exp, [rs], [rs], scale=-0.5)
                    tt(k, "dve", t1.t[:, 0:n], qg.t[:, 0:n], ctab.t[:, h0:h0 + n], ALU.mult, [qg, ctab], [t1])
                    tt(k, "dve", sq.t[:, 0:n], b3.t[:, 0:n], stab.t[:, h0:h0 + n], ALU.mult, [b3, stab], [sq])
                    tt(k, "dve", t1.t[:, 0:n], t1.t[:, 0:n], sq.t[:, 0:n], ALU.add, [t1, sq], [t1])
                    tt(k, "dve", dst.t[:, ch, d0:d0 + n], t1.t[:, 0:n], rs.t[:, 0:n], ALU.mult, [t1, rs], [dst])
                if ATTN_STAGE < 1:
                    continue
                k.op("pool", lambda h: h.memset(VE.t[:, :, :, 64:66], 1.0), [], [VE])
                for b in range(nKB):
                    bk = k.bank()
                    for kc in range(KC):
                        mm(k, bk.t[:, :], hT.t[:, kc, b * 128:(b + 1) * 128], wq.t[:, kc, 1024:1536], kc == 0,
                           kc == KC - 1, [hT, wq], [bk])
                    cp(k, "act", VE.t[:, b, :, 0:64], bk.t[:, :].rearrange("p (h e) -> p h e", e=64), [bk], [VE])
                if w_ == 0:
                    fc = 1 if slot == 0 else 0
                    ts(k, "pool", VE.t[0:64, 0, :, :], VE.t[0:64, 0, :, :], c.s_flags.t[0:64, fc:fc + 1], None,
                       ALU.mult, None, [VE, c.s_flags], [VE])
                if w_ == ntr - 1:
                    fc = 1 if slot == NS - 1 else 0
                    ts(k, "pool", VE.t[64:128, nKB - 1, :, :], VE.t[64:128, nKB - 1, :, :],
                       c.s_flags.t[64:128, fc:fc + 1], None, ALU.mult, None, [VE, c.s_flags], [VE])
                if ATTN_STAGE < 2:
                    continue
                for b in range(nKB):
                    qlo = max(b - 1, 0)
                    qhi = min(b + 1, nQB)
                    wd_ = (qhi - qlo) * 128
                    if b == 0:
                        mb = bb.t[:, 128:256]
                    elif b == nQB:
                        mb = bb.t[:, 0:128]
                    else:
                        mb = bb.t[:, 0:256]
                    for hp in range(4):
                        bk = k.bank()
                        for hh in range(2):
                            pb = 64 * hh
                            o = bk.t[:, hh * 256:hh * 256 + wd_]
                            mm(k, o, idb.t[:, :], mb, True, False, [idb, bb], [bk], inc=False)
                            mm(k, o, KT.t[pb:pb + 64, hp, b * 128:(b + 1) * 128],
                               QT.t[pb:pb + 64, hp, qlo * 128:qhi * 128], False, True, [KT, QT], [bk])
                        src = bk.t[:, :].rearrange("p (h q) -> p h q", q=256)[:, :, 0:wd_]
                        act(k, PT.t[:, b, 2 * hp:2 * hp + 2, 0:wd_], src, AF.Exp, [bk], [PT])
                if ATTN_STAGE < 3:
                    continue
                for qb in range(nQB):
                    oa = k.bank()
                    ob = k.bank()
                    for h_ in range(8):
                        bk = oa if h_ < 4 else ob
                        o = bk.t[:, (h_ % 4) * 66:(h_ % 4) * 66 + 66]
                        for b in (qb, qb + 1):
                            off = 0 if (b == qb + 1 or b == 0) else 128
                            mm(k, o, PT.t[:, b, h_, off:off + 128], VE.t[:, b, h_, :], b == qb, b == qb + 1,
                               [PT, VE], [bk], inc=(b == qb + 1 and h_ % 4 == 3))
                    os_ = osb[on_ % 2]
                    oc_ = oac[on_ % 2]
                    cp(k, "act", os_.t[:, 0:264], oa.t[:, 0:264], [oa], [os_])
                    cp(k, "dve", os_.t[:, 264:528], ob.t[:, 0:264], [ob], [os_])
                    qrow = PAD + slot * SLOT + r + d * (j0 + qb * 128)
                    orows = c.OACC[qrow:qrow + d * 127 + 1:d, :]
                    if gi == 0:
                        k.dma(orows, os_.t[:], [os_], [], os_)
                    else:
                        k.dma(oc_.t[:], orows, [], [oc_], oc_)
                        tt(k, "dve", os_.t[:], os_.t[:], oc_.t[:], ALU.add, [os_, oc_], [os_])
                        if not last:
                            k.dma(orows, os_.t[:], [os_], [], os_)
                    if last:
                        xr = xres[on_ % 2]
                        k.dma(xr.t[:], Xr[qrow:qrow + d * 127 + 1:d, :], [], [xr], xr)
                        ov = os_.t[:].rearrange("p (h e) -> p h e", e=66)
                        k.op("dve", lambda h: h.reciprocal(out=rden.t[:], in_=ov[:, :, 64]), [os_], [rden])
                        for h_ in range(8):
                            ts(k, "dve", on.t[:, h_ * 64:(h_ + 1) * 64], ov[:, h_, 0:64],
                               rden.t[:, h_:h_ + 1], None, ALU.mult, None, [os_, rden], [on])
                        bk = k.bank()
                        for kc in range(4):
                            k.op("pe", lambda h, kc=kc: h.transpose(out=bk.t[:, kc * 128:(kc + 1) * 128],
                                                                     in_=on.t[:, kc * 128:(kc + 1) * 128],
                                                                     identity=c.s_ident.t[:]),
                                 [on, c.s_ident], [bk], inc=(kc == 3))
                        cp(k, "act", oT.t[:].rearrange("p k t -> p (k t)"), bk.t[:, :], [bk], [oT])
                        for half in range(2):
                            bk = k.bank()
                            for kc in range(4):
                                mm(k, bk.t[:, :], oT.t[:, kc, :], wo.t[:, kc, half * 512:(half + 1) * 512], kc == 0,
                                   kc == 3, [oT, wo], [bk])
                            tm_ = tmo[half]
                            tt(k, "dve", tm_.t[:], bk.t[:, :], gb_.t[:, half * 512:(half + 1) * 512], ALU.mult,
                               [bk, gb_], [tm_])
                            tt(k, "pool", xr.t[:, half * 512:(half + 1) * 512], xr.t[:, half * 512:(half + 1) * 512],
                               tm_.t[:], ALU.add, [xr, tm_], [xr])
                        k.dma(Xw[qrow:qrow + d * 127 + 1:d, :], xr.t[:], [xr], [], xr)
                    on_ += 1
    k.end()


def phase_out(c, cur):
    k = c.k
    k.begin()
    z = k.sb("zo", [128, 8], F32)
    T = c.T
    for r0 in range(0, T, 512):
        k.dma(c.y[r0:r0 + 512, :], c.X[cur][PAD + r0:PAD + r0 + 512, :], [], [], z)
    k.end()


def _pm(w, kc):
    n = w.shape[-1]
    return np.ascontiguousarray(w.reshape(kc, 128, n).transpose(1, 0, 2)).reshape(128, kc * n)


def prep_weights(inp):
    f = np.float32
    W = {}
    ada_w = np.asarray(inp["ada_w"], f)
    W["ada_w"] = np.ascontiguousarray(ada_w.reshape(4, KC, 128, 6 * D).transpose(0, 2, 1, 3))
    ada_b = np.asarray(inp["ada_b"], f)
    W["ada_bT"] = np.ascontiguousarray(ada_b.reshape(4, 48, 128).transpose(2, 0, 1))
    brep = np.stack([ada_b[:, 2 * D:3 * D], ada_b[:, 5 * D:6 * D]], axis=1)
    W["ada_brep"] = np.ascontiguousarray(np.broadcast_to(brep[:, :, None, :], (4, 2, 128, D)))
    nm = np.stack([np.asarray(inp["norm_mix"], f), np.asarray(inp["norm_ffn"], f)], axis=1)
    W["nrm"] = np.ascontiguousarray(nm.reshape(4, 2, KC, 128).transpose(3, 0, 1, 2))
    cw = np.asarray(inp["rg_conv_w"], f)
    cb = np.asarray(inp["rg_conv_b"], f)
    cc = np.concatenate([cw, cb[:, None, :]], axis=1)
    W["rg_conv"] = np.ascontiguousarray(cc.reshape(2, 5, KC, 128).transpose(3, 0, 2, 1))
    gb = np.asarray(inp["rg_gate_b"], f)
    W["rg_gb"] = np.ascontiguousarray(gb.reshape(2, 2, 2, KC, 128).transpose(4, 0, 1, 2, 3))
    lam = np.asarray(inp["rg_lambda"], f)
    W["rg_lam"] = np.ascontiguousarray(lam.reshape(2, 2, KC, 128).transpose(3, 0, 1, 2))
    qn = np.asarray(inp["at_q_norm"], f)
    kn = np.asarray(inp["at_k_norm"], f)
    qk = np.stack([qn, kn], axis=2)
    W["at_qkn"] = np.ascontiguousarray(np.concatenate([qk, qk], axis=1).transpose(1, 0, 2))
    rt = np.asarray(inp["moe_router"], f)
    W["moe_rt"] = np.ascontiguousarray(rt.reshape(2, KC, 128, NEXP).transpose(2, 0, 1, 3))
    W["rg_w_in"] = np.stack([_pm(np.asarray(inp["rg_w_in"][j], f), KC) for j in range(2)])
    gw = np.asarray(inp["rg_gate_w"], f)
    W["rg_gate_w"] = np.ascontiguousarray(
        gw.reshape(2, 2, 2, 4, 2, 128, 256).transpose(0, 5, 1, 2, 3, 4, 6)).reshape(2, 128, 8192)
    W["rg_w_out"] = np.stack([_pm(np.asarray(inp["rg_w_out"][j], f), KC) for j in range(2)])
    qkv = np.asarray(inp["at_w_qkv"], f).reshape(2, D, 3, 1536)
    W["at_qkv"] = np.stack([_pm(np.ascontiguousarray(qkv[j, :, g, :]), KC) for j in range(2) for g in range(3)])
    W["at_wo"] = np.stack([_pm(np.asarray(inp["at_w_o"][j], f), 4) for j in range(2)])

    def gu_blocks(w, nff):
        dff = nff * 128
        g = w[:, :dff].reshape(KC, 128, nff, 128)
        u = w[:, dff:].reshape(KC, 128, nff, 128)
        gu_ = np.stack([g, u], axis=3)
        return np.ascontiguousarray(gu_.transpose(2, 1, 0, 3, 4)).reshape(nff, 128, KC * 256)

    W["ff_gu"] = np.concatenate([gu_blocks(np.asarray(inp["ff_w_gu"][j], f), 22) for j in range(2)])
    W["ff_dn"] = np.asarray(inp["ff_w_down"], f).reshape(2 * 22, 128, D)
    mg = np.asarray(inp["moe_w_gu"], f)
    md = np.asarray(inp["moe_w_down"], f)
    for j in range(2):
        for e in range(NEXP):
            W["moe_gu%d" % (j * NEXP + e)] = gu_blocks(mg[j, e], 28)
            W["moe_dn%d" % (j * NEXP + e)] = np.ascontiguousarray(md[j, e]).reshape(28, 128, D)
    W["ident"] = np.eye(128, dtype=f)
    bo = np.zeros((128, 128), f)
    bo[:64, :64] = 1
    bo[64:, 64:] = 1
    W["bones"] = bo
    ps = np.zeros((128, 128), f)
    for hb in (0, 64):
        for i in range(8):
            ps[hb + 8 + i, hb + i] = 1.0
            ps[hb + i, hb + 8 + i] = 1.0
    W["pswap"] = ps
    kk = np.arange(128)[:, None]
    cc_ = np.arange(256)[None, :]
    W["bandb"] = np.where((cc_ - kk >= 0) & (cc_ - kk <= 128), 0.0, -30000.0).astype(f)
    return W


def rope_tables(NSLOT, chained):
    f = np.float32
    T = NSLOT * SLOT
    inv = (500000.0 ** (-np.arange(0, 16, 2, dtype=np.float32) / 16)).astype(f)
    out = np.zeros((3, 2, 128, T + 128), f)
    out[:, 0] = 1.0
    dd = np.arange(128) % 64
    for gi, (_, d) in enumerate(GROUPS):
        L = SLOT // d
        r = np.arange(d)[:, None, None]
        s = np.arange(NSLOT)[None, :, None]
        jx = np.arange(L)[None, None, :]
        pos = (r + d * jx + (s * SLOT if chained else 0 * s)).astype(f).reshape(-1)
        ang = pos[None, :] * inv[:, None]
        cs = np.cos(ang).astype(f)
        sn = np.sin(ang).astype(f)
        for p in range(128):
            dq = dd[p]
            if dq < 8:
                out[gi, 0, p, 64:64 + T] = cs[dq]
                out[gi, 1, p, 64:64 + T] = -sn[dq]
            elif dq < 16:
                out[gi, 0, p, 64:64 + T] = cs[dq - 8]
                out[gi, 1, p, 64:64 + T] = sn[dq - 8]
    return out


def core_inputs(W, x, cvec, chained, NSLOT):
    f = np.float32
    m = dict(W)
    m["x_in"] = np.ascontiguousarray(x, dtype=f)
    m["cT"] = np.ascontiguousarray(np.asarray(cvec, f).reshape(NSLOT, KC, 128).transpose(2, 1, 0))
    fl = np.zeros((128, 2), f)
    fl[:, 0] = 1.0 if chained else 0.0
    m["flags"] = fl
    m["rope"] = rope_tables(NSLOT, chained)
    return m


_NC_CACHE = {}


def kernel(**inputs):
    NSLOT = 4
    W = prep_weights(inputs)
    xp = np.asarray(inputs["x_prompt"], np.float32)
    xs = np.asarray(inputs["x_sample"], np.float32)
    cp_ = np.asarray(inputs["c_prompt"], np.float32)
    cs = np.asarray(inputs["c_sample"], np.float32)
    in_maps = []
    in_maps.append(core_inputs(W, xp[0], np.repeat(cp_, NSLOT, axis=0), True, NSLOT))
    for ci in range(4):
        in_maps.append(core_inputs(W, xs[4 * ci:4 * ci + 4].reshape(NSLOT * SLOT, D), cs[4 * ci:4 * ci + 4], False, NSLOT))
    for ci in range(3):
        in_maps.append(in_maps[1 + ci])
    if "nc" not in _NC_CACHE:
        _NC_CACHE["nc"] = build(NSLOT)
    nc = _NC_CACHE["nc"]
    res = run_bass_kernel_spmd(nc, in_maps, core_ids=list(range(8)))
    yp = res.results[0]["y"].reshape(1, 4 * SLOT, D).astype(np.float32)
    ys = np.concatenate([res.results[1 + ci]["y"].reshape(4, SLOT, D) for ci in range(4)], axis=0).astype(np.float32)
    return (yp, ys)
```

```python
import numpy as np
from contextlib import ExitStack
import concourse.bass as bass
import concourse.mybir as mybir
from concourse.bass_utils import run_bass_kernel_spmd

F32 = mybir.dt.float32
BF16 = mybir.dt.bfloat16
AF = mybir.ActivationFunctionType
ALU = mybir.AluOpType

D = 1024
KC = 8
SLOT = 4096
PAD = 1024
DFF = 2816
DEX = 3584
NEXP = 8
GROUPS = ((128, 1), (512, 4), (2048, 16))
EPS = 1e-6
GEL_C = 1.5957691216057308
SKIP_ATTN = False
SKIP_FFN = False
ATTN_MAXTILES = None
ATTN_STAGE = 9
SKIP_CAST = False
FFN_MAXST = None
MOE_STAGE = 9
SAME_SYNC = True
ATTN_GROUPS = (0, 1, 2)


class Buf:
    __slots__ = ("name", "t", "w", "r", "ds")

    def __init__(self, name, t):
        self.name = name
        self.t = t
        self.w = None
        self.r = {}
        self.ds = None


class Eng:
    def __init__(self, key, h, sem):
        self.key = key
        self.h = h
        self.sem = sem
        self.n = 0
        self.known = {}


class K:
    def __init__(self, nc, es, ndsem=70):
        self.nc = nc
        self.es = es
        self.eng = {}
        self.sems = {}
        self.total = {}
        for key, h in (("pe", nc.tensor), ("act", nc.scalar), ("dve", nc.vector),
                       ("pool", nc.gpsimd), ("sp", nc.sync)):
            sem = es.enter_context(nc.semaphore("s_" + key))
            self.eng[key] = Eng(key, h, sem)
            self.sems[key] = sem
            self.total[key] = 0
        self.free_ds = []
        for i in range(ndsem):
            k = "d%d" % i
            self.sems[k] = es.enter_context(nc.semaphore("sd%d" % i))
            self.total[k] = 0
            self.free_ds.append(k)
        self.pstack = None
        self.pbufs = []
        self.ps = []
        for i in range(8):
            t = es.enter_context(nc.psum_tensor("psb%d" % i, [128, 512], F32))
            self.ps.append(Buf("psb%d" % i, t))
        self.psi = 0
        self.uid = 0

    def gsb(self, name, shape, dt):
        t = self.es.enter_context(self.nc.sbuf_tensor("g_" + name, list(shape), dt))
        return Buf(name, t)

    def sb(self, name, shape, dt):
        self.uid += 1
        t = self.pstack.enter_context(self.nc.sbuf_tensor("p_%s_%d" % (name, self.uid), list(shape), dt))
        b = Buf(name, t)
        self.pbufs.append(b)
        return b

    def bank(self):
        b = self.ps[self.psi]
        self.psi = (self.psi + 1) % 8
        return b

    def begin(self):
        self.pstack = ExitStack()
        self.pbufs = []

    def end(self):
        self.barrier()
        for b in self.pbufs:
            if b.ds is not None:
                self.free_ds.append(b.ds)
                b.ds = None
        self.pstack.close()
        self.pstack = None
        self.pbufs = []

    def _waits(self, E, rd, wr):
        need = {}
        for b in rd:
            if b.w is not None:
                k, v = b.w
                if need.get(k, 0) < v:
                    need[k] = v
        for b in wr:
            if b.w is not None:
                k, v = b.w
                if need.get(k, 0) < v:
                    need[k] = v
            for k, v in b.r.items():
                if need.get(k, 0) < v:
                    need[k] = v
        for k, v in need.items():
            if k == E.key and (k == "pe" or not SAME_SYNC):
                continue
            if E.known.get(k, 0) >= v:
                continue
            E.h.wait_ge(self.sems[k], v)
            E.known[k] = v

    def op(self, e, fn, rd=(), wr=(), inc=True):
        E = self.eng[e]
        self._waits(E, rd, wr)
        ins = fn(E.h)
        if inc:
            E.n += 1
            self.total[E.key] = E.n
            ins.then_inc(E.sem, 1)
            tok = (E.key, E.n)
        else:
            tok = (E.key, E.n + 1)
        for b in rd:
            if b.r.get(tok[0], 0) < tok[1]:
                b.r[tok[0]] = tok[1]
        for b in wr:
            b.w = tok
            b.r = {}
        return ins

    def dma(self, out, in_, rd, wr, owner, q="sp"):
        E = self.eng[q]
        self._waits(E, rd, wr)
        if owner.ds is None:
            owner.ds = self.free_ds.pop()
        k = owner.ds
        ins = E.h.dma_start(out=out, in_=in_)
        self.total[k] += 16
        ins.then_inc(self.sems[k], 16)
        tok = (k, self.total[k])
        for b in rd:
            if b.r.get(k, 0) < tok[1]:
                b.r[k] = tok[1]
        for b in wr:
            b.w = tok
            b.r = {}

    def barrier(self):
        for E in self.eng.values():
            for k, v in self.total.items():
                if v > E.known.get(k, 0):
                    if k == E.key and k == "pe":
                        continue
                    E.h.wait_ge(self.sems[k], v)
                    E.known[k] = v


def act(k, out, in_, func, rd, wr, **kw):
    return k.op("act", lambda h: h.activation(out=out, in_=in_, func=func, **kw), rd, wr)


def ts(k, e, out, in0, s1, s2, op0, op1, rd, wr):
    if s2 is None:
        return k.op(e, lambda h: h.tensor_scalar(out=out, in0=in0, scalar1=s1, scalar2=None, op0=op0), rd, wr)
    return k.op(e, lambda h: h.tensor_scalar(out=out, in0=in0, scalar1=s1, scalar2=s2, op0=op0, op1=op1), rd, wr)


def tt(k, e, out, in0, in1, op, rd, wr):
    return k.op(e, lambda h: h.tensor_tensor(out=out, in0=in0, in1=in1, op=op), rd, wr)


def stt(k, out, in0, scalar, in1, op0, op1, rd, wr):
    return k.op("dve", lambda h: h.scalar_tensor_tensor(out=out, in0=in0, scalar=scalar, in1=in1,
                                                        op0=op0, op1=op1), rd, wr)


def cp(k, e, out, in_, rd, wr):
    if e == "act":
        return act(k, out, in_, AF.Copy, rd, wr)
    return k.op(e, lambda h: h.tensor_copy(out=out, in_=in_), rd, wr)


def mm(k, out, lhsT, rhs, start, stop, rd, wr, inc=None):
    if inc is None:
        inc = stop
    return k.op("pe", lambda h: h.matmul(out, lhsT=lhsT, rhs=rhs, start=start, stop=stop), rd, wr, inc=inc)


class Ctx:
    pass


def build(NSLOT, layers=(0, 1, 2, 3), dbg=None):
    T = NSLOT * SLOT
    TP = T + 2 * PAD
    nc = bass.Bass("TRN2", target_bir_lowering=False)
    c = Ctx()
    c.nc = nc
    c.NSLOT = NSLOT
    c.T = T

    def din(name, shape, dt=F32):
        return nc.dram_tensor(name, list(shape), dt, kind="ExternalInput").ap()

    def dint(name, shape, dt):
        return nc.dram_tensor(name, list(shape), dt, kind="Internal").ap()

    c.x_in = din("x_in", [T, D])
    c.cT = din("cT", [128, KC, NSLOT])
    c.flags = din("flags", [128, 2])
    c.ident = din("ident", [128, 128])
    c.bones = din("bones", [128, 128])
    c.pswap = din("pswap", [128, 128])
    c.bandb = din("bandb", [128, 256])
    c.rope = din("rope", [3, 2, 128, T + 128])
    c.ada_w = din("ada_w", [4, 128, KC, 6 * D])
    c.ada_bT = din("ada_bT", [128, 4, 48])
    c.ada_brep = din("ada_brep", [4, 2, 128, D])
    c.nrm = din("nrm", [128, 4, 2, KC])
    c.rg_conv = din("rg_conv", [128, 2, KC, 5])
    c.rg_gb = din("rg_gb", [128, 2, 2, 2, KC])
    c.rg_lam = din("rg_lam", [128, 2, 2, KC])
    c.at_qkn = din("at_qkn", [128, 2, 2])
    c.moe_rt = din("moe_rt", [128, 2, KC, NEXP])
    wspec = {
        "rg_w_in": [2, 128, KC * 2048],
        "rg_gate_w": [2, 128, 8192],
        "rg_w_out": [2, 128, KC * D],
        "at_qkv": [6, 128, KC * 1536],
        "at_wo": [2, 128, 4 * D],
        "ff_gu": [2 * 22, 128, KC * 256],
        "ff_dn": [2 * 22, 128, D],
    }
    for i in range(2 * NEXP):
        wspec["moe_gu%d" % i] = [28, 128, KC * 256]
        wspec["moe_dn%d" % i] = [28, 128, D]
    c.wf = {}
    c.wb = {}
    for n, shp in wspec.items():
        c.wf[n] = din(n, shp)
        c.wb[n] = dint(n + "_b", shp, BF16)
    c.y = nc.dram_tensor("y", [T, D], F32, kind="ExternalOutput").ap()
    c.X = [dint("XA", [TP, D], F32), dint("XB", [TP, D], F32)]
    c.XBs = dint("XBs", [KC, 128, T + 8], F32)
    c.GYs = dint("GYs", [KC, 128, T], BF16)
    c.XCs = dint("XCs", [KC, 128, T], F32)
    c.HFs = dint("HFs", [KC, 128, T], F32)
    c.OACC = dint("OACC", [TP, 528], F32)
    c.GREP = dint("GREP", [4, 2, NSLOT, 128, D], F32)
    if dbg is not None:
        c.dbg = nc.dram_tensor("dbg", list(dbg), F32, kind="ExternalOutput").ap()

    es = ExitStack()
    with es:
        k = K(nc, es)
        c.k = k
        c.s_ident = k.gsb("ident", [128, 128], F32)
        c.s_flags = k.gsb("flags", [128, 2], F32)
        c.s_mods = k.gsb("mods", [128, 4, 4, KC, NSLOT], F32)
        k.dma(c.s_ident.t[:], c.ident[:, :], [], [c.s_ident], c.s_ident)
        k.dma(c.s_flags.t[:], c.flags[:, :], [], [c.s_flags], c.s_flags)
        phase_init(c)
        if not SKIP_CAST:
            phase_cast(c, layers)
        phase_mods(c, layers)
        cur = 0
        for li in layers:
            j = li // 2
            if li % 2 == 0:
                phase_r1(c, li, j, cur)
                phase_r2(c, li, j)
                phase_r3(c, li, j, cur)
                phase_ffn(c, li, j, cur, moe=False)
            else:
                if not SKIP_ATTN:
                    for gi in ATTN_GROUPS:
                        phase_attn(c, li, j, gi, cur)
                    cur = 1 - cur
                if not SKIP_FFN:
                    phase_ffn(c, li, j, cur, moe=True)
        phase_out(c, cur)
    return nc


def phase_init(c):
    k = c.k
    k.begin()
    z = k.sb("z", [128, D], F32)
    k.op("pool", lambda h: h.memset(z.t[:], 0.0), [], [z])
    dX = [Buf("XA", None), Buf("XB", None)]
    T = c.T
    for xi in range(2):
        X = c.X[xi]
        for r0 in list(range(0, PAD, 128)) + list(range(PAD + T, PAD + T + PAD, 128)):
            k.dma(X[r0:r0 + 128, :], z.t[:], [z], [dX[xi]], z)
    for r0 in range(0, T, 512):
        k.dma(c.X[0][PAD + r0:PAD + r0 + 512, :], c.x_in[r0:r0 + 512, :], [], [dX[0]], z)
    k.end()


def phase_cast(c, layers):
    k = c.k
    k.begin()
    need = set()
    for li in layers:
        if li % 2 == 0:
            need |= {("rg_w_in", li // 2), ("rg_gate_w", li // 2), ("rg_w_out", li // 2),
                     ("ff_gu", li // 2), ("ff_dn", li // 2)}
        else:
            need |= {("at_qkv", li // 2), ("at_wo", li // 2)}
            if not SKIP_FFN:
                need |= {("moe_gu", li // 2), ("moe_dn", li // 2)}
    NB = 3
    st = [k.sb("cst%d" % i, [128, 4096], F32) for i in range(NB)]
    bf = [k.sb("cbf%d" % i, [128, 4096], BF16) for i in range(NB)]
    engs = ["pool", "dve", "act"]
    it = 0
    for n in c.wf:
        src = c.wf[n]
        dst = c.wb[n]
        nb, _, F = src.shape
        per = nb // 2
        ismoe = n.startswith("moe_")
        if ismoe:
            per = nb
        for jj in range(1 if ismoe else 2):
            if ismoe:
                if (n[:6], int(n[6:]) // NEXP) not in need:
                    continue
            elif (n, jj) not in need:
                continue
            if F >= 4096:
                units = [(b, f0, 1) for b in range(jj * per, (jj + 1) * per) for f0 in range(0, F, 4096)]
            else:
                g = 4096 // F
                units = []
                b = jj * per
                while b < (jj + 1) * per:
                    gg = min(g, (jj + 1) * per - b)
                    units.append((b, 0, gg))
                    b += gg
            for (b, f0, gg) in units:
                s = st[it % NB]
                d = bf[it % NB]
                e = engs[it % 3]
                it += 1
                if F >= 4096:
                    k.dma(s.t[:, :], src[b, :, f0:f0 + 4096], [], [s], s)
                    cp(k, e, d.t[:, :], s.t[:, :], [s], [d])
                    k.dma(dst[b, :, f0:f0 + 4096], d.t[:, :], [d], [], d)
                else:
                    sv = s.t[:, 0:gg * F].rearrange("p (n f) -> p n f", f=F)
                    dv = d.t[:, 0:gg * F].rearrange("p (n f) -> p n f", f=F)
                    k.dma(sv, src[b:b + gg, :, :].rearrange("n p f -> p n f"), [], [s], s)
                    cp(k, e, d.t[:, 0:gg * F], s.t[:, 0:gg * F], [s], [d])
                    k.dma(dst[b:b + gg, :, :].rearrange("n p f -> p n f"), dv, [d], [], d)
    k.end()


def phase_mods(c, layers):
    k = c.k
    NS = c.NSLOT
    k.begin()
    cT = k.sb("cT", [128, KC, NS], F32)
    sc = k.sb("sc", [128, KC, NS], F32)
    e1 = k.sb("e1", [128, KC, NS], F32)
    ones = k.sb("ones", [128, 128], F32)
    screp = k.sb("screp", [128, KC, NS, 128], F32)
    abT = k.sb("abT", [128, 4, 48], F32)
    nrm = k.sb("nrm", [128, 4, 2, KC], F32)
    tmp = k.sb("tmp", [128, KC, NS], F32)
    w = [k.sb("w%d" % i, [128, KC, D], F32) for i in range(2)]
    brep = [k.sb("brep%d" % i, [128, D], F32) for i in range(2)]
    go = [k.sb("go%d" % i, [128, D], F32) for i in range(2)]
    k.dma(cT.t[:], c.cT[:, :, :], [], [cT], cT)
    k.dma(abT.t[:], c.ada_bT[:, :, :], [], [abT], abT)
    k.dma(nrm.t[:], c.nrm[:, :, :, :], [], [nrm], nrm)
    k.op("pool", lambda h: h.memset(ones.t[:], 1.0), [], [ones])
    act(k, e1.t[:], cT.t[:], AF.Exp, [cT], [e1], scale=-1.0)
    ts(k, "dve", e1.t[:], e1.t[:], 1.0, None, ALU.add, None, [e1], [e1])
    k.op("dve", lambda h: h.reciprocal(out=e1.t[:], in_=e1.t[:]), [e1], [e1])
    tt(k, "dve", sc.t[:], cT.t[:], e1.t[:], ALU.mult, [cT, e1], [sc])
    for kc in range(KC):
        for s in range(NS):
            ts(k, "pool", screp.t[:, kc, s, :], ones.t[:], sc.t[:, kc, s:s + 1], None, ALU.mult, None,
               [ones, sc], [screp])
    wi = 0
    gi_ = 0
    for li in layers:
        for part in range(6):
            wt = w[wi % 2]
            wi += 1
            k.dma(wt.t[:], c.ada_w[li, :, :, part * D:(part + 1) * D], [], [wt], wt)
            if part in (2, 5):
                g = 0 if part == 2 else 1
                br = brep[gi_ % 2]
                k.dma(br.t[:], c.ada_brep[li, g, :, :], [], [br], br)
                for s in range(NS):
                    gt = go[gi_ % 2]
                    gi_ += 1
                    for half in range(2):
                        bk = k.bank()
                        for kc in range(KC):
                            mm(k, bk.t[:, :], screp.t[:, kc, s, :], wt.t[:, kc, half * 512:(half + 1) * 512],
                               kc == 0, kc == KC - 1, [screp, wt], [bk])
                        tt(k, "dve", gt.t[:, half * 512:(half + 1) * 512], bk.t[:, :],
                           br.t[:, half * 512:(half + 1) * 512], ALU.add, [bk, br], [gt])
                    k.dma(c.GREP[li, g, s, :, :], gt.t[:], [gt], [], gt)
            else:
                bk = k.bank()
                for oc in range(KC):
                    for kc in range(KC):
                        mm(k, bk.t[:, oc * NS:(oc + 1) * NS], wt.t[:, kc, oc * 128:(oc + 1) * 128],
                           sc.t[:, kc, :], kc == 0, kc == KC - 1, [wt, sc], [bk])
                bv = bk.t[:, 0:KC * NS].rearrange("p (o s) -> p o s", s=NS)
                for s in range(NS):
                    tt(k, "dve", tmp.t[:, :, s], bv[:, :, s], abT.t[:, li, part * 8:(part + 1) * 8], ALU.add,
                       [bk, abT], [tmp])
                which = 0 if part < 3 else 1
                if part in (1, 4):
                    for s in range(NS):
                        stt(k, c.s_mods.t[:, li, 2 * which, :, s], tmp.t[:, :, s], 1.0, nrm.t[:, li, which, :],
                            ALU.add, ALU.mult, [tmp, nrm], [c.s_mods])
                else:
                    cp(k, "dve", c.s_mods.t[:, li, 2 * which + 1, :, :], tmp.t[:, :, :], [tmp], [c.s_mods])
    k.end()


def norm_block(c, xin, nsub, ss, lnv, rstd, junk, engs=("dve", "act")):
    k = c.k
    for u in range(nsub):
        act(k, junk.t[:], xin.t[:, u, :], AF.Square, [xin], [junk, ss], accum_out=ss.t[:, u:u + 1])
    act(k, lnv.t[:, 0:nsub], ss.t[:, 0:nsub], AF.Ln, [ss], [lnv], scale=1.0 / D, bias=EPS)
    act(k, rstd.t[:, 0:nsub], lnv.t[:, 0:nsub], AF.Exp, [lnv], [rstd], scale=-0.5)
    for u in range(nsub):
        e = engs[u % len(engs)]
        if e == "act":
            act(k, xin.t[:, u, :], xin.t[:, u, :], AF.Identity, [xin, rstd], [xin], scale=rstd.t[:, u:u + 1])
        else:
            ts(k, e, xin.t[:, u, :], xin.t[:, u, :], rstd.t[:, u:u + 1], None, ALU.mult, None,
               [xin, rstd], [xin])


def transpose_block(c, xin, nsub, hT, col0, li, which, slot, extra=None):
    k = c.k
    A = c.s_mods.t[:, li, 2 * which, :, :]
    B = c.s_mods.t[:, li, 2 * which + 1, :, :]
    n = 0
    for kc in range(KC):
        for u0 in range(0, nsub, 4):
            nu = min(4, nsub - u0)
            bk = k.bank()
            for u in range(nu):
                k.op("pe", lambda h, u=u: h.transpose(out=bk.t[:, u * 128:(u + 1) * 128],
                                                       in_=xin.t[:, u0 + u, kc * 128:(kc + 1) * 128],
                                                       identity=c.s_ident.t[:]),
                     [xin, c.s_ident], [bk], inc=(u == nu - 1))
            o = hT.t[:, kc, col0 + u0 * 128: col0 + (u0 + nu) * 128]
            if n % 2 == 0:
                act(k, o, bk.t[:, 0:nu * 128], AF.Identity, [bk, c.s_mods], [hT],
                    scale=A[:, kc, slot:slot + 1], bias=B[:, kc, slot:slot + 1])
            else:
                ts(k, "dve", o, bk.t[:, 0:nu * 128], A[:, kc, slot:slot + 1], B[:, kc, slot:slot + 1],
                   ALU.mult, ALU.add, [bk, c.s_mods], [hT])
            if extra is not None:
                extra(kc, u0, nu, bk)
            n += 1


def phase_r1(c, li, j, cur):
    k = c.k
    T = c.T
    X = c.X[cur]
    k.begin()
    w_in = k.sb("w_in", [128, KC, 2048], BF16)
    k.dma(w_in.t[:].rearrange("p k n -> p (k n)"), c.wb["rg_w_in"][j, :, :], [], [w_in], w_in)
    xin = [k.sb("xin%d" % i, [128, 4, D], F32) for i in range(2)]
    junk = k.sb("junk", [128, D], F32)
    sm = [[k.sb("sm%d_%d" % (i, q), [128, 4], F32) for q in range(3)] for i in range(2)]
    hT = [k.sb("hT%d" % i, [128, KC, 512], BF16) for i in range(2)]
    xbT = [k.sb("xbT%d" % i, [128, KC, 512], F32) for i in range(2)]
    gyT = [k.sb("gyT%d" % i, [128, KC, 512], BF16) for i in range(2)]
    tm = [[k.sb("tm%d_%d" % (i, q), [128, 512], F32) for q in range(4)] for i in range(2)]
    ntile = T // 512

    def load(i):
        k.dma(xin[i % 2].t[:], X[PAD + i * 512:PAD + i * 512 + 512, :].rearrange("(s p) f -> p s f", p=128),
              [], [xin[i % 2]], xin[i % 2])

    load(0)
    for i in range(ntile):
        t0 = i * 512
        slot = i // 8
        xi = xin[i % 2]
        if i + 1 < ntile:
            load(i + 1)
        ss, lnv, rstd = sm[i % 2]
        norm_block(c, xi, 4, ss, lnv, rstd, junk)
        h = hT[i % 2]
        transpose_block(c, xi, 4, h, 0, li, 0, slot)
        xo = xbT[i % 2]
        go = gyT[i % 2]
        for oc in range(16):
            bk = k.bank()
            for kc in range(KC):
                mm(k, bk.t[:, :], w_in.t[:, kc, oc * 128:(oc + 1) * 128], h.t[:, kc, :], kc == 0, kc == KC - 1,
                   [w_in, h], [bk])
            if oc < 8:
                cp(k, "act" if oc % 2 == 0 else "dve", xo.t[:, oc, :], bk.t[:, :], [bk], [xo])
            else:
                xs, sq, inn, sg = tm[oc % 2]
                cp(k, "act", xs.t[:], bk.t[:, :], [bk], [xs])
                act(k, sq.t[:], bk.t[:, :], AF.Square, [bk], [sq])
                ts(k, "dve", inn.t[:], sq.t[:], 0.044715, 1.0, ALU.mult, ALU.add, [sq], [inn])
                tt(k, "dve", inn.t[:], inn.t[:], xs.t[:], ALU.mult, [inn, xs], [inn])
                act(k, sg.t[:], inn.t[:], AF.Exp, [inn], [sg], scale=-GEL_C)
                act(k, sg.t[:], sg.t[:], AF.Ln, [sg], [sg], bias=1.0)
                act(k, sg.t[:], sg.t[:], AF.Exp, [sg], [sg], scale=-1.0)
                tt(k, "dve", go.t[:, oc - 8, :], xs.t[:], sg.t[:], ALU.mult, [xs, sg], [go])
        k.dma(c.XBs[:, :, 4 + t0:4 + t0 + 512].rearrange("c p t -> p c t"), xo.t[:], [xo], [], xo)
        k.dma(c.GYs[:, :, t0:t0 + 512].rearrange("c p t -> p c t"), go.t[:], [go], [], go)
    k.end()


def rg_consts(c, j, z):
    k = c.k
    gw = k.sb("gw", [128, 8192], BF16)
    k.dma(gw.t[:], c.wb["rg_gate_w"][j, :, :], [], [gw], gw)
    gb = k.sb("gb", [128, 2, 2, 2, KC], F32)
    k.dma(gb.t[:], c.rg_gb[:, :, :, :, :], [], [gb], gb)
    negb = k.sb("negb", [128, 2, KC], F32)
    ts(k, "dve", negb.t[:], gb.t[:, j, z, :, :], -1.0, None, ALU.mult, None, [gb], [negb])
    lam = k.sb("lam", [128, 2, 2, KC], F32)
    k.dma(lam.t[:], c.rg_lam[:, :, :, :], [], [lam], lam)
    c8 = k.sb("c8", [128, KC], F32)
    c16 = k.sb("c16", [128, KC], F32)
    act(k, c8.t[:], lam.t[:, j, z, :], AF.Exp, [lam], [c8], scale=-1.0)
    act(k, c8.t[:], c8.t[:], AF.Ln, [c8], [c8], bias=1.0)
    ts(k, "dve", c16.t[:], c8.t[:], -16.0, None, ALU.mult, None, [c8], [c16])
    ts(k, "dve", c8.t[:], c8.t[:], -8.0, None, ALU.mult, None, [c8], [c8])
    return gw, negb, c8, c16


def rg_gates(c, z, gw, negb, c8, c16, xc, xcb, tmps, hout, carry, reverse):
    k = c.k
    gv = gw.t[:].rearrange("p (z g n k d) -> p z g n k d", z=2, g=2, n=4, k=2)
    for g0 in (0, 4):
        ocs = list(range(g0, g0 + 4))
        bk_r = {}
        bk_i = {}
        for oc in ocs:
            n_, half = oc // 2, oc % 2
            bk_r[oc] = k.bank()
            for kk in range(2):
                mm(k, bk_r[oc].t[:, :], gv[:, z, 0, n_, kk, half * 128:(half + 1) * 128], xcb.t[:, 2 * n_ + kk, :],
                   kk == 0, kk == 1, [gw, xcb], [bk_r[oc]])
            bk_i[oc] = k.bank()
            for kk in range(2):
                mm(k, bk_i[oc].t[:, :], gv[:, z, 1, n_, kk, half * 128:(half + 1) * 128], xcb.t[:, 2 * n_ + kk, :],
                   kk == 0, kk == 1, [gw, xcb], [bk_i[oc]])
        T_ = {oc: tmps[oc % 4] for oc in ocs}
        for oc in ocs:
            er, ei, a, a2, u = T_[oc]
            act(k, er.t[:], bk_r[oc].t[:, :], AF.Exp, [bk_r[oc], negb], [er], scale=-1.0, bias=negb.t[:, 0, oc:oc + 1])
            act(k, ei.t[:], bk_i[oc].t[:, :], AF.Exp, [bk_i[oc], negb], [ei], scale=-1.0, bias=negb.t[:, 1, oc:oc + 1])
        for oc in ocs:
            er = T_[oc][0]
            ts(k, "dve", er.t[:], er.t[:], 1.0, None, ALU.add, None, [er], [er])
        for oc in ocs:
            ei = T_[oc][1]
            act(k, ei.t[:], ei.t[:], AF.Ln, [ei], [ei], bias=1.0)
        for oc in ocs:
            er = T_[oc][0]
            k.op("dve", lambda h, er=er: h.reciprocal(out=er.t[:], in_=er.t[:]), [er], [er])
        for oc in ocs:
            ei = T_[oc][1]
            act(k, ei.t[:], ei.t[:], AF.Exp, [ei], [ei], scale=-1.0)
        for oc in ocs:
            er, ei, a, a2, u = T_[oc]
            tt(k, "pool", u.t[:], ei.t[:], xc.t[:, oc, :], ALU.mult, [ei, xc], [u])
        for oc in ocs:
            er, ei, a, a2, u = T_[oc]
            act(k, a.t[:], er.t[:], AF.Exp, [er, c8], [a], scale=c8.t[:, oc:oc + 1])
        for oc in ocs:
            er, ei, a, a2, u = T_[oc]
            act(k, a2.t[:], er.t[:], AF.Exp, [er, c16], [a2], scale=c16.t[:, oc:oc + 1])
        for oc in ocs:
            a2 = T_[oc][3]
            act(k, a2.t[:], a2.t[:], AF.Ln, [a2], [a2], scale=-1.0, bias=1.0)
        for oc in ocs:
            a2 = T_[oc][3]
            act(k, a2.t[:], a2.t[:], AF.Exp, [a2], [a2], scale=0.5)
        for oc in ocs:
            er, ei, a, a2, u = T_[oc]
            tt(k, "dve", u.t[:], u.t[:], a2.t[:], ALU.mult, [u, a2], [u])
        for oc in ocs:
            er, ei, a, a2, u = T_[oc]
            cr = carry[oc]
            if reverse:
                k.op("dve", lambda h, a=a, u=u, cr=cr, oc=oc: h.tensor_tensor_scan(
                    out=hout[oc].t[:, oc, ::-1], data0=a.t[:, ::-1], data1=u.t[:, ::-1], initial=cr.t[:, 0:1],
                    op0=ALU.mult, op1=ALU.add), [a, u, cr], [hout[oc]])
                cp(k, "pool", cr.t[:, 0:1], hout[oc].t[:, oc, 0:1], [hout[oc]], [cr])
            else:
                k.op("dve", lambda h, a=a, u=u, cr=cr, oc=oc: h.tensor_tensor_scan(
                    out=hout[oc].t[:, oc, :], data0=a.t[:, :], data1=u.t[:, :], initial=cr.t[:, 0:1],
                    op0=ALU.mult, op1=ALU.add), [a, u, cr], [hout[oc]])
                cp(k, "pool", cr.t[:, 0:1], hout[oc].t[:, oc, 511:512], [hout[oc]], [cr])


def phase_r2(c, li, j):
    k = c.k
    T = c.T
    k.begin()
    gw, negb, c8, c16 = rg_consts(c, j, 0)
    cw = k.sb("cw", [128, 2, KC, 5], F32)
    k.dma(cw.t[:], c.rg_conv[:, :, :, :], [], [cw], cw)
    dg = k.sb("dg", [128, KC, 4, 128], F32)
    for ch in range(KC):
        for tp in range(4):
            ts(k, "pool", dg.t[:, ch, tp, :], c.s_ident.t[:], cw.t[:, j, ch, tp:tp + 1], None, ALU.mult, None,
               [c.s_ident, cw], [dg])
    xbw = [k.sb("xbw%d" % i, [128, KC, 515], F32) for i in range(2)]
    xc = [k.sb("xc%d" % i, [128, KC, 512], F32) for i in range(2)]
    xcb = [k.sb("xcb%d" % i, [128, KC, 512], BF16) for i in range(2)]
    hf = [k.sb("hf%d" % i, [128, KC, 512], F32) for i in range(2)]
    hfv = [[hf[i]] + [Buf("hfv", hf[i].t) for _ in range(KC - 1)] for i in range(2)]
    tmps = [[k.sb("gt%d_%d" % (i, q), [128, 512], F32) for q in range(5)] for i in range(4)]
    carry = [k.sb("carry%d" % i, [128, 1], F32) for i in range(KC)]
    for cr in carry:
        k.op("pool", lambda h, cr=cr: h.memset(cr.t[:], 0.0), [], [cr])
    ntile = T // 512
    def load(i):
        k.dma(xbw[i % 2].t[:], c.XBs[:, :, 4 + i * 512 - 2:4 + i * 512 + 513].rearrange("c p t -> p c t"),
              [], [xbw[i % 2]], xbw[i % 2])

    load(0)
    for i in range(ntile):
        t0 = i * 512
        xw = xbw[i % 2]
        if i + 1 < ntile:
            load(i + 1)
        if i == 0:
            k.op("pool", lambda h: h.memset(xw.t[:, :, 0:2], 0.0), [], [xw])
        elif i % 8 == 0:
            ts(k, "pool", xw.t[:, :, 0:2], xw.t[:, :, 0:2], c.s_flags.t[:, 0:1], None, ALU.mult, None,
               [xw, c.s_flags], [xw])
            for cr in carry:
                ts(k, "pool", cr.t[:], cr.t[:], c.s_flags.t[:, 0:1], None, ALU.mult, None,
                   [cr, c.s_flags], [cr])
        if i == ntile - 1:
            k.op("pool", lambda h: h.memset(xw.t[:, :, 514:515], 0.0), [], [xw])
        elif i % 8 == 7:
            ts(k, "pool", xw.t[:, :, 514:515], xw.t[:, :, 514:515], c.s_flags.t[:, 0:1], None, ALU.mult, None,
               [xw, c.s_flags], [xw])
        xcc = xc[i % 2]
        xcbb = xcb[i % 2]
        for ch in range(KC):
            bk = k.bank()
            for tp in range(4):
                mm(k, bk.t[:, :], dg.t[:, ch, tp, :], xw.t[:, ch, tp:tp + 512], tp == 0, tp == 3, [dg, xw], [bk])
            act(k, xcc.t[:, ch, :], bk.t[:, :], AF.Identity, [bk, cw], [xcc], bias=cw.t[:, j, ch, 4:5])
            cp(k, "pool", xcbb.t[:, ch, :], xcc.t[:, ch, :], [xcc], [xcbb])
        hh = hfv[i % 2]
        rg_gates(c, 0, gw, negb, c8, c16, xcc, xcbb, tmps, hh, carry, False)
        k.dma(c.XCs[:, :, t0:t0 + 512].rearrange("c p t -> p c t"), xcc.t[:], [xcc], [], xcc)
        k.dma(c.HFs[:, :, t0:t0 + 512].rearrange("c p t -> p c t"), hh[0].t[:], hh, [], hh[0])
    k.end()


def phase_r3(c, li, j, cur):
    k = c.k
    T = c.T
    X = c.X[cur]
    k.begin()
    gw, negb, c8, c16 = rg_consts(c, j, 1)
    w_out = k.sb("w_out", [128, KC, D], BF16)
    k.dma(w_out.t[:].rearrange("p k n -> p (k n)"), c.wb["rg_w_out"][j, :, :], [], [w_out], w_out)
    xcs = [k.sb("xc%d" % i, [128, KC, 512], F32) for i in range(2)]
    xcb = k.sb("xcb", [128, KC, 512], BF16)
    hfl = k.sb("hfl", [128, KC, 512], F32)
    gyl = k.sb("gyl", [128, KC, 512], BF16)
    hb = k.sb("hb", [128, KC, 512], F32)
    hbv = [hb] + [Buf("hbv", hb.t) for _ in range(KC - 1)]
    yT = k.sb("yT", [128, KC, 512], BF16)
    xin = k.sb("xin", [128, 4, D], F32)
    g1 = [k.sb("g1_%d" % i, [128, D], F32) for i in range(2)]
    tmps = [[k.sb("gt%d_%d" % (i, q), [128, 512], F32) for q in range(5)] for i in range(4)]
    tmo = [k.sb("tmo%d" % i, [128, 512], F32) for i in range(2)]
    carry = [k.sb("carry%d" % i, [128, 1], F32) for i in range(KC)]
    for cr in carry:
        k.op("pool", lambda h, cr=cr: h.memset(cr.t[:], 0.0), [], [cr])
    ntile = T // 512
    gcur = None
    for i in range(ntile - 1, -1, -1):
        t0 = i * 512
        slot = i // 8
        if gcur is None or gcur[0] != slot:
            gb_ = g1[slot % 2]
            k.dma(gb_.t[:], c.GREP[li, 0, slot, :, :], [], [gb_], gb_)
            gcur = (slot, gb_)
        gb_ = gcur[1]
        xc = xcs[i % 2]
        if i == ntile - 1:
            k.dma(xc.t[:], c.XCs[:, :, t0:t0 + 512].rearrange("c p t -> p c t"), [], [xc], xc)
        if i - 1 >= 0:
            xn_ = xcs[(i - 1) % 2]
            k.dma(xn_.t[:], c.XCs[:, :, t0 - 512:t0].rearrange("c p t -> p c t"), [], [xn_], xn_)
        k.dma(hfl.t[:], c.HFs[:, :, t0:t0 + 512].rearrange("c p t -> p c t"), [], [hfl], hfl)
        k.dma(gyl.t[:], c.GYs[:, :, t0:t0 + 512].rearrange("c p t -> p c t"), [], [gyl], gyl)
        k.dma(xin.t[:], X[PAD + t0:PAD + t0 + 512, :].rearrange("(s p) f -> p s f", p=128), [], [xin], xin)
        if i % 8 == 7 and i != ntile - 1:
            for cr in carry:
                ts(k, "pool", cr.t[:], cr.t[:], c.s_flags.t[:, 0:1], None, ALU.mult, None,
                   [cr, c.s_flags], [cr])
        for ch in range(KC):
            cp(k, "pool", xcb.t[:, ch, :], xc.t[:, ch, :], [xc], [xcb])
        rg_gates(c, 1, gw, negb, c8, c16, xc, xcb, tmps, hbv, carry, True)
        for ch in range(KC):
            tt(k, "pool", hfl.t[:, ch, :], hfl.t[:, ch, :], hb.t[:, ch, :], ALU.add, [hfl, hbv[ch]], [hfl])
            tt(k, "dve", yT.t[:, ch, :], hfl.t[:, ch, :], gyl.t[:, ch, :], ALU.mult, [hfl, gyl], [yT])
        n = 0
        for u in range(4):
            for half in range(2):
                bk = k.bank()
                for kc in range(KC):
                    mm(k, bk.t[:, :], yT.t[:, kc, u * 128:(u + 1) * 128], w_out.t[:, kc, half * 512:(half + 1) * 512],
                       kc == 0, kc == KC - 1, [yT, w_out], [bk])
                tm_ = tmo[n % 2]
                n += 1
                tt(k, "dve", tm_.t[:], bk.t[:, :], gb_.t[:, half * 512:(half + 1) * 512], ALU.mult, [bk, gb_], [tm_])
                tt(k, "pool", xin.t[:, u, half * 512:(half + 1) * 512], xin.t[:, u, half * 512:(half + 1) * 512],
                   tm_.t[:], ALU.add, [xin, tm_], [xin])
        k.dma(X[PAD + t0:PAD + t0 + 512, :].rearrange("(s p) f -> p s f", p=128), xin.t[:], [xin], [], xin)
    k.end()


def phase_ffn(c, li, j, cur, moe):
    k = c.k
    T = c.T
    X = c.X[cur]
    ST = 1024
    k.begin()
    xin = [k.sb("xin%d" % i, [128, 2, D], F32) for i in range(2)]
    junk = k.sb("junk", [128, D], F32)
    sm = [[k.sb("sm%d_%d" % (i, q), [128, 4], F32) for q in range(3)] for i in range(2)]
    hTs = [k.sb("hT%d" % i, [128, KC, ST], BF16) for i in range(2)]
    acc = k.sb("acc", [128, 8, D], F32)
    SL = 4
    hs = [k.sb("hs%d" % i, [128, SL, ST], BF16) for i in range(2)]
    wgu = [k.sb("wgu%d" % i, [128, KC, 256], BF16) for i in range(4)]
    wdn = [k.sb("wdn%d" % i, [128, SL, D], BF16) for i in range(2)]
    sg = [k.sb("sg%d" % i, [128, 512], F32) for i in range(4)]
    tmo = [k.sb("tmo%d" % i, [128, 512], F32) for i in range(2)]
    g2 = [k.sb("g2_%d" % i, [128, D], F32) for i in range(2)]
    if moe:
        rt = k.sb("rt", [128, 2, KC, NEXP], F32)
        k.dma(rt.t[:], c.moe_rt[:, :, :, :], [], [rt], rt)
        rtb = k.sb("rtb", [128, KC, 128], BF16)
        k.op("pool", lambda h: h.memset(rtb.t[:], 0.0), [], [rtb])
        cp(k, "dve", rtb.t[:, :, 0:NEXP], rt.t[:, j, :, :], [rt], [rtb])
        lgs = [k.sb("lg%d" % i, [128, 8, NEXP], F32) for i in range(2)]
        comb = k.sb("comb", [128, 8, NEXP], F32)
        m1 = k.sb("m1", [128, 8], F32)
        m2 = k.sb("m2", [128, 8], F32)
        k1 = k.sb("k1", [128, 8, NEXP], F32)
        k2 = k.sb("k2", [128, 8, NEXP], F32)
        l2 = k.sb("l2", [128, 8, NEXP], F32)
        w1 = k.sb("w1", [128, 8], F32)
        w2 = k.sb("w2", [128, 8], F32)
    if moe:
        nch = 28
        experts = range(NEXP)
        gu_src = None
        dn_src = None
    else:
        nch = 22
        experts = [0]
        gu_src = c.wb["ff_gu"]
        dn_src = c.wb["ff_dn"]
    slabs = []
    for e in experts:
        f = 0
        while f < nch:
            n = min(SL, nch - f)
            base = 0 if moe else (j * 22)
            slabs.append((e, base + f, n))
            f += n
    nst = T // ST
    wg_i = 0
    evn = 0
    gcur = None
    def get_g2(slot):
        nonlocal gcur
        if gcur is None or gcur[0] != slot:
            gb__ = g2[slot % 2]
            k.dma(gb__.t[:], c.GREP[li, 1, slot, :, :], [], [gb__], gb__)
            gcur = (slot, gb__)
        return gcur[1]

    def normT(q):
        t0 = q * ST
        slot = t0 // SLOT
        hTq = hTs[q % 2]
        lgq = lgs[q % 2] if moe else None
        for pr in range(4):
            xi = xin[pr % 2]
            r0 = PAD + t0 + pr * 256
            k.dma(xi.t[:], X[r0:r0 + 256, :].rearrange("(s p) f -> p s f", p=128), [], [xi], xi)
            ss, lnv, rstd = sm[pr % 2]
            norm_block(c, xi, 2, ss, lnv, rstd, junk, engs=("act", "dve"))
            transpose_block(c, xi, 2, hTq, pr * 256, li, 1, slot)
            if moe:
                for u in range(2):
                    sub = pr * 2 + u
                    bk = k.bank()
                    for kc in range(KC):
                        mm(k, bk.t[:, 0:128], hTq.t[:, kc, sub * 128:(sub + 1) * 128], rtb.t[:, kc, :], kc == 0,
                           kc == KC - 1, [hTq, rtb], [bk])
                    cp(k, "dve", lgq.t[:, sub, :], bk.t[:, 0:NEXP], [bk], [lgq])

    if FFN_MAXST is not None:
        nst = min(nst, FFN_MAXST)
    normT(0)
    for q in range(nst):
        t0 = q * ST
        slot = t0 // SLOT
        gb_ = get_g2(slot)
        hT = hTs[q % 2]
        if moe:
            lg = lgs[q % 2]
        if moe and MOE_STAGE < 1:
            k.op("pool", lambda h: h.memset(comb.t[:], 0.125), [], [comb])
        if moe and MOE_STAGE >= 1:
            k.op("dve", lambda h: h.tensor_reduce(out=m1.t[:], in_=lg.t[:], axis=mybir.AxisListType.X, op=ALU.max),
                 [lg], [m1])
            for sub in range(8):
                ts(k, "dve", k1.t[:, sub, :], lg.t[:, sub, :], m1.t[:, sub:sub + 1], None, ALU.is_equal, None,
                   [lg, m1], [k1])
            stt(k, l2.t[:], k1.t[:], -1e30, lg.t[:], ALU.mult, ALU.add, [k1, lg], [l2])
            k.op("dve", lambda h: h.tensor_reduce(out=m2.t[:], in_=l2.t[:], axis=mybir.AxisListType.X, op=ALU.max),
                 [l2], [m2])
            for sub in range(8):
                ts(k, "dve", k2.t[:, sub, :], l2.t[:, sub, :], m2.t[:, sub:sub + 1], None, ALU.is_equal, None,
                   [l2, m2], [k2])
            tt(k, "dve", w2.t[:], m2.t[:], m1.t[:], ALU.subtract, [m1, m2], [w2])
            act(k, w2.t[:], w2.t[:], AF.Exp, [w2], [w2])
            ts(k, "dve", w1.t[:], w2.t[:], 1.0, None, ALU.add, None, [w2], [w1])
            k.op("dve", lambda h: h.reciprocal(out=w1.t[:], in_=w1.t[:]), [w1], [w1])
            tt(k, "dve", w2.t[:], w2.t[:], w1.t[:], ALU.mult, [w2, w1], [w2])
            for sub in range(8):
                ts(k, "dve", k1.t[:, sub, :], k1.t[:, sub, :], w1.t[:, sub:sub + 1], None, ALU.mult, None,
                   [k1, w1], [k1])
                stt(k, comb.t[:, sub, :], k2.t[:, sub, :], w2.t[:, sub:sub + 1], k1.t[:, sub, :], ALU.mult, ALU.add,
                    [k2, w2, k1], [comb])
        first_acc = [True] * 16

        def do_gu(si):
            nonlocal wg_i
            e, cb, n = slabs[si]
            hsl = hs[si % 2]
            gsrc = c.wb["moe_gu%d" % (j * NEXP + e)] if moe else gu_src
            for f in range(n):
                wg = wgu[wg_i % 4]
                wg_i += 1
                k.dma(wg.t[:].rearrange("p k n -> p (k n)"), gsrc[cb + f, :, :], [], [wg], wg)
                for tk in range(2):
                    bg = k.bank()
                    for kc in range(KC):
                        mm(k, bg.t[:, :], wg.t[:, kc, 0:128], hT.t[:, kc, tk * 512:(tk + 1) * 512], kc == 0,
                           kc == KC - 1, [wg, hT], [bg])
                    bu = k.bank()
                    for kc in range(KC):
                        mm(k, bu.t[:, :], wg.t[:, kc, 128:256], hT.t[:, kc, tk * 512:(tk + 1) * 512], kc == 0,
                           kc == KC - 1, [wg, hT], [bu])
                    s_ = sg[(f * 2 + tk) % 4]
                    act(k, s_.t[:], bg.t[:, :], AF.Silu, [bg], [s_])
                    tt(k, "dve", hsl.t[:, f, tk * 512:(tk + 1) * 512], s_.t[:], bu.t[:, :], ALU.mult, [s_, bu], [hsl])

        def pre_dn(si):
            e, cb, n = slabs[si]
            wd = wdn[si % 2]
            dsrc = c.wb["moe_dn%d" % (j * NEXP + e)] if moe else dn_src
            k.dma(wd.t[:, 0:n, :], dsrc[cb:cb + n, :, :].rearrange("n p f -> p n f"), [], [wd], wd)

        def do_dn(si):
            nonlocal evn
            e, cb, n = slabs[si]
            hsl = hs[si % 2]
            wd = wdn[si % 2]
            for sub in range(8):
                for half in range(2):
                    bk = k.bank()
                    for f in range(n):
                        mm(k, bk.t[:, :], hsl.t[:, f, sub * 128:(sub + 1) * 128], wd.t[:, f, half * 512:(half + 1) * 512],
                           f == 0, f == n - 1, [hsl, wd], [bk])
                    av = acc.t[:, sub, half * 512:(half + 1) * 512]
                    bi = sub * 2 + half
                    if moe:
                        cs = comb.t[:, sub, e:e + 1]
                        if first_acc[bi]:
                            ts(k, "dve", av, bk.t[:, :], cs, None, ALU.mult, None, [bk, comb], [acc])
                        elif evn % 2 == 0:
                            stt(k, av, bk.t[:, :], cs, av, ALU.mult, ALU.add, [bk, comb, acc], [acc])
                        else:
                            tm_ = tmo[(evn // 2) % 2]
                            act(k, tm_.t[:], bk.t[:, :], AF.Identity, [bk, comb], [tm_], scale=cs)
                            tt(k, "pool", av, av, tm_.t[:], ALU.add, [acc, tm_], [acc])
                    else:
                        if first_acc[bi]:
                            cp(k, "act", av, bk.t[:, :], [bk], [acc])
                        elif evn % 2 == 0:
                            tt(k, "dve", av, bk.t[:, :], av, ALU.add, [bk, acc], [acc])
                        else:
                            tm_ = tmo[(evn // 2) % 2]
                            cp(k, "act", tm_.t[:], bk.t[:, :], [bk], [tm_])
                            tt(k, "pool", av, av, tm_.t[:], ALU.add, [acc, tm_], [acc])
                    first_acc[bi] = False
                    evn += 1

        do_gu(0)
        nmid = max(0, len(slabs) - 3)
        for si in range(len(slabs)):
            pre_dn(si)
            if si + 1 < len(slabs):
                do_gu(si + 1)
            if si == nmid and q + 1 < nst:
                normT(q + 1)
            do_dn(si)
        for pr in range(4):
            xi = xin[pr % 2]
            r0 = PAD + t0 + pr * 256
            k.dma(xi.t[:], X[r0:r0 + 256, :].rearrange("(s p) f -> p s f", p=128), [], [xi], xi)
            for u in range(2):
                sub = pr * 2 + u
                tt(k, "dve", acc.t[:, sub, :], acc.t[:, sub, :], gb_.t[:], ALU.mult, [acc, gb_], [acc])
                tt(k, "pool", xi.t[:, u, :], xi.t[:, u, :], acc.t[:, sub, :], ALU.add, [xi, acc], [xi])
            k.dma(X[r0:r0 + 256, :].rearrange("(s p) f -> p s f", p=128), xi.t[:], [xi], [], xi)
    k.end()


def phase_attn(c, li, j, gi, cur):
    k = c.k
    T = c.T
    NS = c.NSLOT
    Xr = c.X[cur]
    Xw = c.X[1 - cur]
    win, d = GROUPS[gi]
    L = SLOT // d
    NT = 512 if L >= 512 else L
    nQB = NT // 128
    nKB = nQB + 1
    W = NT + 128
    ntr = L // NT
    last = (gi == 2)
    k.begin()
    wq = k.sb("wq", [128, KC, 1536], BF16)
    k.dma(wq.t[:].rearrange("p k n -> p (k n)"), c.wb["at_qkv"][j * 3 + gi, :, :], [], [wq], wq)
    qkn = k.sb("qkn", [128, 2, 2], F32)
    k.dma(qkn.t[:], c.at_qkn[:, :, :], [], [qkn], qkn)
    gq = k.sb("gq", [128, 2], F32)
    cp(k, "dve", gq.t[:], qkn.t[:, j, :], [qkn], [gq])
    ts(k, "dve", gq.t[:, 0:1], gq.t[:, 0:1], 0.125, None, ALU.mult, None, [gq], [gq])
    bones = k.sb("bones", [128, 128], F32)
    k.dma(bones.t[:], c.bones[:, :], [], [bones], bones)
    pswap = k.sb("pswap", [128, 128], F32)
    k.dma(pswap.t[:], c.pswap[:, :], [], [pswap], pswap)
    bb32 = k.sb("bb32", [128, 256], F32)
    k.dma(bb32.t[:], c.bandb[:, :], [], [bb32], bb32)
    bb = k.sb("bb", [128, 256], BF16)
    cp(k, "dve", bb.t[:], bb32.t[:], [bb32], [bb])
    idb = k.sb("idb", [128, 128], BF16)
    cp(k, "dve", idb.t[:], c.s_ident.t[:], [c.s_ident], [idb])
    xins = [k.sb("xin%d" % i, [128, nKB, D], F32) for i in range(2)]
    junk = k.sb("junk", [128, D], F32)
    sm = [k.sb("sm%d" % q, [128, 8], F32) for q in range(3)]
    hT = k.sb("hT", [128, KC, W], BF16)
    ctabs = [k.sb("ctab%d" % i, [128, W], F32) for i in range(2)]
    stabs = [k.sb("stab%d" % i, [128, W], F32) for i in range(2)]
    QT = k.sb("QT", [128, 4, NT], BF16)
    KT = k.sb("KT", [128, 4, W], BF16)
    qt_ = [[k.sb("qt%d_%d" % (i, q), [128, 512], F32) for q in range(4)] for i in range(2)]
    VE = k.sb("VE", [128, nKB, 8, 66], BF16)
    PT = k.sb("PT", [128, nKB, 8, 256], BF16)
    osb = [k.sb("osb%d" % i, [128, 528], F32) for i in range(2)]
    oac = [k.sb("oac%d" % i, [128, 528], F32) for i in range(2)]
    if last:
        wo = k.sb("wo", [128, 4, D], BF16)
        k.dma(wo.t[:].rearrange("p k n -> p (k n)"), c.wb["at_wo"][j, :, :], [], [wo], wo)
        rden = k.sb("rden", [128, 8], F32)
        on = k.sb("on", [128, 512], F32)
        oT = k.sb("oT", [128, 4, 128], BF16)
        xres = [k.sb("xres%d" % i, [128, D], F32) for i in range(2)]
        g1 = [k.sb("g1_%d" % i, [128, D], F32) for i in range(2)]
        tmo = [k.sb("tmo%d" % i, [128, 512], F32) for i in range(2)]
    gcur = None
    un = 0
    on_ = 0
    tiles = [(slot, r, w_) for slot in range(NS) for r in range(d) for w_ in range(ntr)]
    if ATTN_MAXTILES is not None:
        tiles = tiles[:ATTN_MAXTILES]
    tidx = 0

    def load_tile(ti):
        slot, r, w_ = tiles[ti]
        j0 = w_ * NT
        xin = xins[ti % 2]
        row0 = PAD + slot * SLOT + r + d * (j0 - 64)
        k.dma(xin.t[:], Xr[row0:row0 + d * (W - 1) + 1:d, :].rearrange("(b p) f -> p b f", p=128), [], [xin], xin)
        tcol = 64 + r * (NS * L) + slot * L + j0 - 64
        k.dma(ctabs[ti % 2].t[:], c.rope[gi, 0, :, tcol:tcol + W], [], [ctabs[ti % 2]], ctabs[ti % 2])
        k.dma(stabs[ti % 2].t[:], c.rope[gi, 1, :, tcol:tcol + W], [], [stabs[ti % 2]], stabs[ti % 2])

    for slot in range(NS):
        if last:
            gb_ = g1[slot % 2]
            k.dma(gb_.t[:], c.GREP[li, 0, slot, :, :], [], [gb_], gb_)
        for r in range(d):
            for w_ in range(ntr):
                if tidx >= len(tiles):
                    continue
                j0 = w_ * NT
                if tidx == 0:
                    load_tile(0)
                if tidx + 1 < len(tiles):
                    load_tile(tidx + 1)
                xin = xins[tidx % 2]
                ctab = ctabs[tidx % 2]
                stab = stabs[tidx % 2]
                tidx += 1
                norm_block(c, xin, nKB, sm[0], sm[1], sm[2], junk)
                transpose_block(c, xin, nKB, hT, 0, li, 0, slot)
                units = []
                for ch in range(4):
                    units.append((0, ch, 64, NT, QT, 0))
                for ch in range(4):
                    c0 = 0
                    while c0 < W:
                        n = min(512, W - c0)
                        units.append((1, ch, c0, n, KT, c0))
                        c0 += n
                def part1(ui):
                    (isk, ch, h0, n, dst, d0) = units[ui]
                    qg, sq, rs, t1 = qt_[(un + ui) % 2]
                    bk = k.bank()
                    wc = isk * 512 + ch * 128
                    for kc in range(KC):
                        mm(k, bk.t[:, 0:n], wq.t[:, kc, wc:wc + 128], hT.t[:, kc, h0:h0 + n], kc == 0, kc == KC - 1,
                           [wq, hT], [bk])
                    act(k, qg.t[:, 0:n], bk.t[:, 0:n], AF.Identity, [bk, gq], [qg], scale=gq.t[:, isk:isk + 1])
                    act(k, sq.t[:, 0:n], bk.t[:, 0:n], AF.Square, [bk], [sq])

                part1(0)
                for ui, (isk, ch, h0, n, dst, d0) in enumerate(units):
                    qg, sq, rs, t1 = qt_[(un + ui) % 2]
                    if ui + 1 < len(units):
                        part1(ui + 1)
                    b2 = k.bank()
                    mm(k, b2.t[:, 0:n], bones.t[:, :], sq.t[:, 0:n], True, True, [bones, sq], [b2])
                    b3 = k.bank()
                    mm(k, b3.t[:, 0:n], pswap.t[:, :], qg.t[:, 0:n], True, True, [pswap, qg], [b3])
                    act(k, rs.t[:, 0:n], b2.t[:, 0:n], AF.Ln, [b2], [rs], scale=1.0 / 64, bias=EPS)
                    act(k, rs.t[:, 0:n], rs.t[:, 0:n], AF.Exp, [rs], [rs], scale=-0.5)
                    tt(k, "dve", t1.t[:, 0:n], qg.t[:, 0:n], ctab.t[:, h0:h0 + n], ALU.mult, [qg, ctab], [t1])
                    tt(k, "dve", sq.t[:, 0:n], b3.t[:, 0:n], stab.t[:, h0:h0 + n], ALU.mult, [b3, stab], [sq])
                    tt(k, "dve", t1.t[:, 0:n], t1.t[:, 0:n], sq.t[:, 0:n], ALU.add, [t1, sq], [t1])
                    tt(k, "dve", dst.t[:, ch, d0:d0 + n], t1.t[:, 0:n], rs.t[:, 0:n], ALU.mult, [t1, rs], [dst])
                un += len(units)
                if ATTN_STAGE < 1:
                    continue
                k.op("pool", lambda h: h.memset(VE.t[:, :, :, 64:66], 1.0), [], [VE])
                for b in range(nKB):
                    bk = k.bank()
                    for kc in range(KC):
                        mm(k, bk.t[:, :], hT.t[:, kc, b * 128:(b + 1) * 128], wq.t[:, kc, 1024:1536], kc == 0,
                           kc == KC - 1, [hT, wq], [bk])
                    cp(k, "act", VE.t[:, b, :, 0:64], bk.t[:, :].rearrange("p (h e) -> p h e", e=64), [bk], [VE])
                if w_ == 0:
                    fc = 1 if slot == 0 else 0
                    ts(k, "pool", VE.t[0:64, 0, :, :], VE.t[0:64, 0, :, :], c.s_flags.t[0:64, fc:fc + 1], None,
                       ALU.mult, None, [VE, c.s_flags], [VE])
                if w_ == ntr - 1:
                    fc = 1 if slot == NS - 1 else 0
                    ts(k, "pool", VE.t[64:128, nKB - 1, :, :], VE.t[64:128, nKB - 1, :, :],
                       c.s_flags.t[64:128, fc:fc + 1], None, ALU.mult, None, [VE, c.s_flags], [VE])
                if ATTN_STAGE < 2:
                    continue
                for b in range(nKB):
                    qlo = max(b - 1, 0)
                    qhi = min(b + 1, nQB)
                    wd_ = (qhi - qlo) * 128
                    if b == 0:
                        mb = bb.t[:, 128:256]
                    elif b == nQB:
                        mb = bb.t[:, 0:128]
                    else:
                        mb = bb.t[:, 0:256]
                    for hp in range(4):
                        bk = k.bank()
                        for hh in range(2):
                            pb = 64 * hh
                            o = bk.t[:, hh * 256:hh * 256 + wd_]
                            mm(k, o, idb.t[:, :], mb, True, False, [idb, bb], [bk], inc=False)
                            mm(k, o, KT.t[pb:pb + 64, hp, b * 128:(b + 1) * 128],
                               QT.t[pb:pb + 64, hp, qlo * 128:qhi * 128], False, True, [KT, QT], [bk])
                        src = bk.t[:, :].rearrange("p (h q) -> p h q", q=256)[:, :, 0:wd_]
                        act(k, PT.t[:, b, 2 * hp:2 * hp + 2, 0:wd_], src, AF.Exp, [bk], [PT])
                if ATTN_STAGE < 3:
                    continue
                for qb in range(nQB):
                    oa = k.bank()
                    ob = k.bank()
                    for h_ in range(8):
                        bk = oa if h_ < 4 else ob
                        o = bk.t[:, (h_ % 4) * 66:(h_ % 4) * 66 + 66]
                        for b in (qb, qb + 1):
                            off = 0 if (b == qb + 1 or b == 0) else 128
                            mm(k, o, PT.t[:, b, h_, off:off + 128], VE.t[:, b, h_, :], b == qb, b == qb + 1,
                               [PT, VE], [bk], inc=(b == qb + 1 and h_ % 4 == 3))
                    os_ = osb[on_ % 2]
                    oc_ = oac[on_ % 2]
                    cp(k, "act", os_.t[:, 0:264], oa.t[:, 0:264], [oa], [os_])
                    cp(k, "dve", os_.t[:, 264:528], ob.t[:, 0:264], [ob], [os_])
                    qrow = PAD + slot * SLOT + r + d * (j0 + qb * 128)
                    orows = c.OACC[qrow:qrow + d * 127 + 1:d, :]
                    if gi == 0:
                        k.dma(orows, os_.t[:], [os_], [], os_)
                    else:
                        k.dma(oc_.t[:], orows, [], [oc_], oc_)
                        tt(k, "dve", os_.t[:], os_.t[:], oc_.t[:], ALU.add, [os_, oc_], [os_])
                        if not last:
                            k.dma(orows, os_.t[:], [os_], [], os_)
                    if last:
                        xr = xres[on_ % 2]
                        k.dma(xr.t[:], Xr[qrow:qrow + d * 127 + 1:d, :], [], [xr], xr)
                        ov = os_.t[:].rearrange("p (h e) -> p h e", e=66)
                        k.op("dve", lambda h: h.reciprocal(out=rden.t[:], in_=ov[:, :, 64]), [os_], [rden])
                        for h_ in range(8):
                            ts(k, "dve", on.t[:, h_ * 64:(h_ + 1) * 64], ov[:, h_, 0:64],
                               rden.t[:, h_:h_ + 1], None, ALU.mult, None, [os_, rden], [on])
                        bk = k.bank()
                        for kc in range(4):
                            k.op("pe", lambda h, kc=kc: h.transpose(out=bk.t[:, kc * 128:(kc + 1) * 128],
                                                                     in_=on.t[:, kc * 128:(kc + 1) * 128],
                                                                     identity=c.s_ident.t[:]),
                                 [on, c.s_ident], [bk], inc=(kc == 3))
                        cp(k, "act", oT.t[:].rearrange("p k t -> p (k t)"), bk.t[:, :], [bk], [oT])
                        for half in range(2):
                            bk = k.bank()
                            for kc in range(4):
                                mm(k, bk.t[:, :], oT.t[:, kc, :], wo.t[:, kc, half * 512:(half + 1) * 512], kc == 0,
                                   kc == 3, [oT, wo], [bk])
                            tm_ = tmo[half]
                            tt(k, "dve", tm_.t[:], bk.t[:, :], gb_.t[:, half * 512:(half + 1) * 512], ALU.mult,
                               [bk, gb_], [tm_])
                            tt(k, "pool", xr.t[:, half * 512:(half + 1) * 512], xr.t[:, half * 512:(half + 1) * 512],
                               tm_.t[:], ALU.add, [xr, tm_], [xr])
                        k.dma(Xw[qrow:qrow + d * 127 + 1:d, :], xr.t[:], [xr], [], xr)
                    on_ += 1
    k.end()


def phase_out(c, cur):
    k = c.k
    k.begin()
    z = k.sb("zo", [128, 8], F32)
    T = c.T
    for r0 in range(0, T, 512):
        k.dma(c.y[r0:r0 + 512, :], c.X[cur][PAD + r0:PAD + r0 + 512, :], [], [], z)
    k.end()


def _pm(w, kc):
    n = w.shape[-1]
    return np.ascontiguousarray(w.reshape(kc, 128, n).transpose(1, 0, 2)).reshape(128, kc * n)


def prep_weights(inp):
    f = np.float32
    W = {}
    ada_w = np.asarray(inp["ada_w"], f)
    W["ada_w"] = np.ascontiguousarray(ada_w.reshape(4, KC, 128, 6 * D).transpose(0, 2, 1, 3))
    ada_b = np.asarray(inp["ada_b"], f)
    W["ada_bT"] = np.ascontiguousarray(ada_b.reshape(4, 48, 128).transpose(2, 0, 1))
    brep = np.stack([ada_b[:, 2 * D:3 * D], ada_b[:, 5 * D:6 * D]], axis=1)
    W["ada_brep"] = np.ascontiguousarray(np.broadcast_to(brep[:, :, None, :], (4, 2, 128, D)))
    nm = np.stack([np.asarray(inp["norm_mix"], f), np.asarray(inp["norm_ffn"], f)], axis=1)
    W["nrm"] = np.ascontiguousarray(nm.reshape(4, 2, KC, 128).transpose(3, 0, 1, 2))
    cw = np.asarray(inp["rg_conv_w"], f)
    cb = np.asarray(inp["rg_conv_b"], f)
    cc = np.concatenate([cw, cb[:, None, :]], axis=1)
    W["rg_conv"] = np.ascontiguousarray(cc.reshape(2, 5, KC, 128).transpose(3, 0, 2, 1))
    gb = np.asarray(inp["rg_gate_b"], f)
    W["rg_gb"] = np.ascontiguousarray(gb.reshape(2, 2, 2, KC, 128).transpose(4, 0, 1, 2, 3))
    lam = np.asarray(inp["rg_lambda"], f)
    W["rg_lam"] = np.ascontiguousarray(lam.reshape(2, 2, KC, 128).transpose(3, 0, 1, 2))
    qn = np.asarray(inp["at_q_norm"], f)
    kn = np.asarray(inp["at_k_norm"], f)
    qk = np.stack([qn, kn], axis=2)
    W["at_qkn"] = np.ascontiguousarray(np.concatenate([qk, qk], axis=1).transpose(1, 0, 2))
    rt = np.asarray(inp["moe_router"], f)
    W["moe_rt"] = np.ascontiguousarray(rt.reshape(2, KC, 128, NEXP).transpose(2, 0, 1, 3))
    W["rg_w_in"] = np.stack([_pm(np.asarray(inp["rg_w_in"][j], f), KC) for j in range(2)])
    gw = np.asarray(inp["rg_gate_w"], f)
    W["rg_gate_w"] = np.ascontiguousarray(
        gw.reshape(2, 2, 2, 4, 2, 128, 256).transpose(0, 5, 1, 2, 3, 4, 6)).reshape(2, 128, 8192)
    W["rg_w_out"] = np.stack([_pm(np.asarray(inp["rg_w_out"][j], f), KC) for j in range(2)])
    qkv = np.asarray(inp["at_w_qkv"], f).reshape(2, D, 3, 1536)
    W["at_qkv"] = np.stack([_pm(np.ascontiguousarray(qkv[j, :, g, :]), KC) for j in range(2) for g in range(3)])
    W["at_wo"] = np.stack([_pm(np.asarray(inp["at_w_o"][j], f), 4) for j in range(2)])

    def gu_blocks(w, nff):
        dff = nff * 128
        g = w[:, :dff].reshape(KC, 128, nff, 128)
        u = w[:, dff:].reshape(KC, 128, nff, 128)
        gu_ = np.stack([g, u], axis=3)
        return np.ascontiguousarray(gu_.transpose(2, 1, 0, 3, 4)).reshape(nff, 128, KC * 256)

    W["ff_gu"] = np.concatenate([gu_blocks(np.asarray(inp["ff_w_gu"][j], f), 22) for j in range(2)])
    W["ff_dn"] = np.asarray(inp["ff_w_down"], f).reshape(2 * 22, 128, D)
    mg = np.asarray(inp["moe_w_gu"], f)
    md = np.asarray(inp["moe_w_down"], f)
    for j in range(2):
        for e in range(NEXP):
            W["moe_gu%d" % (j * NEXP + e)] = gu_blocks(mg[j, e], 28)
            W["moe_dn%d" % (j * NEXP + e)] = np.ascontiguousarray(md[j, e]).reshape(28, 128, D)
    W["ident"] = np.eye(128, dtype=f)
    bo = np.zeros((128, 128), f)
    bo[:64, :64] = 1
    bo[64:, 64:] = 1
    W["bones"] = bo
    ps = np.zeros((128, 128), f)
    for hb in (0, 64):
        for i in range(8):
            ps[hb + 8 + i, hb + i] = 1.0
            ps[hb + i, hb + 8 + i] = 1.0
    W["pswap"] = ps
    kk = np.arange(128)[:, None]
    cc_ = np.arange(256)[None, :]
    W["bandb"] = np.where((cc_ - kk >= 0) & (cc_ - kk <= 128), 0.0, -30000.0).astype(f)
    return W


def rope_tables(NSLOT, chained):
    f = np.float32
    T = NSLOT * SLOT
    inv = (500000.0 ** (-np.arange(0, 16, 2, dtype=np.float32) / 16)).astype(f)
    out = np.zeros((3, 2, 128, T + 128), f)
    out[:, 0] = 1.0
    dd = np.arange(128) % 64
    for gi, (_, d) in enumerate(GROUPS):
        L = SLOT // d
        r = np.arange(d)[:, None, None]
        s = np.arange(NSLOT)[None, :, None]
        jx = np.arange(L)[None, None, :]
        pos = (r + d * jx + (s * SLOT if chained else 0 * s)).astype(f).reshape(-1)
        ang = pos[None, :] * inv[:, None]
        cs = np.cos(ang).astype(f)
        sn = np.sin(ang).astype(f)
        for p in range(128):
            dq = dd[p]
            if dq < 8:
                out[gi, 0, p, 64:64 + T] = cs[dq]
                out[gi, 1, p, 64:64 + T] = -sn[dq]
            elif dq < 16:
                out[gi, 0, p, 64:64 + T] = cs[dq - 8]
                out[gi, 1, p, 64:64 + T] = sn[dq - 8]
    return out


def core_inputs(W, x, cvec, chained, NSLOT):
    f = np.float32
    m = dict(W)
    m["x_in"] = np.ascontiguousarray(x, dtype=f)
    m["cT"] = np.ascontiguousarray(np.asarray(cvec, f).reshape(NSLOT, KC, 128).transpose(2, 1, 0))
    fl = np.zeros((128, 2), f)
    fl[:, 0] = 1.0 if chained else 0.0
    m["flags"] = fl
    m["rope"] = rope_tables(NSLOT, chained)
    return m


_NC_CACHE = {}


def kernel(**inputs):
    NSLOT = 4
    W = prep_weights(inputs)
    xp = np.asarray(inputs["x_prompt"], np.float32)
    xs = np.asarray(inputs["x_sample"], np.float32)
    cp_ = np.asarray(inputs["c_prompt"], np.float32)
    cs = np.asarray(inputs["c_sample"], np.float32)
    in_maps = []
    in_maps.append(core_inputs(W, xp[0], np.repeat(cp_, NSLOT, axis=0), True, NSLOT))
    for ci in range(4):
        in_maps.append(core_inputs(W, xs[4 * ci:4 * ci + 4].reshape(NSLOT * SLOT, D), cs[4 * ci:4 * ci + 4], False, NSLOT))
    for ci in range(3):
        in_maps.append(in_maps[1 + ci])
    if "nc" not in _NC_CACHE:
        _NC_CACHE["nc"] = build(NSLOT)
    nc = _NC_CACHE["nc"]
    res = run_bass_kernel_spmd(nc, in_maps, core_ids=list(range(8)))
    yp = res.results[0]["y"].reshape(1, 4 * SLOT, D).astype(np.float32)
    ys = np.concatenate([res.results[1 + ci]["y"].reshape(4, SLOT, D) for ci in range(4)], axis=0).astype(np.float32)
    return (yp, ys)
```

```python
import numpy as np
from contextlib import ExitStack
import concourse.bass as bass
import concourse.mybir as mybir
from concourse.bass_utils import run_bass_kernel_spmd

F32 = mybir.dt.float32
BF16 = mybir.dt.bfloat16
AF = mybir.ActivationFunctionType
ALU = mybir.AluOpType

D = 1024
KC = 8
SLOT = 4096
PAD = 1024
DFF = 2816
DEX = 3584
NEXP = 8
GROUPS = ((128, 1), (512, 4), (2048, 16))
EPS = 1e-6
GEL_C = 1.5957691216057308
SKIP_ATTN = False
SKIP_FFN = False
ATTN_MAXTILES = None
ATTN_STAGE = 9
SKIP_CAST = False
FFN_MAXST = None
MOE_STAGE = 9
SAME_SYNC = True
ATTN_GROUPS = (0, 1, 2)


class Buf:
    __slots__ = ("name", "t", "w", "r", "ds")

    def __init__(self, name, t):
        self.name = name
        self.t = t
        self.w = None
        self.r = {}
        self.ds = None


class Eng:
    def __init__(self, key, h, sem):
        self.key = key
        self.h = h
        self.sem = sem
        self.n = 0
        self.known = {}


class K:
    def __init__(self, nc, es, ndsem=70):
        self.nc = nc
        self.es = es
        self.eng = {}
        self.sems = {}
        self.total = {}
        for key, h in (("pe", nc.tensor), ("act", nc.scalar), ("dve", nc.vector),
                       ("pool", nc.gpsimd), ("sp", nc.sync)):
            sem = es.enter_context(nc.semaphore("s_" + key))
            self.eng[key] = Eng(key, h, sem)
            self.sems[key] = sem
            self.total[key] = 0
        self.free_ds = []
        for i in range(ndsem):
            k = "d%d" % i
            self.sems[k] = es.enter_context(nc.semaphore("sd%d" % i))
            self.total[k] = 0
            self.free_ds.append(k)
        self.pstack = None
        self.pbufs = []
        self.ps = []
        for i in range(8):
            t = es.enter_context(nc.psum_tensor("psb%d" % i, [128, 512], F32))
            self.ps.append(Buf("psb%d" % i, t))
        self.psi = 0
        self.uid = 0

    def gsb(self, name, shape, dt):
        t = self.es.enter_context(self.nc.sbuf_tensor("g_" + name, list(shape), dt))
        return Buf(name, t)

    def sb(self, name, shape, dt):
        self.uid += 1
        t = self.pstack.enter_context(self.nc.sbuf_tensor("p_%s_%d" % (name, self.uid), list(shape), dt))
        b = Buf(name, t)
        self.pbufs.append(b)
        return b

    def bank(self):
        b = self.ps[self.psi]
        self.psi = (self.psi + 1) % 8
        return b

    def begin(self):
        self.pstack = ExitStack()
        self.pbufs = []

    def end(self):
        self.barrier()
        for b in self.pbufs:
            if b.ds is not None:
                self.free_ds.append(b.ds)
                b.ds = None
        self.pstack.close()
        self.pstack = None
        self.pbufs = []

    def _waits(self, E, rd, wr):
        need = {}
        for b in rd:
            if b.w is not None:
                k, v = b.w
                if need.get(k, 0) < v:
                    need[k] = v
        for b in wr:
            if b.w is not None:
                k, v = b.w
                if need.get(k, 0) < v:
                    need[k] = v
            for k, v in b.r.items():
                if need.get(k, 0) < v:
                    need[k] = v
        for k, v in need.items():
            if k == E.key and (k == "pe" or not SAME_SYNC):
                continue
            if E.known.get(k, 0) >= v:
                continue
            E.h.wait_ge(self.sems[k], v)
            E.known[k] = v

    def op(self, e, fn, rd=(), wr=(), inc=True):
        E = self.eng[e]
        self._waits(E, rd, wr)
        ins = fn(E.h)
        if inc:
            E.n += 1
            self.total[E.key] = E.n
            ins.then_inc(E.sem, 1)
            tok = (E.key, E.n)
        else:
            tok = (E.key, E.n + 1)
        for b in rd:
            if b.r.get(tok[0], 0) < tok[1]:
                b.r[tok[0]] = tok[1]
        for b in wr:
            b.w = tok
            b.r = {}
        return ins

    def dma(self, out, in_, rd, wr, owner, q="sp"):
        E = self.eng[q]
        self._waits(E, rd, wr)
        if owner.ds is None:
            owner.ds = self.free_ds.pop()
        k = owner.ds
        ins = E.h.dma_start(out=out, in_=in_)
        self.total[k] += 16
        ins.then_inc(self.sems[k], 16)
        tok = (k, self.total[k])
        for b in rd:
            if b.r.get(k, 0) < tok[1]:
                b.r[k] = tok[1]
        for b in wr:
            b.w = tok
            b.r = {}

    def barrier(self):
        for E in self.eng.values():
            for k, v in self.total.items():
                if v > E.known.get(k, 0):
                    if k == E.key and k == "pe":
                        continue
                    E.h.wait_ge(self.sems[k], v)
                    E.known[k] = v


def act(k, out, in_, func, rd, wr, **kw):
    return k.op("act", lambda h: h.activation(out=out, in_=in_, func=func, **kw), rd, wr)


def ts(k, e, out, in0, s1, s2, op0, op1, rd, wr):
    if s2 is None:
        return k.op(e, lambda h: h.tensor_scalar(out=out, in0=in0, scalar1=s1, scalar2=None, op0=op0), rd, wr)
    return k.op(e, lambda h: h.tensor_scalar(out=out, in0=in0, scalar1=s1, scalar2=s2, op0=op0, op1=op1), rd, wr)


def tt(k, e, out, in0, in1, op, rd, wr):
    return k.op(e, lambda h: h.tensor_tensor(out=out, in0=in0, in1=in1, op=op), rd, wr)


def stt(k, out, in0, scalar, in1, op0, op1, rd, wr):
    return k.op("dve", lambda h: h.scalar_tensor_tensor(out=out, in0=in0, scalar=scalar, in1=in1,
                                                        op0=op0, op1=op1), rd, wr)


def cp(k, e, out, in_, rd, wr):
    if e == "act":
        return act(k, out, in_, AF.Copy, rd, wr)
    return k.op(e, lambda h: h.tensor_copy(out=out, in_=in_), rd, wr)


def mm(k, out, lhsT, rhs, start, stop, rd, wr, inc=None):
    if inc is None:
        inc = stop
    return k.op("pe", lambda h: h.matmul(out, lhsT=lhsT, rhs=rhs, start=start, stop=stop), rd, wr, inc=inc)


class Ctx:
    pass


def build(NSLOT, layers=(0, 1, 2, 3), dbg=None):
    T = NSLOT * SLOT
    TP = T + 2 * PAD
    nc = bass.Bass("TRN2", target_bir_lowering=False)
    c = Ctx()
    c.nc = nc
    c.NSLOT = NSLOT
    c.T = T

    def din(name, shape, dt=F32):
        return nc.dram_tensor(name, list(shape), dt, kind="ExternalInput").ap()

    def dint(name, shape, dt):
        return nc.dram_tensor(name, list(shape), dt, kind="Internal").ap()

    c.x_in = din("x_in", [T, D])
    c.cT = din("cT", [128, KC, NSLOT])
    c.flags = din("flags", [128, 2])
    c.ident = din("ident", [128, 128])
    c.bones = din("bones", [128, 128])
    c.pswap = din("pswap", [128, 128])
    c.bandb = din("bandb", [128, 256])
    c.rope = din("rope", [3, 2, 128, T + 128])
    c.ada_w = din("ada_w", [4, 128, KC, 6 * D])
    c.ada_bT = din("ada_bT", [128, 4, 48])
    c.ada_brep = din("ada_brep", [4, 2, 128, D])
    c.nrm = din("nrm", [128, 4, 2, KC])
    c.rg_conv = din("rg_conv", [128, 2, KC, 5])
    c.rg_gb = din("rg_gb", [128, 2, 2, 2, KC])
    c.rg_lam = din("rg_lam", [128, 2, 2, KC])
    c.at_qkn = din("at_qkn", [128, 2, 2])
    c.moe_rt = din("moe_rt", [128, 2, KC, NEXP])
    wspec = {
        "rg_w_in": [2, 128, KC * 2048],
        "rg_gate_w": [2, 128, 8192],
        "rg_w_out": [2, 128, KC * D],
        "at_qkv": [6, 128, KC * 1536],
        "at_wo": [2, 128, 4 * D],
        "ff_gu": [2 * 22, 128, KC * 256],
        "ff_dn": [2 * 22, 128, D],
    }
    for i in range(2 * NEXP):
        wspec["moe_gu%d" % i] = [28, 128, KC * 256]
        wspec["moe_dn%d" % i] = [28, 128, D]
    c.wf = {}
    c.wb = {}
    for n, shp in wspec.items():
        c.wf[n] = din(n, shp)
        c.wb[n] = dint(n + "_b", shp, BF16)
    c.y = nc.dram_tensor("y", [T, D], F32, kind="ExternalOutput").ap()
    c.X = [dint("XA", [TP, D], F32), dint("XB", [TP, D], F32)]
    c.XBs = dint("XBs", [KC, 128, T + 8], F32)
    c.GYs = dint("GYs", [KC, 128, T], BF16)
    c.XCs = dint("XCs", [KC, 128, T], F32)
    c.HFs = dint("HFs", [KC, 128, T], F32)
    c.OACC = dint("OACC", [TP, 528], F32)
    c.GREP = dint("GREP", [4, 2, NSLOT, 128, D], F32)
    if dbg is not None:
        c.dbg = nc.dram_tensor("dbg", list(dbg), F32, kind="ExternalOutput").ap()

    es = ExitStack()
    with es:
        k = K(nc, es)
        c.k = k
        c.s_ident = k.gsb("ident", [128, 128], F32)
        c.s_flags = k.gsb("flags", [128, 2], F32)
        c.s_mods = k.gsb("mods", [128, 4, 4, KC, NSLOT], F32)
        k.dma(c.s_ident.t[:], c.ident[:, :], [], [c.s_ident], c.s_ident)
        k.dma(c.s_flags.t[:], c.flags[:, :], [], [c.s_flags], c.s_flags)
        phase_init(c)
        if not SKIP_CAST:
            phase_cast(c, layers)
        phase_mods(c, layers)
        cur = 0
        wrote_y = False
        for li in layers:
            j = li // 2
            if li % 2 == 0:
                phase_r1(c, li, j, cur)
                phase_r2(c, li, j)
                phase_r3(c, li, j, cur)
                phase_ffn(c, li, j, cur, moe=False, final=(li == layers[-1] and FFN_MAXST is None))
                wrote_y = (li == layers[-1] and FFN_MAXST is None)
            else:
                if not SKIP_ATTN:
                    for gi in ATTN_GROUPS:
                        phase_attn(c, li, j, gi, cur)
                    cur = 1 - cur
                if not SKIP_FFN:
                    phase_ffn(c, li, j, cur, moe=True, final=(li == layers[-1] and FFN_MAXST is None))
                    wrote_y = (li == layers[-1] and FFN_MAXST is None)
        if not wrote_y:
            phase_out(c, cur)
    return nc


def phase_init(c):
    k = c.k
    k.begin()
    z = k.sb("z", [128, D], F32)
    k.op("pool", lambda h: h.memset(z.t[:], 0.0), [], [z])
    dX = [Buf("XA", None), Buf("XB", None)]
    T = c.T
    for xi in range(2):
        X = c.X[xi]
        for r0 in list(range(0, PAD, 128)) + list(range(PAD + T, PAD + T + PAD, 128)):
            k.dma(X[r0:r0 + 128, :], z.t[:], [z], [dX[xi]], z)
    for r0 in range(0, T, 512):
        k.dma(c.X[0][PAD + r0:PAD + r0 + 512, :], c.x_in[r0:r0 + 512, :], [], [dX[0]], z)
    k.end()


def phase_cast(c, layers):
    k = c.k
    k.begin()
    need = set()
    for li in layers:
        if li % 2 == 0:
            need |= {("rg_w_in", li // 2), ("rg_gate_w", li // 2), ("rg_w_out", li // 2),
                     ("ff_gu", li // 2), ("ff_dn", li // 2)}
        else:
            need |= {("at_qkv", li // 2), ("at_wo", li // 2)}
            if not SKIP_FFN:
                need |= {("moe_gu", li // 2), ("moe_dn", li // 2)}
    NB = 3
    st = [k.sb("cst%d" % i, [128, 4096], F32) for i in range(NB)]
    bf = [k.sb("cbf%d" % i, [128, 4096], BF16) for i in range(NB)]
    engs = ["pool", "dve", "act"]
    it = 0
    for n in c.wf:
        src = c.wf[n]
        dst = c.wb[n]
        nb, _, F = src.shape
        per = nb // 2
        ismoe = n.startswith("moe_")
        if ismoe:
            per = nb
        for jj in range(1 if ismoe else 2):
            if ismoe:
                if (n[:6], int(n[6:]) // NEXP) not in need:
                    continue
            elif (n, jj) not in need:
                continue
            if F >= 4096:
                units = [(b, f0, 1) for b in range(jj * per, (jj + 1) * per) for f0 in range(0, F, 4096)]
            else:
                g = 4096 // F
                units = []
                b = jj * per
                while b < (jj + 1) * per:
                    gg = min(g, (jj + 1) * per - b)
                    units.append((b, 0, gg))
                    b += gg
            for (b, f0, gg) in units:
                s = st[it % NB]
                d = bf[it % NB]
                e = engs[it % 3]
                it += 1
                if F >= 4096:
                    k.dma(s.t[:, :], src[b, :, f0:f0 + 4096], [], [s], s)
                    cp(k, e, d.t[:, :], s.t[:, :], [s], [d])
                    k.dma(dst[b, :, f0:f0 + 4096], d.t[:, :], [d], [], d)
                else:
                    sv = s.t[:, 0:gg * F].rearrange("p (n f) -> p n f", f=F)
                    dv = d.t[:, 0:gg * F].rearrange("p (n f) -> p n f", f=F)
                    k.dma(sv, src[b:b + gg, :, :].rearrange("n p f -> p n f"), [], [s], s)
                    cp(k, e, d.t[:, 0:gg * F], s.t[:, 0:gg * F], [s], [d])
                    k.dma(dst[b:b + gg, :, :].rearrange("n p f -> p n f"), dv, [d], [], d)
    k.end()


def phase_mods(c, layers):
    k = c.k
    NS = c.NSLOT
    k.begin()
    cT = k.sb("cT", [128, KC, NS], F32)
    sc = k.sb("sc", [128, KC, NS], F32)
    e1 = k.sb("e1", [128, KC, NS], F32)
    ones = k.sb("ones", [128, 128], F32)
    screp = k.sb("screp", [128, KC, NS, 128], F32)
    abT = k.sb("abT", [128, 4, 48], F32)
    nrm = k.sb("nrm", [128, 4, 2, KC], F32)
    tmp = k.sb("tmp", [128, KC, NS], F32)
    w = [k.sb("w%d" % i, [128, KC, D], F32) for i in range(2)]
    brep = [k.sb("brep%d" % i, [128, D], F32) for i in range(2)]
    go = [k.sb("go%d" % i, [128, D], F32) for i in range(2)]
    k.dma(cT.t[:], c.cT[:, :, :], [], [cT], cT)
    k.dma(abT.t[:], c.ada_bT[:, :, :], [], [abT], abT)
    k.dma(nrm.t[:], c.nrm[:, :, :, :], [], [nrm], nrm)
    k.op("pool", lambda h: h.memset(ones.t[:], 1.0), [], [ones])
    act(k, e1.t[:], cT.t[:], AF.Exp, [cT], [e1], scale=-1.0)
    ts(k, "dve", e1.t[:], e1.t[:], 1.0, None, ALU.add, None, [e1], [e1])
    k.op("dve", lambda h: h.reciprocal(out=e1.t[:], in_=e1.t[:]), [e1], [e1])
    tt(k, "dve", sc.t[:], cT.t[:], e1.t[:], ALU.mult, [cT, e1], [sc])
    for kc in range(KC):
        for s in range(NS):
            ts(k, "pool", screp.t[:, kc, s, :], ones.t[:], sc.t[:, kc, s:s + 1], None, ALU.mult, None,
               [ones, sc], [screp])
    wi = 0
    gi_ = 0
    for li in layers:
        for part in range(6):
            wt = w[wi % 2]
            wi += 1
            k.dma(wt.t[:], c.ada_w[li, :, :, part * D:(part + 1) * D], [], [wt], wt)
            if part in (2, 5):
                g = 0 if part == 2 else 1
                br = brep[gi_ % 2]
                k.dma(br.t[:], c.ada_brep[li, g, :, :], [], [br], br)
                for s in range(NS):
                    gt = go[gi_ % 2]
                    gi_ += 1
                    for half in range(2):
                        bk = k.bank()
                        for kc in range(KC):
                            mm(k, bk.t[:, :], screp.t[:, kc, s, :], wt.t[:, kc, half * 512:(half + 1) * 512],
                               kc == 0, kc == KC - 1, [screp, wt], [bk])
                        tt(k, "dve", gt.t[:, half * 512:(half + 1) * 512], bk.t[:, :],
                           br.t[:, half * 512:(half + 1) * 512], ALU.add, [bk, br], [gt])
                    k.dma(c.GREP[li, g, s, :, :], gt.t[:], [gt], [], gt)
            else:
                bk = k.bank()
                for oc in range(KC):
                    for kc in range(KC):
                        mm(k, bk.t[:, oc * NS:(oc + 1) * NS], wt.t[:, kc, oc * 128:(oc + 1) * 128],
                           sc.t[:, kc, :], kc == 0, kc == KC - 1, [wt, sc], [bk])
                bv = bk.t[:, 0:KC * NS].rearrange("p (o s) -> p o s", s=NS)
                for s in range(NS):
                    tt(k, "dve", tmp.t[:, :, s], bv[:, :, s], abT.t[:, li, part * 8:(part + 1) * 8], ALU.add,
                       [bk, abT], [tmp])
                which = 0 if part < 3 else 1
                if part in (1, 4):
                    for s in range(NS):
                        stt(k, c.s_mods.t[:, li, 2 * which, :, s], tmp.t[:, :, s], 1.0, nrm.t[:, li, which, :],
                            ALU.add, ALU.mult, [tmp, nrm], [c.s_mods])
                else:
                    cp(k, "dve", c.s_mods.t[:, li, 2 * which + 1, :, :], tmp.t[:, :, :], [tmp], [c.s_mods])
    k.end()


def norm_block(c, xin, nsub, ss, lnv, rstd, junk, engs=("dve", "act")):
    k = c.k
    for u in range(nsub):
        act(k, junk.t[:], xin.t[:, u, :], AF.Square, [xin], [junk, ss], accum_out=ss.t[:, u:u + 1])
    act(k, lnv.t[:, 0:nsub], ss.t[:, 0:nsub], AF.Ln, [ss], [lnv], scale=1.0 / D, bias=EPS)
    act(k, rstd.t[:, 0:nsub], lnv.t[:, 0:nsub], AF.Exp, [lnv], [rstd], scale=-0.5)
    for u in range(nsub):
        e = engs[u % len(engs)]
        if e == "act":
            act(k, xin.t[:, u, :], xin.t[:, u, :], AF.Identity, [xin, rstd], [xin], scale=rstd.t[:, u:u + 1])
        else:
            ts(k, e, xin.t[:, u, :], xin.t[:, u, :], rstd.t[:, u:u + 1], None, ALU.mult, None,
               [xin, rstd], [xin])


def transpose_block(c, xin, nsub, hT, col0, li, which, slot, extra=None):
    k = c.k
    A = c.s_mods.t[:, li, 2 * which, :, :]
    B = c.s_mods.t[:, li, 2 * which + 1, :, :]
    n = 0
    for kc in range(KC):
        for u0 in range(0, nsub, 4):
            nu = min(4, nsub - u0)
            bk = k.bank()
            for u in range(nu):
                k.op("pe", lambda h, u=u: h.transpose(out=bk.t[:, u * 128:(u + 1) * 128],
                                                       in_=xin.t[:, u0 + u, kc * 128:(kc + 1) * 128],
                                                       identity=c.s_ident.t[:]),
                     [xin, c.s_ident], [bk], inc=(u == nu - 1))
            o = hT.t[:, kc, col0 + u0 * 128: col0 + (u0 + nu) * 128]
            if n % 2 == 0:
                act(k, o, bk.t[:, 0:nu * 128], AF.Identity, [bk, c.s_mods], [hT],
                    scale=A[:, kc, slot:slot + 1], bias=B[:, kc, slot:slot + 1])
            else:
                ts(k, "dve", o, bk.t[:, 0:nu * 128], A[:, kc, slot:slot + 1], B[:, kc, slot:slot + 1],
                   ALU.mult, ALU.add, [bk, c.s_mods], [hT])
            if extra is not None:
                extra(kc, u0, nu, bk)
            n += 1


def phase_r1(c, li, j, cur):
    k = c.k
    T = c.T
    X = c.X[cur]
    k.begin()
    w_in = k.sb("w_in", [128, KC, 2048], BF16)
    k.dma(w_in.t[:].rearrange("p k n -> p (k n)"), c.wb["rg_w_in"][j, :, :], [], [w_in], w_in)
    xin = [k.sb("xin%d" % i, [128, 4, D], F32) for i in range(2)]
    junk = k.sb("junk", [128, D], F32)
    sm = [[k.sb("sm%d_%d" % (i, q), [128, 4], F32) for q in range(3)] for i in range(2)]
    hT = [k.sb("hT%d" % i, [128, KC, 512], BF16) for i in range(2)]
    xbT = [k.sb("xbT%d" % i, [128, KC, 512], F32) for i in range(2)]
    gyT = [k.sb("gyT%d" % i, [128, KC, 512], BF16) for i in range(2)]
    tm = [[k.sb("tm%d_%d" % (i, q), [128, 512], F32) for q in range(4)] for i in range(2)]
    ntile = T // 512

    def load(i):
        k.dma(xin[i % 2].t[:], X[PAD + i * 512:PAD + i * 512 + 512, :].rearrange("(s p) f -> p s f", p=128),
              [], [xin[i % 2]], xin[i % 2])

    load(0)
    for i in range(ntile):
        t0 = i * 512
        slot = i // 8
        xi = xin[i % 2]
        if i + 1 < ntile:
            load(i + 1)
        ss, lnv, rstd = sm[i % 2]
        norm_block(c, xi, 4, ss, lnv, rstd, junk)
        h = hT[i % 2]
        transpose_block(c, xi, 4, h, 0, li, 0, slot)
        xo = xbT[i % 2]
        go = gyT[i % 2]
        for oc in range(16):
            bk = k.bank()
            for kc in range(KC):
                mm(k, bk.t[:, :], w_in.t[:, kc, oc * 128:(oc + 1) * 128], h.t[:, kc, :], kc == 0, kc == KC - 1,
                   [w_in, h], [bk])
            if oc < 8:
                cp(k, "act" if oc % 2 == 0 else "dve", xo.t[:, oc, :], bk.t[:, :], [bk], [xo])
            else:
                xs, sq, inn, sg = tm[oc % 2]
                cp(k, "act", xs.t[:], bk.t[:, :], [bk], [xs])
                act(k, sq.t[:], bk.t[:, :], AF.Square, [bk], [sq])
                ts(k, "dve", inn.t[:], sq.t[:], 0.044715, 1.0, ALU.mult, ALU.add, [sq], [inn])
                tt(k, "dve", inn.t[:], inn.t[:], xs.t[:], ALU.mult, [inn, xs], [inn])
                act(k, sg.t[:], inn.t[:], AF.Exp, [inn], [sg], scale=-GEL_C)
                act(k, sg.t[:], sg.t[:], AF.Ln, [sg], [sg], bias=1.0)
                act(k, sg.t[:], sg.t[:], AF.Exp, [sg], [sg], scale=-1.0)
                tt(k, "dve", go.t[:, oc - 8, :], xs.t[:], sg.t[:], ALU.mult, [xs, sg], [go])
        k.dma(c.XBs[:, :, 4 + t0:4 + t0 + 512].rearrange("c p t -> p c t"), xo.t[:], [xo], [], xo)
        k.dma(c.GYs[:, :, t0:t0 + 512].rearrange("c p t -> p c t"), go.t[:], [go], [], go)
    k.end()


def rg_consts(c, j, z):
    k = c.k
    gw = k.sb("gw", [128, 8192], BF16)
    k.dma(gw.t[:], c.wb["rg_gate_w"][j, :, :], [], [gw], gw)
    gb = k.sb("gb", [128, 2, 2, 2, KC], F32)
    k.dma(gb.t[:], c.rg_gb[:, :, :, :, :], [], [gb], gb)
    negb = k.sb("negb", [128, 2, KC], F32)
    ts(k, "dve", negb.t[:], gb.t[:, j, z, :, :], -1.0, None, ALU.mult, None, [gb], [negb])
    lam = k.sb("lam", [128, 2, 2, KC], F32)
    k.dma(lam.t[:], c.rg_lam[:, :, :, :], [], [lam], lam)
    c8 = k.sb("c8", [128, KC], F32)
    c16 = k.sb("c16", [128, KC], F32)
    act(k, c8.t[:], lam.t[:, j, z, :], AF.Exp, [lam], [c8], scale=-1.0)
    act(k, c8.t[:], c8.t[:], AF.Ln, [c8], [c8], bias=1.0)
    ts(k, "dve", c16.t[:], c8.t[:], -16.0, None, ALU.mult, None, [c8], [c16])
    ts(k, "dve", c8.t[:], c8.t[:], -8.0, None, ALU.mult, None, [c8], [c8])
    return gw, negb, c8, c16


def rg_gates(c, z, gw, negb, c8, c16, xc, xcb, tmps, hout, carry, reverse):
    k = c.k
    gv = gw.t[:].rearrange("p (z g n k d) -> p z g n k d", z=2, g=2, n=4, k=2)
    for g0 in (0, 4):
        ocs = list(range(g0, g0 + 4))
        bk_r = {}
        bk_i = {}
        for oc in ocs:
            n_, half = oc // 2, oc % 2
            bk_r[oc] = k.bank()
            for kk in range(2):
                mm(k, bk_r[oc].t[:, :], gv[:, z, 0, n_, kk, half * 128:(half + 1) * 128], xcb.t[:, 2 * n_ + kk, :],
                   kk == 0, kk == 1, [gw, xcb], [bk_r[oc]])
            bk_i[oc] = k.bank()
            for kk in range(2):
                mm(k, bk_i[oc].t[:, :], gv[:, z, 1, n_, kk, half * 128:(half + 1) * 128], xcb.t[:, 2 * n_ + kk, :],
                   kk == 0, kk == 1, [gw, xcb], [bk_i[oc]])
        T_ = {oc: tmps[oc % 4] for oc in ocs}
        for oc in ocs:
            er, ei, a, a2, u = T_[oc]
            act(k, er.t[:], bk_r[oc].t[:, :], AF.Exp, [bk_r[oc], negb], [er], scale=-1.0, bias=negb.t[:, 0, oc:oc + 1])
            act(k, ei.t[:], bk_i[oc].t[:, :], AF.Exp, [bk_i[oc], negb], [ei], scale=-1.0, bias=negb.t[:, 1, oc:oc + 1])
        for oc in ocs:
            er = T_[oc][0]
            ts(k, "dve", er.t[:], er.t[:], 1.0, None, ALU.add, None, [er], [er])
        for oc in ocs:
            ei = T_[oc][1]
            act(k, ei.t[:], ei.t[:], AF.Ln, [ei], [ei], bias=1.0)
        for oc in ocs:
            er = T_[oc][0]
            k.op("dve", lambda h, er=er: h.reciprocal(out=er.t[:], in_=er.t[:]), [er], [er])
        for oc in ocs:
            ei = T_[oc][1]
            act(k, ei.t[:], ei.t[:], AF.Exp, [ei], [ei], scale=-1.0)
        for oc in ocs:
            er, ei, a, a2, u = T_[oc]
            tt(k, "pool", u.t[:], ei.t[:], xc.t[:, oc, :], ALU.mult, [ei, xc], [u])
        for oc in ocs:
            er, ei, a, a2, u = T_[oc]
            act(k, a.t[:], er.t[:], AF.Exp, [er, c8], [a], scale=c8.t[:, oc:oc + 1])
        for oc in ocs:
            er, ei, a, a2, u = T_[oc]
            act(k, a2.t[:], er.t[:], AF.Exp, [er, c16], [a2], scale=c16.t[:, oc:oc + 1])
        for oc in ocs:
            a2 = T_[oc][3]
            act(k, a2.t[:], a2.t[:], AF.Ln, [a2], [a2], scale=-1.0, bias=1.0)
        for oc in ocs:
            a2 = T_[oc][3]
            act(k, a2.t[:], a2.t[:], AF.Exp, [a2], [a2], scale=0.5)
        for oc in ocs:
            er, ei, a, a2, u = T_[oc]
            tt(k, "dve", u.t[:], u.t[:], a2.t[:], ALU.mult, [u, a2], [u])
        for oc in ocs:
            er, ei, a, a2, u = T_[oc]
            cr = carry[oc]
            if reverse:
                k.op("dve", lambda h, a=a, u=u, cr=cr, oc=oc: h.tensor_tensor_scan(
                    out=hout[oc].t[:, oc, ::-1], data0=a.t[:, ::-1], data1=u.t[:, ::-1], initial=cr.t[:, 0:1],
                    op0=ALU.mult, op1=ALU.add), [a, u, cr], [hout[oc]])
                cp(k, "pool", cr.t[:, 0:1], hout[oc].t[:, oc, 0:1], [hout[oc]], [cr])
            else:
                k.op("dve", lambda h, a=a, u=u, cr=cr, oc=oc: h.tensor_tensor_scan(
                    out=hout[oc].t[:, oc, :], data0=a.t[:, :], data1=u.t[:, :], initial=cr.t[:, 0:1],
                    op0=ALU.mult, op1=ALU.add), [a, u, cr], [hout[oc]])
                cp(k, "pool", cr.t[:, 0:1], hout[oc].t[:, oc, 511:512], [hout[oc]], [cr])


def phase_r2(c, li, j):
    k = c.k
    T = c.T
    k.begin()
    gw, negb, c8, c16 = rg_consts(c, j, 0)
    cw = k.sb("cw", [128, 2, KC, 5], F32)
    k.dma(cw.t[:], c.rg_conv[:, :, :, :], [], [cw], cw)
    dg = k.sb("dg", [128, KC, 4, 128], F32)
    for ch in range(KC):
        for tp in range(4):
            ts(k, "pool", dg.t[:, ch, tp, :], c.s_ident.t[:], cw.t[:, j, ch, tp:tp + 1], None, ALU.mult, None,
               [c.s_ident, cw], [dg])
    xbw = [k.sb("xbw%d" % i, [128, KC, 515], F32) for i in range(2)]
    xc = [k.sb("xc%d" % i, [128, KC, 512], F32) for i in range(2)]
    xcb = [k.sb("xcb%d" % i, [128, KC, 512], BF16) for i in range(2)]
    hf = [k.sb("hf%d" % i, [128, KC, 512], F32) for i in range(2)]
    hfv = [[hf[i]] + [Buf("hfv", hf[i].t) for _ in range(KC - 1)] for i in range(2)]
    tmps = [[k.sb("gt%d_%d" % (i, q), [128, 512], F32) for q in range(5)] for i in range(4)]
    carry = [k.sb("carry%d" % i, [128, 1], F32) for i in range(KC)]
    for cr in carry:
        k.op("pool", lambda h, cr=cr: h.memset(cr.t[:], 0.0), [], [cr])
    ntile = T // 512
    def load(i):
        k.dma(xbw[i % 2].t[:], c.XBs[:, :, 4 + i * 512 - 2:4 + i * 512 + 513].rearrange("c p t -> p c t"),
              [], [xbw[i % 2]], xbw[i % 2])

    load(0)
    for i in range(ntile):
        t0 = i * 512
        xw = xbw[i % 2]
        if i + 1 < ntile:
            load(i + 1)
        if i == 0:
            k.op("pool", lambda h: h.memset(xw.t[:, :, 0:2], 0.0), [], [xw])
        elif i % 8 == 0:
            ts(k, "pool", xw.t[:, :, 0:2], xw.t[:, :, 0:2], c.s_flags.t[:, 0:1], None, ALU.mult, None,
               [xw, c.s_flags], [xw])
            for cr in carry:
                ts(k, "pool", cr.t[:], cr.t[:], c.s_flags.t[:, 0:1], None, ALU.mult, None,
                   [cr, c.s_flags], [cr])
        if i == ntile - 1:
            k.op("pool", lambda h: h.memset(xw.t[:, :, 514:515], 0.0), [], [xw])
        elif i % 8 == 7:
            ts(k, "pool", xw.t[:, :, 514:515], xw.t[:, :, 514:515], c.s_flags.t[:, 0:1], None, ALU.mult, None,
               [xw, c.s_flags], [xw])
        xcc = xc[i % 2]
        xcbb = xcb[i % 2]
        for ch in range(KC):
            bk = k.bank()
            for tp in range(4):
                mm(k, bk.t[:, :], dg.t[:, ch, tp, :], xw.t[:, ch, tp:tp + 512], tp == 0, tp == 3, [dg, xw], [bk])
            act(k, xcc.t[:, ch, :], bk.t[:, :], AF.Identity, [bk, cw], [xcc], bias=cw.t[:, j, ch, 4:5])
            cp(k, "pool", xcbb.t[:, ch, :], xcc.t[:, ch, :], [xcc], [xcbb])
        hh = hfv[i % 2]
        rg_gates(c, 0, gw, negb, c8, c16, xcc, xcbb, tmps, hh, carry, False)
        k.dma(c.XCs[:, :, t0:t0 + 512].rearrange("c p t -> p c t"), xcc.t[:], [xcc], [], xcc)
        k.dma(c.HFs[:, :, t0:t0 + 512].rearrange("c p t -> p c t"), hh[0].t[:], hh, [], hh[0])
    k.end()


def phase_r3(c, li, j, cur):
    k = c.k
    T = c.T
    X = c.X[cur]
    k.begin()
    gw, negb, c8, c16 = rg_consts(c, j, 1)
    w_out = k.sb("w_out", [128, KC, D], BF16)
    k.dma(w_out.t[:].rearrange("p k n -> p (k n)"), c.wb["rg_w_out"][j, :, :], [], [w_out], w_out)
    xcs = [k.sb("xc%d" % i, [128, KC, 512], F32) for i in range(2)]
    xcb = k.sb("xcb", [128, KC, 512], BF16)
    hfl = k.sb("hfl", [128, KC, 512], F32)
    gyl = k.sb("gyl", [128, KC, 512], BF16)
    hb = k.sb("hb", [128, KC, 512], F32)
    hbv = [hb] + [Buf("hbv", hb.t) for _ in range(KC - 1)]
    yT = k.sb("yT", [128, KC, 512], BF16)
    xin = k.sb("xin", [128, 4, D], F32)
    g1 = [k.sb("g1_%d" % i, [128, D], F32) for i in range(2)]
    tmps = [[k.sb("gt%d_%d" % (i, q), [128, 512], F32) for q in range(5)] for i in range(4)]
    tmo = [k.sb("tmo%d" % i, [128, 512], F32) for i in range(2)]
    carry = [k.sb("carry%d" % i, [128, 1], F32) for i in range(KC)]
    for cr in carry:
        k.op("pool", lambda h, cr=cr: h.memset(cr.t[:], 0.0), [], [cr])
    ntile = T // 512
    gcur = None
    for i in range(ntile - 1, -1, -1):
        t0 = i * 512
        slot = i // 8
        if gcur is None or gcur[0] != slot:
            gb_ = g1[slot % 2]
            k.dma(gb_.t[:], c.GREP[li, 0, slot, :, :], [], [gb_], gb_)
            gcur = (slot, gb_)
        gb_ = gcur[1]
        xc = xcs[i % 2]
        if i == ntile - 1:
            k.dma(xc.t[:], c.XCs[:, :, t0:t0 + 512].rearrange("c p t -> p c t"), [], [xc], xc)
        if i - 1 >= 0:
            xn_ = xcs[(i - 1) % 2]
            k.dma(xn_.t[:], c.XCs[:, :, t0 - 512:t0].rearrange("c p t -> p c t"), [], [xn_], xn_)
        k.dma(hfl.t[:], c.HFs[:, :, t0:t0 + 512].rearrange("c p t -> p c t"), [], [hfl], hfl)
        k.dma(gyl.t[:], c.GYs[:, :, t0:t0 + 512].rearrange("c p t -> p c t"), [], [gyl], gyl)
        k.dma(xin.t[:], X[PAD + t0:PAD + t0 + 512, :].rearrange("(s p) f -> p s f", p=128), [], [xin], xin)
        if i % 8 == 7 and i != ntile - 1:
            for cr in carry:
                ts(k, "pool", cr.t[:], cr.t[:], c.s_flags.t[:, 0:1], None, ALU.mult, None,
                   [cr, c.s_flags], [cr])
        for ch in range(KC):
            cp(k, "pool", xcb.t[:, ch, :], xc.t[:, ch, :], [xc], [xcb])
        rg_gates(c, 1, gw, negb, c8, c16, xc, xcb, tmps, hbv, carry, True)
        for ch in range(KC):
            tt(k, "pool", hfl.t[:, ch, :], hfl.t[:, ch, :], hb.t[:, ch, :], ALU.add, [hfl, hbv[ch]], [hfl])
            tt(k, "dve", yT.t[:, ch, :], hfl.t[:, ch, :], gyl.t[:, ch, :], ALU.mult, [hfl, gyl], [yT])
        n = 0
        for u in range(4):
            for half in range(2):
                bk = k.bank()
                for kc in range(KC):
                    mm(k, bk.t[:, :], yT.t[:, kc, u * 128:(u + 1) * 128], w_out.t[:, kc, half * 512:(half + 1) * 512],
                       kc == 0, kc == KC - 1, [yT, w_out], [bk])
                tm_ = tmo[n % 2]
                n += 1
                tt(k, "dve", tm_.t[:], bk.t[:, :], gb_.t[:, half * 512:(half + 1) * 512], ALU.mult, [bk, gb_], [tm_])
                tt(k, "pool", xin.t[:, u, half * 512:(half + 1) * 512], xin.t[:, u, half * 512:(half + 1) * 512],
                   tm_.t[:], ALU.add, [xin, tm_], [xin])
        k.dma(X[PAD + t0:PAD + t0 + 512, :].rearrange("(s p) f -> p s f", p=128), xin.t[:], [xin], [], xin)
    k.end()


def phase_ffn(c, li, j, cur, moe, final=False):
    k = c.k
    T = c.T
    X = c.X[cur]
    ST = 1024
    k.begin()
    xin = [k.sb("xin%d" % i, [128, 2, D], F32) for i in range(2)]
    junk = k.sb("junk", [128, D], F32)
    sm = [[k.sb("sm%d_%d" % (i, q), [128, 4], F32) for q in range(3)] for i in range(2)]
    hTs = [k.sb("hT%d" % i, [128, KC, ST], BF16) for i in range(2)]
    acc = k.sb("acc", [128, 8, D], F32)
    SL = 4
    hs = [k.sb("hs%d" % i, [128, SL, ST], BF16) for i in range(2)]
    wgu = [k.sb("wgu%d" % i, [128, KC, 256], BF16) for i in range(4)]
    wdn = [k.sb("wdn%d" % i, [128, SL, D], BF16) for i in range(2)]
    sg = [k.sb("sg%d" % i, [128, 512], F32) for i in range(4)]
    tmo = [k.sb("tmo%d" % i, [128, 512], F32) for i in range(2)]
    g2 = [k.sb("g2_%d" % i, [128, D], F32) for i in range(2)]
    if moe:
        rt = k.sb("rt", [128, 2, KC, NEXP], F32)
        k.dma(rt.t[:], c.moe_rt[:, :, :, :], [], [rt], rt)
        rtb = k.sb("rtb", [128, KC, 128], BF16)
        k.op("pool", lambda h: h.memset(rtb.t[:], 0.0), [], [rtb])
        cp(k, "dve", rtb.t[:, :, 0:NEXP], rt.t[:, j, :, :], [rt], [rtb])
        lgs = [k.sb("lg%d" % i, [128, 8, NEXP], F32) for i in range(2)]
        comb = k.sb("comb", [128, 8, NEXP], F32)
        m1 = k.sb("m1", [128, 8], F32)
        m2 = k.sb("m2", [128, 8], F32)
        k1 = k.sb("k1", [128, 8, NEXP], F32)
        k2 = k.sb("k2", [128, 8, NEXP], F32)
        l2 = k.sb("l2", [128, 8, NEXP], F32)
        w1 = k.sb("w1", [128, 8], F32)
        w2 = k.sb("w2", [128, 8], F32)
    if moe:
        nch = 28
        experts = range(NEXP)
        gu_src = None
        dn_src = None
    else:
        nch = 22
        experts = [0]
        gu_src = c.wb["ff_gu"]
        dn_src = c.wb["ff_dn"]
    slabs = []
    for e in experts:
        f = 0
        while f < nch:
            n = min(SL, nch - f)
            base = 0 if moe else (j * 22)
            slabs.append((e, base + f, n))
            f += n
    nst = T // ST
    wg_i = 0
    evn = 0
    gcur = None
    def get_g2(slot):
        nonlocal gcur
        if gcur is None or gcur[0] != slot:
            gb__ = g2[slot % 2]
            k.dma(gb__.t[:], c.GREP[li, 1, slot, :, :], [], [gb__], gb__)
            gcur = (slot, gb__)
        return gcur[1]

    def normT(q):
        t0 = q * ST
        slot = t0 // SLOT
        hTq = hTs[q % 2]
        lgq = lgs[q % 2] if moe else None
        for pr in range(4):
            xi = xin[pr % 2]
            r0 = PAD + t0 + pr * 256
            k.dma(xi.t[:], X[r0:r0 + 256, :].rearrange("(s p) f -> p s f", p=128), [], [xi], xi)
            ss, lnv, rstd = sm[pr % 2]
            norm_block(c, xi, 2, ss, lnv, rstd, junk, engs=("act", "dve"))
            transpose_block(c, xi, 2, hTq, pr * 256, li, 1, slot)
            if moe:
                for u in range(2):
                    sub = pr * 2 + u
                    bk = k.bank()
                    for kc in range(KC):
                        mm(k, bk.t[:, 0:128], hTq.t[:, kc, sub * 128:(sub + 1) * 128], rtb.t[:, kc, :], kc == 0,
                           kc == KC - 1, [hTq, rtb], [bk])
                    cp(k, "dve", lgq.t[:, sub, :], bk.t[:, 0:NEXP], [bk], [lgq])

    if FFN_MAXST is not None:
        nst = min(nst, FFN_MAXST)
    normT(0)
    for q in range(nst):
        t0 = q * ST
        slot = t0 // SLOT
        gb_ = get_g2(slot)
        hT = hTs[q % 2]
        if moe:
            lg = lgs[q % 2]
        if moe and MOE_STAGE < 1:
            k.op("pool", lambda h: h.memset(comb.t[:], 0.125), [], [comb])
        if moe and MOE_STAGE >= 1:
            k.op("dve", lambda h: h.tensor_reduce(out=m1.t[:], in_=lg.t[:], axis=mybir.AxisListType.X, op=ALU.max),
                 [lg], [m1])
            for sub in range(8):
                ts(k, "dve", k1.t[:, sub, :], lg.t[:, sub, :], m1.t[:, sub:sub + 1], None, ALU.is_equal, None,
                   [lg, m1], [k1])
            stt(k, l2.t[:], k1.t[:], -1e30, lg.t[:], ALU.mult, ALU.add, [k1, lg], [l2])
            k.op("dve", lambda h: h.tensor_reduce(out=m2.t[:], in_=l2.t[:], axis=mybir.AxisListType.X, op=ALU.max),
                 [l2], [m2])
            for sub in range(8):
                ts(k, "dve", k2.t[:, sub, :], l2.t[:, sub, :], m2.t[:, sub:sub + 1], None, ALU.is_equal, None,
                   [l2, m2], [k2])
            tt(k, "dve", w2.t[:], m2.t[:], m1.t[:], ALU.subtract, [m1, m2], [w2])
            act(k, w2.t[:], w2.t[:], AF.Exp, [w2], [w2])
            ts(k, "dve", w1.t[:], w2.t[:], 1.0, None, ALU.add, None, [w2], [w1])
            k.op("dve", lambda h: h.reciprocal(out=w1.t[:], in_=w1.t[:]), [w1], [w1])
            tt(k, "dve", w2.t[:], w2.t[:], w1.t[:], ALU.mult, [w2, w1], [w2])
            for sub in range(8):
                ts(k, "dve", k1.t[:, sub, :], k1.t[:, sub, :], w1.t[:, sub:sub + 1], None, ALU.mult, None,
                   [k1, w1], [k1])
                stt(k, comb.t[:, sub, :], k2.t[:, sub, :], w2.t[:, sub:sub + 1], k1.t[:, sub, :], ALU.mult, ALU.add,
                    [k2, w2, k1], [comb])
        first_acc = [True] * 16

        def do_gu(si):
            nonlocal wg_i
            e, cb, n = slabs[si]
            hsl = hs[si % 2]
            gsrc = c.wb["moe_gu%d" % (j * NEXP + e)] if moe else gu_src
            for f in range(n):
                wg = wgu[wg_i % 4]
                wg_i += 1
                k.dma(wg.t[:].rearrange("p k n -> p (k n)"), gsrc[cb + f, :, :], [], [wg], wg)
                for tk in range(2):
                    bg = k.bank()
                    for kc in range(KC):
                        mm(k, bg.t[:, :], wg.t[:, kc, 0:128], hT.t[:, kc, tk * 512:(tk + 1) * 512], kc == 0,
                           kc == KC - 1, [wg, hT], [bg])
                    bu = k.bank()
                    for kc in range(KC):
                        mm(k, bu.t[:, :], wg.t[:, kc, 128:256], hT.t[:, kc, tk * 512:(tk + 1) * 512], kc == 0,
                           kc == KC - 1, [wg, hT], [bu])
                    s_ = sg[(f * 2 + tk) % 4]
                    act(k, s_.t[:], bg.t[:, :], AF.Silu, [bg], [s_])
                    tt(k, "dve", hsl.t[:, f, tk * 512:(tk + 1) * 512], s_.t[:], bu.t[:, :], ALU.mult, [s_, bu], [hsl])

        def pre_dn(si):
            e, cb, n = slabs[si]
            wd = wdn[si % 2]
            dsrc = c.wb["moe_dn%d" % (j * NEXP + e)] if moe else dn_src
            k.dma(wd.t[:, 0:n, :], dsrc[cb:cb + n, :, :].rearrange("n p f -> p n f"), [], [wd], wd)

        def do_dn(si):
            nonlocal evn
            e, cb, n = slabs[si]
            hsl = hs[si % 2]
            wd = wdn[si % 2]
            for sub in range(8):
                for half in range(2):
                    bk = k.bank()
                    for f in range(n):
                        mm(k, bk.t[:, :], hsl.t[:, f, sub * 128:(sub + 1) * 128], wd.t[:, f, half * 512:(half + 1) * 512],
                           f == 0, f == n - 1, [hsl, wd], [bk])
                    av = acc.t[:, sub, half * 512:(half + 1) * 512]
                    bi = sub * 2 + half
                    if moe:
                        cs = comb.t[:, sub, e:e + 1]
                        if first_acc[bi]:
                            ts(k, "dve", av, bk.t[:, :], cs, None, ALU.mult, None, [bk, comb], [acc])
                        elif evn % 2 == 0:
                            stt(k, av, bk.t[:, :], cs, av, ALU.mult, ALU.add, [bk, comb, acc], [acc])
                        else:
                            tm_ = tmo[(evn // 2) % 2]
                            act(k, tm_.t[:], bk.t[:, :], AF.Identity, [bk, comb], [tm_], scale=cs)
                            tt(k, "pool", av, av, tm_.t[:], ALU.add, [acc, tm_], [acc])
                    else:
                        if first_acc[bi]:
                            cp(k, "act", av, bk.t[:, :], [bk], [acc])
                        elif evn % 2 == 0:
                            tt(k, "dve", av, bk.t[:, :], av, ALU.add, [bk, acc], [acc])
                        else:
                            tm_ = tmo[(evn // 2) % 2]
                            cp(k, "act", tm_.t[:], bk.t[:, :], [bk], [tm_])
                            tt(k, "pool", av, av, tm_.t[:], ALU.add, [acc, tm_], [acc])
                    first_acc[bi] = False
                    evn += 1

        do_gu(0)
        nmid = max(0, len(slabs) - 3)
        for si in range(len(slabs)):
            pre_dn(si)
            if si + 1 < len(slabs):
                do_gu(si + 1)
            if si == nmid and q + 1 < nst:
                normT(q + 1)
            do_dn(si)
        for pr in range(4):
            xi = xin[pr % 2]
            r0 = PAD + t0 + pr * 256
            k.dma(xi.t[:], X[r0:r0 + 256, :].rearrange("(s p) f -> p s f", p=128), [], [xi], xi)
            for u in range(2):
                sub = pr * 2 + u
                tt(k, "dve", acc.t[:, sub, :], acc.t[:, sub, :], gb_.t[:], ALU.mult, [acc, gb_], [acc])
                tt(k, "pool", xi.t[:, u, :], xi.t[:, u, :], acc.t[:, sub, :], ALU.add, [xi, acc], [xi])
            if final:
                y0 = t0 + pr * 256
                k.dma(c.y[y0:y0 + 256, :].rearrange("(s p) f -> p s f", p=128), xi.t[:], [xi], [], xi)
            else:
                k.dma(X[r0:r0 + 256, :].rearrange("(s p) f -> p s f", p=128), xi.t[:], [xi], [], xi)
    k.end()


def phase_attn(c, li, j, gi, cur):
    k = c.k
    T = c.T
    NS = c.NSLOT
    Xr = c.X[cur]
    Xw = c.X[1 - cur]
    win, d = GROUPS[gi]
    L = SLOT // d
    NT = 512 if L >= 512 else L
    nQB = NT // 128
    nKB = nQB + 1
    W = NT + 128
    ntr = L // NT
    last = (gi == 2)
    k.begin()
    wq = k.sb("wq", [128, KC, 1536], BF16)
    k.dma(wq.t[:].rearrange("p k n -> p (k n)"), c.wb["at_qkv"][j * 3 + gi, :, :], [], [wq], wq)
    qkn = k.sb("qkn", [128, 2, 2], F32)
    k.dma(qkn.t[:], c.at_qkn[:, :, :], [], [qkn], qkn)
    gq = k.sb("gq", [128, 2], F32)
    cp(k, "dve", gq.t[:], qkn.t[:, j, :], [qkn], [gq])
    ts(k, "dve", gq.t[:, 0:1], gq.t[:, 0:1], 0.125, None, ALU.mult, None, [gq], [gq])
    bones = k.sb("bones", [128, 128], F32)
    k.dma(bones.t[:], c.bones[:, :], [], [bones], bones)
    pswap = k.sb("pswap", [128, 128], F32)
    k.dma(pswap.t[:], c.pswap[:, :], [], [pswap], pswap)
    bb32 = k.sb("bb32", [128, 256], F32)
    k.dma(bb32.t[:], c.bandb[:, :], [], [bb32], bb32)
    bb = k.sb("bb", [128, 256], BF16)
    cp(k, "dve", bb.t[:], bb32.t[:], [bb32], [bb])
    idb = k.sb("idb", [128, 128], BF16)
    cp(k, "dve", idb.t[:], c.s_ident.t[:], [c.s_ident], [idb])
    xins = [k.sb("xin%d" % i, [128, nKB, D], F32) for i in range(2)]
    junk = k.sb("junk", [128, D], F32)
    sm = [k.sb("sm%d" % q, [128, 8], F32) for q in range(3)]
    hT = k.sb("hT", [128, KC, W], BF16)
    ctabs = [k.sb("ctab%d" % i, [128, W], F32) for i in range(2)]
    stabs = [k.sb("stab%d" % i, [128, W], F32) for i in range(2)]
    QT = k.sb("QT", [128, 4, NT], BF16)
    KT = k.sb("KT", [128, 4, W], BF16)
    qt_ = [[k.sb("qt%d_%d" % (i, q), [128, 512], F32) for q in range(4)] for i in range(2)]
    VE = k.sb("VE", [128, nKB, 8, 66], BF16)
    PT = k.sb("PT", [128, nKB, 8, 256], BF16)
    osb = [k.sb("osb%d" % i, [128, 528], F32) for i in range(2)]
    oac = [k.sb("oac%d" % i, [128, 528], F32) for i in range(2)]
    if last:
        wo = k.sb("wo", [128, 4, D], BF16)
        k.dma(wo.t[:].rearrange("p k n -> p (k n)"), c.wb["at_wo"][j, :, :], [], [wo], wo)
        rden = k.sb("rden", [128, 8], F32)
        on = k.sb("on", [128, 512], F32)
        oT = k.sb("oT", [128, 4, 128], BF16)
        xres = [k.sb("xres%d" % i, [128, D], F32) for i in range(2)]
        g1 = [k.sb("g1_%d" % i, [128, D], F32) for i in range(2)]
        tmo = [k.sb("tmo%d" % i, [128, 512], F32) for i in range(2)]
    gcur = None
    un = 0
    on_ = 0
    tiles = [(slot, r, w_) for slot in range(NS) for r in range(d) for w_ in range(ntr)]
    if ATTN_MAXTILES is not None:
        tiles = tiles[:ATTN_MAXTILES]
    tidx = 0

    def load_tile(ti):
        slot, r, w_ = tiles[ti]
        j0 = w_ * NT
        xin = xins[ti % 2]
        row0 = PAD + slot * SLOT + r + d * (j0 - 64)
        k.dma(xin.t[:], Xr[row0:row0 + d * (W - 1) + 1:d, :].rearrange("(b p) f -> p b f", p=128), [], [xin], xin)
        tcol = 64 + r * (NS * L) + slot * L + j0 - 64
        k.dma(ctabs[ti % 2].t[:], c.rope[gi, 0, :, tcol:tcol + W], [], [ctabs[ti % 2]], ctabs[ti % 2])
        k.dma(stabs[ti % 2].t[:], c.rope[gi, 1, :, tcol:tcol + W], [], [stabs[ti % 2]], stabs[ti % 2])

    for slot in range(NS):
        if last:
            gb_ = g1[slot % 2]
            k.dma(gb_.t[:], c.GREP[li, 0, slot, :, :], [], [gb_], gb_)
        for r in range(d):
            for w_ in range(ntr):
                if tidx >= len(tiles):
                    continue
                j0 = w_ * NT
                if tidx == 0:
                    load_tile(0)
                if tidx + 1 < len(tiles):
                    load_tile(tidx + 1)
                xin = xins[tidx % 2]
                ctab = ctabs[tidx % 2]
                stab = stabs[tidx % 2]
                tidx += 1
                norm_block(c, xin, nKB, sm[0], sm[1], sm[2], junk)
                transpose_block(c, xin, nKB, hT, 0, li, 0, slot)
                units = []
                for ch in range(4):
                    units.append((0, ch, 64, NT, QT, 0))
                for ch in range(4):
                    c0 = 0
                    while c0 < W:
                        n = min(512, W - c0)
                        units.append((1, ch, c0, n, KT, c0))
                        c0 += n
                def part1(ui):
                    (isk, ch, h0, n, dst, d0) = units[ui]
                    qg, sq, rs, t1 = qt_[(un + ui) % 2]
                    bk = k.bank()
                    wc = isk * 512 + ch * 128
                    for kc in range(KC):
                        mm(k, bk.t[:, 0:n], wq.t[:, kc, wc:wc + 128], hT.t[:, kc, h0:h0 + n], kc == 0, kc == KC - 1,
                           [wq, hT], [bk])
                    act(k, qg.t[:, 0:n], bk.t[:, 0:n], AF.Identity, [bk, gq], [qg], scale=gq.t[:, isk:isk + 1])
                    act(k, sq.t[:, 0:n], bk.t[:, 0:n], AF.Square, [bk], [sq])

                part1(0)
                for ui, (isk, ch, h0, n, dst, d0) in enumerate(units):
                    qg, sq, rs, t1 = qt_[(un + ui) % 2]
                    if ui + 1 < len(units):
                        part1(ui + 1)
                    b2 = k.bank()
                    mm(k, b2.t[:, 0:n], bones.t[:, :], sq.t[:, 0:n], True, True, [bones, sq], [b2])
                    b3 = k.bank()
                    mm(k, b3.t[:, 0:n], pswap.t[:, :], qg.t[:, 0:n], True, True, [pswap, qg], [b3])
                    act(k, rs.t[:, 0:n], b2.t[:, 0:n], AF.Ln, [b2], [rs], scale=1.0 / 64, bias=EPS)
                    act(k, rs.t[:, 0:n], rs.t[:, 0:n], AF.Exp, [rs], [rs], scale=-0.5)
                    tt(k, "dve", t1.t[:, 0:n], qg.t[:, 0:n], ctab.t[:, h0:h0 + n], ALU.mult, [qg, ctab], [t1])
                    tt(k, "dve", sq.t[:, 0:n], b3.t[:, 0:n], stab.t[:, h0:h0 + n], ALU.mult, [b3, stab], [sq])
                    tt(k, "dve", t1.t[:, 0:n], t1.t[:, 0:n], sq.t[:, 0:n], ALU.add, [t1, sq], [t1])
                    tt(k, "dve", dst.t[:, ch, d0:d0 + n], t1.t[:, 0:n], rs.t[:, 0:n], ALU.mult, [t1, rs], [dst])
                un += len(units)
                if ATTN_STAGE < 1:
                    continue
                k.op("pool", lambda h: h.memset(VE.t[:, :, :, 64:66], 1.0), [], [VE])
                for b in range(nKB):
                    bk = k.bank()
                    for kc in range(KC):
                        mm(k, bk.t[:, :], hT.t[:, kc, b * 128:(b + 1) * 128], wq.t[:, kc, 1024:1536], kc == 0,
                           kc == KC - 1, [hT, wq], [bk])
                    cp(k, "act", VE.t[:, b, :, 0:64], bk.t[:, :].rearrange("p (h e) -> p h e", e=64), [bk], [VE])
                if w_ == 0:
                    fc = 1 if slot == 0 else 0
                    ts(k, "pool", VE.t[0:64, 0, :, :], VE.t[0:64, 0, :, :], c.s_flags.t[0:64, fc:fc + 1], None,
                       ALU.mult, None, [VE, c.s_flags], [VE])
                if w_ == ntr - 1:
                    fc = 1 if slot == NS - 1 else 0
                    ts(k, "pool", VE.t[64:128, nKB - 1, :, :], VE.t[64:128, nKB - 1, :, :],
                       c.s_flags.t[64:128, fc:fc + 1], None, ALU.mult, None, [VE, c.s_flags], [VE])
                if ATTN_STAGE < 2:
                    continue
                for b in range(nKB):
                    qlo = max(b - 1, 0)
                    qhi = min(b + 1, nQB)
                    wd_ = (qhi - qlo) * 128
                    if b == 0:
                        mb = bb.t[:, 128:256]
                    elif b == nQB:
                        mb = bb.t[:, 0:128]
                    else:
                        mb = bb.t[:, 0:256]
                    for hp in range(4):
                        bk = k.bank()
                        for hh in range(2):
                            pb = 64 * hh
                            o = bk.t[:, hh * 256:hh * 256 + wd_]
                            mm(k, o, idb.t[:, :], mb, True, False, [idb, bb], [bk], inc=False)
                            mm(k, o, KT.t[pb:pb + 64, hp, b * 128:(b + 1) * 128],
                               QT.t[pb:pb + 64, hp, qlo * 128:qhi * 128], False, True, [KT, QT], [bk])
                        src = bk.t[:, :].rearrange("p (h q) -> p h q", q=256)[:, :, 0:wd_]
                        act(k, PT.t[:, b, 2 * hp:2 * hp + 2, 0:wd_], src, AF.Exp, [bk], [PT])
                if ATTN_STAGE < 3:
                    continue
                for qb in range(nQB):
                    oa = k.bank()
                    ob = k.bank()
                    for h_ in range(8):
                        bk = oa if h_ < 4 else ob
                        o = bk.t[:, (h_ % 4) * 66:(h_ % 4) * 66 + 66]
                        for b in (qb, qb + 1):
                            off = 0 if (b == qb + 1 or b == 0) else 128
                            mm(k, o, PT.t[:, b, h_, off:off + 128], VE.t[:, b, h_, :], b == qb, b == qb + 1,
                               [PT, VE], [bk], inc=(b == qb + 1 and h_ % 4 == 3))
                    os_ = osb[on_ % 2]
                    oc_ = oac[on_ % 2]
                    cp(k, "act", os_.t[:, 0:264], oa.t[:, 0:264], [oa], [os_])
                    cp(k, "dve", os_.t[:, 264:528], ob.t[:, 0:264], [ob], [os_])
                    qrow = PAD + slot * SLOT + r + d * (j0 + qb * 128)
                    orows = c.OACC[qrow:qrow + d * 127 + 1:d, :]
                    if gi == 0:
                        k.dma(orows, os_.t[:], [os_], [], os_)
                    else:
                        k.dma(oc_.t[:], orows, [], [oc_], oc_)
                        tt(k, "dve", os_.t[:], os_.t[:], oc_.t[:], ALU.add, [os_, oc_], [os_])
                        if not last:
                            k.dma(orows, os_.t[:], [os_], [], os_)
                    if last:
                        xr = xres[on_ % 2]
                        k.dma(xr.t[:], Xr[qrow:qrow + d * 127 + 1:d, :], [], [xr], xr)
                        ov = os_.t[:].rearrange("p (h e) -> p h e", e=66)
                        k.op("dve", lambda h: h.reciprocal(out=rden.t[:], in_=ov[:, :, 64]), [os_], [rden])
                        for h_ in range(8):
                            ts(k, "dve", on.t[:, h_ * 64:(h_ + 1) * 64], ov[:, h_, 0:64],
                               rden.t[:, h_:h_ + 1], None, ALU.mult, None, [os_, rden], [on])
                        bk = k.bank()
                        for kc in range(4):
                            k.op("pe", lambda h, kc=kc: h.transpose(out=bk.t[:, kc * 128:(kc + 1) * 128],
                                                                     in_=on.t[:, kc * 128:(kc + 1) * 128],
                                                                     identity=c.s_ident.t[:]),
                                 [on, c.s_ident], [bk], inc=(kc == 3))
                        cp(k, "act", oT.t[:].rearrange("p k t -> p (k t)"), bk.t[:, :], [bk], [oT])
                        for half in range(2):
                            bk = k.bank()
                            for kc in range(4):
                                mm(k, bk.t[:, :], oT.t[:, kc, :], wo.t[:, kc, half * 512:(half + 1) * 512], kc == 0,
                                   kc == 3, [oT, wo], [bk])
                            tm_ = tmo[half]
                            tt(k, "dve", tm_.t[:], bk.t[:, :], gb_.t[:, half * 512:(half + 1) * 512], ALU.mult,
                               [bk, gb_], [tm_])
                            tt(k, "pool", xr.t[:, half * 512:(half + 1) * 512], xr.t[:, half * 512:(half + 1) * 512],
                               tm_.t[:], ALU.add, [xr, tm_], [xr])
                        k.dma(Xw[qrow:qrow + d * 127 + 1:d, :], xr.t[:], [xr], [], xr)
                    on_ += 1
    k.end()


def phase_out(c, cur):
    k = c.k
    k.begin()
    z = k.sb("zo", [128, 8], F32)
    T = c.T
    for r0 in range(0, T, 512):
        k.dma(c.y[r0:r0 + 512, :], c.X[cur][PAD + r0:PAD + r0 + 512, :], [], [], z)
    k.end()


def _pm(w, kc):
    n = w.shape[-1]
    return np.ascontiguousarray(w.reshape(kc, 128, n).transpose(1, 0, 2)).reshape(128, kc * n)


def prep_weights(inp):
    f = np.float32
    W = {}
    ada_w = np.asarray(inp["ada_w"], f)
    W["ada_w"] = np.ascontiguousarray(ada_w.reshape(4, KC, 128, 6 * D).transpose(0, 2, 1, 3))
    ada_b = np.asarray(inp["ada_b"], f)
    W["ada_bT"] = np.ascontiguousarray(ada_b.reshape(4, 48, 128).transpose(2, 0, 1))
    brep = np.stack([ada_b[:, 2 * D:3 * D], ada_b[:, 5 * D:6 * D]], axis=1)
    W["ada_brep"] = np.ascontiguousarray(np.broadcast_to(brep[:, :, None, :], (4, 2, 128, D)))
    nm = np.stack([np.asarray(inp["norm_mix"], f), np.asarray(inp["norm_ffn"], f)], axis=1)
    W["nrm"] = np.ascontiguousarray(nm.reshape(4, 2, KC, 128).transpose(3, 0, 1, 2))
    cw = np.asarray(inp["rg_conv_w"], f)
    cb = np.asarray(inp["rg_conv_b"], f)
    cc = np.concatenate([cw, cb[:, None, :]], axis=1)
    W["rg_conv"] = np.ascontiguousarray(cc.reshape(2, 5, KC, 128).transpose(3, 0, 2, 1))
    gb = np.asarray(inp["rg_gate_b"], f)
    W["rg_gb"] = np.ascontiguousarray(gb.reshape(2, 2, 2, KC, 128).transpose(4, 0, 1, 2, 3))
    lam = np.asarray(inp["rg_lambda"], f)
    W["rg_lam"] = np.ascontiguousarray(lam.reshape(2, 2, KC, 128).transpose(3, 0, 1, 2))
    qn = np.asarray(inp["at_q_norm"], f)
    kn = np.asarray(inp["at_k_norm"], f)
    qk = np.stack([qn, kn], axis=2)
    W["at_qkn"] = np.ascontiguousarray(np.concatenate([qk, qk], axis=1).transpose(1, 0, 2))
    rt = np.asarray(inp["moe_router"], f)
    W["moe_rt"] = np.ascontiguousarray(rt.reshape(2, KC, 128, NEXP).transpose(2, 0, 1, 3))
    W["rg_w_in"] = np.stack([_pm(np.asarray(inp["rg_w_in"][j], f), KC) for j in range(2)])
    gw = np.asarray(inp["rg_gate_w"], f)
    W["rg_gate_w"] = np.ascontiguousarray(
        gw.reshape(2, 2, 2, 4, 2, 128, 256).transpose(0, 5, 1, 2, 3, 4, 6)).reshape(2, 128, 8192)
    W["rg_w_out"] = np.stack([_pm(np.asarray(inp["rg_w_out"][j], f), KC) for j in range(2)])
    qkv = np.asarray(inp["at_w_qkv"], f).reshape(2, D, 3, 1536)
    W["at_qkv"] = np.stack([_pm(np.ascontiguousarray(qkv[j, :, g, :]), KC) for j in range(2) for g in range(3)])
    W["at_wo"] = np.stack([_pm(np.asarray(inp["at_w_o"][j], f), 4) for j in range(2)])

    def gu_blocks(w, nff):
        dff = nff * 128
        g = w[:, :dff].reshape(KC, 128, nff, 128)
        u = w[:, dff:].reshape(KC, 128, nff, 128)
        gu_ = np.stack([g, u], axis=3)
        return np.ascontiguousarray(gu_.transpose(2, 1, 0, 3, 4)).reshape(nff, 128, KC * 256)

    W["ff_gu"] = np.concatenate([gu_blocks(np.asarray(inp["ff_w_gu"][j], f), 22) for j in range(2)])
    W["ff_dn"] = np.asarray(inp["ff_w_down"], f).reshape(2 * 22, 128, D)
    mg = np.asarray(inp["moe_w_gu"], f)
    md = np.asarray(inp["moe_w_down"], f)
    for j in range(2):
        for e in range(NEXP):
            W["moe_gu%d" % (j * NEXP + e)] = gu_blocks(mg[j, e], 28)
            W["moe_dn%d" % (j * NEXP + e)] = np.ascontiguousarray(md[j, e]).reshape(28, 128, D)
    W["ident"] = np.eye(128, dtype=f)
    bo = np.zeros((128, 128), f)
    bo[:64, :64] = 1
    bo[64:, 64:] = 1
    W["bones"] = bo
    ps = np.zeros((128, 128), f)
    for hb in (0, 64):
        for i in range(8):
            ps[hb + 8 + i, hb + i] = 1.0
            ps[hb + i, hb + 8 + i] = 1.0
    W["pswap"] = ps
    kk = np.arange(128)[:, None]
    cc_ = np.arange(256)[None, :]
    W["bandb"] = np.where((cc_ - kk >= 0) & (cc_ - kk <= 128), 0.0, -30000.0).astype(f)
    return W


def rope_tables(NSLOT, chained):
    f = np.float32
    T = NSLOT * SLOT
    inv = (500000.0 ** (-np.arange(0, 16, 2, dtype=np.float32) / 16)).astype(f)
    out = np.zeros((3, 2, 128, T + 128), f)
    out[:, 0] = 1.0
    dd = np.arange(128) % 64
    for gi, (_, d) in enumerate(GROUPS):
        L = SLOT // d
        r = np.arange(d)[:, None, None]
        s = np.arange(NSLOT)[None, :, None]
        jx = np.arange(L)[None, None, :]
        pos = (r + d * jx + (s * SLOT if chained else 0 * s)).astype(f).reshape(-1)
        ang = pos[None, :] * inv[:, None]
        cs = np.cos(ang).astype(f)
        sn = np.sin(ang).astype(f)
        for p in range(128):
            dq = dd[p]
            if dq < 8:
                out[gi, 0, p, 64:64 + T] = cs[dq]
                out[gi, 1, p, 64:64 + T] = -sn[dq]
            elif dq < 16:
                out[gi, 0, p, 64:64 + T] = cs[dq - 8]
                out[gi, 1, p, 64:64 + T] = sn[dq - 8]
    return out


def core_inputs(W, x, cvec, chained, NSLOT):
    f = np.float32
    m = dict(W)
    m["x_in"] = np.ascontiguousarray(x, dtype=f)
    m["cT"] = np.ascontiguousarray(np.asarray(cvec, f).reshape(NSLOT, KC, 128).transpose(2, 1, 0))
    fl = np.zeros((128, 2), f)
    fl[:, 0] = 1.0 if chained else 0.0
    m["flags"] = fl
    m["rope"] = rope_tables(NSLOT, chained)
    return m


_NC_CACHE = {}


def kernel(**inputs):
    NSLOT = 4
    W = prep_weights(inputs)
    xp = np.asarray(inputs["x_prompt"], np.float32)
    xs = np.asarray(inputs["x_sample"], np.float32)
    cp_ = np.asarray(inputs["c_prompt"], np.float32)
    cs = np.asarray(inputs["c_sample"], np.float32)
    in_maps = []
    in_maps.append(core_inputs(W, xp[0], np.repeat(cp_, NSLOT, axis=0), True, NSLOT))
    for ci in range(4):
        in_maps.append(core_inputs(W, xs[4 * ci:4 * ci + 4].reshape(NSLOT * SLOT, D), cs[4 * ci:4 * ci + 4], False, NSLOT))
    for ci in range(3):
        in_maps.append(in_maps[1 + ci])
    if "nc" not in _NC_CACHE:
        _NC_CACHE["nc"] = build(NSLOT)
    nc = _NC_CACHE["nc"]
    res = run_bass_kernel_spmd(nc, in_maps, core_ids=list(range(8)))
    yp = res.results[0]["y"].reshape(1, 4 * SLOT, D).astype(np.float32)
    ys = np.concatenate([res.results[1 + ci]["y"].reshape(4, SLOT, D) for ci in range(4)], axis=0).astype(np.float32)
    return (yp, ys)
```
